# Optimizing a Trainium2 kernel written in Bass

```python
import math
import jax
import jax.numpy as jnp
from jax import lax
import numpy as np

D_MODEL = 1024
BATCH = 8
SEQ = 4096
DEPTH = 4

CTX_LEN = 256
GRID_W = 64
Q_BLOCK = 128
ROPE_THETA = 10000.0
NORM_EPS = 1e-6

HY_WIDTH = D_MODEL // 4
HY_SHORT = 3
HY_EMB = 33
HY_FILTER_HIDDEN = 64
HY_FAST_DECAY = 0.3
HY_SLOW_DECAY = 1.5
HY_DECAY_TARGET = 1e-2

DIFF_HEADS = 4
DIFF_WIDTH = D_MODEL // 4
DIFF_V_DIM = DIFF_WIDTH // DIFF_HEADS
DIFF_QK_DIM = DIFF_V_DIM // 2

GQA_HEAD_DIM = 64
GQA_WIDTH = D_MODEL // 2
GQA_HEADS = GQA_WIDTH // GQA_HEAD_DIM
GQA_KV_HEADS = GQA_HEADS // 4
GQA_REP = GQA_HEADS // GQA_KV_HEADS

MIX_WIDTH = HY_WIDTH + DIFF_WIDTH + GQA_WIDTH

OFF_DQ = 3 * HY_WIDTH
OFF_DK = OFF_DQ + 2 * DIFF_HEADS * DIFF_QK_DIM
OFF_DV = OFF_DK + 2 * DIFF_HEADS * DIFF_QK_DIM
OFF_GQ = OFF_DV + DIFF_WIDTH
OFF_GK = OFF_GQ + GQA_WIDTH
OFF_GV = OFF_GK + GQA_KV_HEADS * GQA_HEAD_DIM
IN_COLS = OFF_GV + GQA_KV_HEADS * GQA_HEAD_DIM
IN_SPLITS = [OFF_DQ, OFF_DK, OFF_DV, OFF_GQ, OFF_GK, OFF_GV]

D_FF = 256 * math.ceil(8 * D_MODEL / 3 / 256)
N_EXPERTS = 8
TOP_K = 2
D_FF_EXPERT = 7 * D_MODEL // 2

kernel_name = 'hybrid_diffusion_trunk'


def rms_norm(x, g):
    xf = x.astype(jnp.float32)
    y = xf * lax.rsqrt(jnp.mean(xf * xf, axis=-1, keepdims=True) + NORM_EPS)
    return y.astype(x.dtype) * g


def axial_rope_tables(length, head_dim, dtype):
    rows = length // GRID_W
    t = jnp.arange(rows * GRID_W)
    row = (t // GRID_W).astype(jnp.float32)
    col = (t % GRID_W).astype(jnp.float32)
    n = head_dim // 4
    inv = ROPE_THETA ** (-jnp.arange(n, dtype=jnp.float32) / n)
    ang = jnp.concatenate([row[:, None] * inv, col[:, None] * inv], axis=-1)
    return jnp.cos(ang).astype(dtype), jnp.sin(ang).astype(dtype)


def apply_rope(x, cos, sin):
    shape = (1, cos.shape[0]) + (1,) * (x.ndim - 3) + (cos.shape[1],)
    cos = cos.reshape(shape)
    sin = sin.reshape(shape)
    x1, x2 = jnp.split(x, 2, axis=-1)
    return jnp.concatenate([x1 * cos - x2 * sin, x1 * sin + x2 * cos], axis=-1)


def sweep_query_blocks(fn, q):
    b, l = q.shape[:2]
    nb = l // Q_BLOCK
    qb = jnp.moveaxis(q.reshape((b, nb, Q_BLOCK) + q.shape[2:]), 1, 0)
    ob = lax.map(fn, qb)
    return jnp.moveaxis(ob, 0, 1).reshape((b, l) + ob.shape[3:])


def diff_attention(q, k, v, lam):
    scale = DIFF_QK_DIM ** -0.5

    def block(qb):
        s = jnp.einsum('bqhmd,bkhmd->bhmqk', qb, k) * scale
        p = jax.nn.softmax(s.astype(jnp.float32), axis=-1)
        a = (p[:, :, 0] - lam * p[:, :, 1]).astype(v.dtype)
        return jnp.einsum('bhqk,bkhd->bqhd', a, v)

    return sweep_query_blocks(block, q)


def gqa_attention(q, k, v):
    scale = GQA_HEAD_DIM ** -0.5

    def block(qb):
        s = jnp.einsum('bqgrd,bkgd->bgrqk', qb, k) * scale
        p = jax.nn.softmax(s.astype(jnp.float32), axis=-1).astype(v.dtype)
        return jnp.einsum('bgrqk,bkgd->bqgrd', p, v)

    return sweep_query_blocks(block, q)


def short_conv(z, w, b):
    l = z.shape[1]
    pad = HY_SHORT // 2
    zp = jnp.pad(z, ((0, 0), (pad, HY_SHORT - 1 - pad), (0, 0)))
    y = b
    for j in range(HY_SHORT):
        y = y + zp[:, j:j + l] * w[j]
    return y


def hyena_filter(length, w1, b1, w2, b2, w3, freq):
    t = jnp.linspace(0.0, 1.0, length, dtype=jnp.float32)[:, None]
    bands = (HY_EMB - 1) // 2
    ang = (2.0 * math.pi / length) * jnp.arange(length, dtype=jnp.float32)[:, None] \
        * jnp.linspace(1e-4, bands - 1, bands, dtype=jnp.float32)
    feats = jnp.concatenate([t, jnp.cos(ang), -jnp.sin(ang)], axis=-1)
    a = jnp.sin(freq * (feats @ w1 + b1))
    a = jnp.sin(freq * (a @ w2 + b2))
    hfilt = (a @ w3).astype(jnp.float32)
    max_decay = math.log(HY_DECAY_TARGET) / HY_FAST_DECAY
    min_decay = math.log(HY_DECAY_TARGET) / HY_SLOW_DECAY
    deltas = jnp.abs(jnp.linspace(min_decay, max_decay, HY_WIDTH, dtype=jnp.float32))
    decay = jnp.exp(-t * deltas)
    h_fwd = hfilt[:, :HY_WIDTH] * decay
    h_bwd = hfilt[:, HY_WIDTH:] * decay
    zero = jnp.zeros((1, HY_WIDTH), jnp.float32)
    return jnp.concatenate([h_fwd, zero, h_bwd[1:][::-1]], axis=0)


def bidir_long_conv(v, h_circ):
    l = v.shape[1]
    vf = jnp.fft.rfft(v.astype(jnp.float32), n=2 * l, axis=1)
    hf = jnp.fft.rfft(h_circ, n=2 * l, axis=0)
    y = jnp.fft.irfft(vf * hf[None], n=2 * l, axis=1)[:, :l]
    return y.astype(v.dtype)


def hyena_mixer(z, conv_w, conv_b, w1, b1, w2, b2, w3, freq, skip):
    z = short_conv(z, conv_w, conv_b)
    x0, x1, v = jnp.split(z, 3, axis=-1)
    h_circ = hyena_filter(z.shape[1], w1, b1, w2, b2, w3, freq)
    v = v * x1
    v = bidir_long_conv(v, h_circ) + v * skip
    return v * x0


def merge_head_groups(y_hy, o_diff, o_gqa, subln_g, lam_init):
    b, l = y_hy.shape[:2]
    o_diff = rms_norm(o_diff, subln_g) * (1.0 - lam_init)
    return jnp.concatenate([y_hy, o_diff.reshape(b, l, -1), o_gqa.reshape(b, l, -1)], axis=-1)


def swiglu(t, wg, wu, wd):
    return (jax.nn.silu(t @ wg) * (t @ wu)) @ wd


def moe_swiglu(t, router, wg, wu, wd):
    logits = (t @ router).astype(jnp.float32)
    top_val, top_idx = lax.top_k(logits, TOP_K)
    top_w = jax.nn.softmax(top_val, axis=-1)
    gates = jnp.sum(jax.nn.one_hot(top_idx, N_EXPERTS, dtype=jnp.float32) * top_w[..., None],
                    axis=1).astype(t.dtype)
    out = jnp.zeros_like(t)
    for e in range(N_EXPERTS):
        out = out + gates[:, e:e + 1] * swiglu(t, wg[e], wu[e], wd[e])
    return out


def setup_inputs(seed: int = 0) -> dict:
    key = jax.random.key(seed)
    keys = jax.random.split(key, 48)
    count = [0]

    def normal(shape, scale):
        k = keys[count[0]]
        count[0] += 1
        return jax.random.normal(k, shape, jnp.float32) * scale

    def gain(shape):
        return 1.0 + normal(shape, 0.05)

    d = D_MODEL
    n_dense = (DEPTH + 1) // 2
    n_moe = DEPTH // 2
    return {
        'x': normal((BATCH, SEQ, d), 1.0),
        'c': normal((BATCH, d), 1.0),
        'ctx': normal((BATCH, CTX_LEN, d), 1.0),
        'c_ctx': normal((d,), 1.0),
        'w_ada': normal((DEPTH, d, 6 * d), 0.5 * d ** -0.5),
        'b_ada': normal((DEPTH, 6 * d), 0.02),
        'g_mix': gain((DEPTH, d)),
        'g_ffn': gain((DEPTH, d)),
        'w_in': normal((DEPTH, d, IN_COLS), d ** -0.5),
        'w_out': normal((DEPTH, MIX_WIDTH, d), MIX_WIDTH ** -0.5),
        'hy_conv_w': normal((DEPTH, HY_SHORT, 3 * HY_WIDTH), HY_SHORT ** -0.5),
        'hy_conv_b': normal((DEPTH, 3 * HY_WIDTH), 0.02),
        'hy_w1': normal((DEPTH, HY_EMB, HY_FILTER_HIDDEN), HY_EMB ** -0.5),
        'hy_b1': normal((DEPTH, HY_FILTER_HIDDEN), 0.1),
        'hy_w2': normal((DEPTH, HY_FILTER_HIDDEN, HY_FILTER_HIDDEN), HY_FILTER_HIDDEN ** -0.5),
        'hy_b2': normal((DEPTH, HY_FILTER_HIDDEN), 0.1),
        'hy_w3': normal((DEPTH, HY_FILTER_HIDDEN, 2 * HY_WIDTH), 0.005),
        'hy_freq': gain((DEPTH, HY_FILTER_HIDDEN)),
        'hy_skip': normal((DEPTH, HY_WIDTH), 1.0),
        'diff_lq1': normal((DEPTH, DIFF_QK_DIM), 0.1),
        'diff_lk1': normal((DEPTH, DIFF_QK_DIM), 0.1),
        'diff_lq2': normal((DEPTH, DIFF_QK_DIM), 0.1),
        'diff_lk2': normal((DEPTH, DIFF_QK_DIM), 0.1),
        'diff_subln': gain((DEPTH, DIFF_V_DIM)),
        'gqa_qnorm': gain((DEPTH, GQA_HEAD_DIM)),
        'gqa_knorm': gain((DEPTH, GQA_HEAD_DIM)),
        'ffn_wg': normal((n_dense, d, D_FF), d ** -0.5),
        'ffn_wu': normal((n_dense, d, D_FF), d ** -0.5),
        'ffn_wd': normal((n_dense, D_FF, d), D_FF ** -0.5),
        'moe_router': normal((n_moe, d, N_EXPERTS), d ** -0.5),
        'moe_wg': normal((n_moe, N_EXPERTS, d, D_FF_EXPERT), d ** -0.5),
        'moe_wu': normal((n_moe, N_EXPERTS, d, D_FF_EXPERT), d ** -0.5),
        'moe_wd': normal((n_moe, N_EXPERTS, D_FF_EXPERT, d), D_FF_EXPERT ** -0.5),
        'g_final': gain((d,)),
    }


def reference(x, c, ctx, c_ctx, w_ada, b_ada, g_mix, g_ffn, w_in, w_out,
              hy_conv_w, hy_conv_b, hy_w1, hy_b1, hy_w2, hy_b2, hy_w3, hy_freq, hy_skip,
              diff_lq1, diff_lk1, diff_lq2, diff_lk2, diff_subln, gqa_qnorm, gqa_knorm,
              ffn_wg, ffn_wu, ffn_wd, moe_router, moe_wg, moe_wu, moe_wd, g_final):
    b, seq, d = x.shape
    n_ctx = ctx.shape[1]
    cos_d, sin_d = axial_rope_tables(seq, DIFF_QK_DIM, x.dtype)
    cos_g, sin_g = axial_rope_tables(seq, GQA_HEAD_DIM, x.dtype)
    silu_c = jax.nn.silu(c)
    silu_cc = jax.nn.silu(c_ctx)
    h, hc = x, ctx
    for i in range(DEPTH):
        last = i == DEPTH - 1
        lam_init = 0.8 - 0.6 * math.exp(-0.3 * i)
        lam = (jnp.exp(jnp.sum(diff_lq1[i].astype(jnp.float32) * diff_lk1[i].astype(jnp.float32)))
               - jnp.exp(jnp.sum(diff_lq2[i].astype(jnp.float32) * diff_lk2[i].astype(jnp.float32)))
               + lam_init)
        hy_params = (hy_conv_w[i], hy_conv_b[i], hy_w1[i], hy_b1[i], hy_w2[i], hy_b2[i],
                     hy_w3[i], hy_freq[i], hy_skip[i])

        sh_m, sc_m, gt_m, sh_f, sc_f, gt_f = jnp.split(
            (silu_c @ w_ada[i] + b_ada[i])[:, None, :], 6, axis=-1)
        csh_m, csc_m, cgt_m, csh_f, csc_f, cgt_f = jnp.split(
            silu_cc @ w_ada[i] + b_ada[i], 6, axis=-1)

        u = rms_norm(h, g_mix[i]) * (1.0 + sc_m) + sh_m
        uc = rms_norm(hc, g_mix[i]) * (1.0 + csc_m) + csh_m
        z_hy, z_dq, z_dk, z_dv, z_gq, z_gk, z_gv = jnp.split(u @ w_in[i], IN_SPLITS, axis=-1)
        if last:
            zc_dk, zc_dv = jnp.split(uc @ w_in[i][:, OFF_DK:OFF_GQ], 2, axis=-1)
            zc_gk, zc_gv = jnp.split(uc @ w_in[i][:, OFF_GK:], 2, axis=-1)
        else:
            zc_hy, zc_dq, zc_dk, zc_dv, zc_gq, zc_gk, zc_gv = jnp.split(
                uc @ w_in[i], IN_SPLITS, axis=-1)

        kc_d = zc_dk.reshape(b, n_ctx, DIFF_HEADS, 2, DIFF_QK_DIM)
        vc_d = zc_dv.reshape(b, n_ctx, DIFF_HEADS, DIFF_V_DIM)
        kc_g = rms_norm(zc_gk.reshape(b, n_ctx, GQA_KV_HEADS, GQA_HEAD_DIM), gqa_knorm[i])
        vc_g = zc_gv.reshape(b, n_ctx, GQA_KV_HEADS, GQA_HEAD_DIM)

        q_d = apply_rope(z_dq.reshape(b, seq, DIFF_HEADS, 2, DIFF_QK_DIM), cos_d, sin_d)
        k_d = apply_rope(z_dk.reshape(b, seq, DIFF_HEADS, 2, DIFF_QK_DIM), cos_d, sin_d)
        v_d = z_dv.reshape(b, seq, DIFF_HEADS, DIFF_V_DIM)
        q_g = apply_rope(rms_norm(z_gq.reshape(b, seq, GQA_KV_HEADS, GQA_REP, GQA_HEAD_DIM),
                                  gqa_qnorm[i]), cos_g, sin_g)
        k_g = apply_rope(rms_norm(z_gk.reshape(b, seq, GQA_KV_HEADS, GQA_HEAD_DIM),
                                  gqa_knorm[i]), cos_g, sin_g)
        v_g = z_gv.reshape(b, seq, GQA_KV_HEADS, GQA_HEAD_DIM)

        y_hy = hyena_mixer(z_hy, *hy_params)
        o_d = diff_attention(q_d, jnp.concatenate([k_d, kc_d], axis=1),
                             jnp.concatenate([v_d, vc_d], axis=1), lam)
        o_g = gqa_attention(q_g, jnp.concatenate([k_g, kc_g], axis=1),
                            jnp.concatenate([v_g, vc_g], axis=1))
        y = merge_head_groups(y_hy, o_d, o_g, diff_subln[i], lam_init) @ w_out[i]
        h = h + gt_m * y
        if not last:
            qc_d = zc_dq.reshape(b, n_ctx, DIFF_HEADS, 2, DIFF_QK_DIM)
            qc_g = rms_norm(zc_gq.reshape(b, n_ctx, GQA_KV_HEADS, GQA_REP, GQA_HEAD_DIM),
                            gqa_qnorm[i])
            yc = merge_head_groups(hyena_mixer(zc_hy, *hy_params),
                                   diff_attention(qc_d, kc_d, vc_d, lam),
                                   gqa_attention(qc_g, kc_g, vc_g),
                                   diff_subln[i], lam_init) @ w_out[i]
            hc = hc + cgt_m * yc

        u = rms_norm(h, g_ffn[i]) * (1.0 + sc_f) + sh_f
        tokens = u.reshape(-1, d)
        n_c = 0
        if not last:
            uc = rms_norm(hc, g_ffn[i]) * (1.0 + csc_f) + csh_f
            tokens = jnp.concatenate([uc.reshape(-1, d), tokens], axis=0)
            n_c = b * n_ctx
        if i % 2 == 0:
            out = swiglu(tokens, ffn_wg[i // 2], ffn_wu[i // 2], ffn_wd[i // 2])
        else:
            out = moe_swiglu(tokens, moe_router[i // 2], moe_wg[i // 2], moe_wu[i // 2],
                             moe_wd[i // 2])
        h = h + gt_f * out[n_c:].reshape(h.shape)
        if not last:
            hc = hc + cgt_f * out[:n_c].reshape(hc.shape)
    return rms_norm(h, g_final)
```

```python
import math
from contextlib import ExitStack

import numpy as np
import ml_dtypes
import concourse.bass as bass
import concourse.mybir as mybir
from concourse.bass_utils import run_bass_kernel_spmd

F32 = mybir.dt.float32
BF16 = mybir.dt.bfloat16
AF = mybir.ActivationFunctionType
ALU = mybir.AluOpType
AX = mybir.AxisListType

D = 1024
NJ = 8
SEQ = 4096
NCX = 256
NT = SEQ + NCX
DEPTH = 4
GRID_W = 64
EPS = 1e-6
DFF = 2816
DFE = 3584
NEXP = 8
OFF_DQ, OFF_DK, OFF_DV, OFF_GQ, OFF_GK, OFF_GV, IN_COLS = 768, 1024, 1280, 1536, 2048, 2176, 2304
PI = float(np.pi)


class Buf:
    __slots__ = ("name", "w", "r", "excl")

    def __init__(self, name="", excl=False):
        self.name = name
        self.w = None
        self.r = {}
        self.excl = excl


class _Eng:
    def __init__(self, name, eng, sem):
        self.name = name
        self.eng = eng
        self.sem = sem
        self.tick = 0
        self.pending = False
        self.seen = {}


class FW:
    NDMA = 32

    def __init__(self, nc, stack):
        self.nc = nc
        self.engs = {}
        for nm, e in (("pe", nc.tensor), ("dve", nc.vector), ("act", nc.scalar),
                      ("pool", nc.gpsimd), ("sp", nc.sync)):
            sem = stack.enter_context(nc.semaphore("s_" + nm))
            self.engs[nm] = _Eng(nm, e, sem)
        self.dma_sems = [stack.enter_context(nc.semaphore(f"s_dma{i}")) for i in range(self.NDMA)]
        self.dma_cnt = [0] * self.NDMA
        self.dma_next = 0
        self.n_inst = 0

    def _wait(self, E, prod, tick):
        if E.seen.get(prod, 0) >= tick:
            return
        if prod == E.name and tick > E.tick:
            return
        E.seen[prod] = tick
        if prod.startswith("dma"):
            sem = self.dma_sems[int(prod[3:])]
        else:
            sem = self.engs[prod].sem
        E.eng.wait_ge(sem, tick)

    def _deps(self, E, reads, writes, same):
        for b in reads:
            if b.w is not None and (same or b.w[0] != E.name):
                self._wait(E, *b.w)
            if b.excl:
                for p, t in b.r.items():
                    if p != E.name:
                        self._wait(E, p, t)
        for b in writes:
            if b.w is not None and (same or b.w[0] != E.name):
                self._wait(E, *b.w)
            for p, t in b.r.items():
                if p != E.name:
                    self._wait(E, p, t)

    @staticmethod
    def _mark(prod, tick, reads, writes):
        for b in reads:
            if b.r.get(prod, 0) < tick:
                b.r[prod] = tick
        for b in writes:
            b.w = (prod, tick)
            b.r = {}

    def op(self, engname, fn, reads=(), writes=(), same=True, inc=True):
        E = self.engs[engname]
        self._deps(E, reads, writes, same)
        ins = fn(E.eng)
        if inc:
            E.tick += 1
            E.pending = False
            ins.then_inc(E.sem, 1)
            self._mark(E.name, E.tick, reads, writes)
        else:
            E.pending = True
            self._mark(E.name, E.tick + 1, reads, writes)
        self.n_inst += 1
        return ins

    def dma(self, qname, out, in_, reads=(), writes=(), **kw):
        E = self.engs[qname]
        slot = self.dma_next
        self.dma_next = (self.dma_next + 1) % self.NDMA
        pname = f"dma{slot}"
        if self.dma_cnt[slot] > 0:
            self._wait(E, pname, 16 * self.dma_cnt[slot])
        self._deps(E, reads, writes, True)
        ins = E.eng.dma_start(out=out, in_=in_, **kw)
        self.dma_cnt[slot] += 1
        ins.then_inc(self.dma_sems[slot], 16)
        self._mark(pname, 16 * self.dma_cnt[slot], reads, writes)
        self.n_inst += 1
        return ins

    def _flush_pending(self):
        for nm, E in self.engs.items():
            if E.pending:
                E.tick += 1
                E.pending = False
                E.eng.nop().then_inc(E.sem, 1)

    def barrier(self):
        self._flush_pending()
        for nm, E in self.engs.items():
            for nm2, E2 in self.engs.items():
                if nm2 != nm and E2.tick:
                    self._wait(E, nm2, E2.tick)
            for s in range(self.NDMA):
                if self.dma_cnt[s]:
                    self._wait(E, f"dma{s}", 16 * self.dma_cnt[s])

    def finish(self):
        self._flush_pending()
        E = self.engs["sp"]
        for s in range(self.NDMA):
            if self.dma_cnt[s]:
                self._wait(E, f"dma{s}", 16 * self.dma_cnt[s])
        for nm, e in self.engs.items():
            if nm != "sp" and e.tick:
                self._wait(E, nm, e.tick)


def _rope_tables(head_dim):
    t = np.arange(SEQ)
    row = (t // GRID_W).astype(np.float32)
    col = (t % GRID_W).astype(np.float32)
    n = head_dim // 4
    inv = (10000.0 ** (-np.arange(n, dtype=np.float32) / n)).astype(np.float32)
    ang = np.concatenate([row[:, None] * inv, col[:, None] * inv], axis=-1).astype(np.float32)
    cos = np.cos(ang).astype(np.float32)
    sin = np.sin(ang).astype(np.float32)
    half = head_dim // 2
    C = np.ones((128, NT), np.float32)
    S = np.zeros((128, NT), np.float32)
    for p in range(128):
        dd = p % head_dim
        a = dd % half
        C[p, NCX:] = cos[:, a]
        S[p, NCX:] = -sin[:, a] if dd < half else sin[:, a]
    return C, S


def _hyena_tables(L):
    t = np.linspace(0.0, 1.0, L, dtype=np.float32)[:, None]
    bands = 16
    ang = (np.float32(2.0 * math.pi / L) * np.arange(L, dtype=np.float32)[:, None]
           * np.linspace(1e-4, bands - 1, bands, dtype=np.float32)).astype(np.float32)
    feats = np.concatenate([t, np.cos(ang), -np.sin(ang)], axis=-1).astype(np.float32)
    max_decay = math.log(1e-2) / 0.3
    min_decay = math.log(1e-2) / 1.5
    deltas = np.abs(np.linspace(min_decay, max_decay, 256, dtype=np.float32))
    decay = np.exp(-t * deltas).astype(np.float32)
    featsT = np.ascontiguousarray(feats.T)
    decT = np.ascontiguousarray(decay.T.reshape(2, 128, L).transpose(1, 0, 2))
    return featsT, np.ascontiguousarray(featsT[:, ::-1]), decT, np.ascontiguousarray(decT[:, :, ::-1])


def _pj(v, n=NJ):
    return np.ascontiguousarray(np.asarray(v, np.float32).reshape(n, 128).T)


_CONST_CACHE = {}


def _constants():
    if _CONST_CACHE:
        return _CONST_CACHE
    c = _CONST_CACHE
    c["cos_g"], c["sin_g"] = _rope_tables(64)
    c["cos_d"], c["sin_d"] = _rope_tables(32)
    c["featsT"], c["featsTr"], c["decT"], c["decTr"] = _hyena_tables(SEQ)
    c["featsTc"], c["featsTcr"], c["decTc"], c["decTcr"] = _hyena_tables(NCX)
    bf = ml_dtypes.bfloat16
    c["ident_bf"] = np.eye(128, dtype=np.float32).astype(bf)
    c["anti_bf"] = np.ascontiguousarray(np.eye(128, dtype=np.float32)[::-1]).astype(bf)
    c["ident_f"] = np.eye(128, dtype=np.float32)
    c["onesD_bf"] = np.full((128, 128), 1.0 / D, np.float32).astype(bf)
    bd = np.zeros((128, 128), np.float32)
    bd[:64, :64] = 1.0 / 64
    bd[64:, 64:] = 1.0 / 64
    c["bd64_bf"] = bd.astype(bf)
    c["ones_f"] = np.ones((128, 128), np.float32)
    sel = np.zeros((8, 8, 128), np.float32)
    for e in range(8):
        sel[e, e, :] = 1.0
    c["sel_bf"] = sel.astype(bf)
    m = np.zeros((128, 2), np.float32)
    for p in range(128):
        m[p, (p // 32) % 2] = 1.0
    c["dmask"] = m
    c["ones64_bf"] = np.ones((128, 64), np.float32).astype(bf)
    return c


def _layer_small(inputs, b):
    s = {}
    s["cvec"] = np.ascontiguousarray(np.stack([_pj(inputs["c"][b]), _pj(inputs["c_ctx"])], axis=-1))
    s["b_ada_l"] = np.ascontiguousarray(np.stack([_pj(inputs["b_ada"][i], 48) for i in range(DEPTH)]))
    s["g_mix_l"] = np.ascontiguousarray(np.stack([_pj(inputs["g_mix"][i]) for i in range(DEPTH)]))
    s["g_ffn_l"] = np.ascontiguousarray(np.stack([_pj(inputs["g_ffn"][i]) for i in range(DEPTH)]))
    s["g_final_l"] = _pj(inputs["g_final"])
    cw = np.asarray(inputs["hy_conv_w"], np.float32)
    s["hy_cw_l"] = np.ascontiguousarray(cw.reshape(DEPTH, 3, 6, 128).transpose(0, 3, 2, 1))
    s["hy_cb_l"] = np.ascontiguousarray(np.asarray(inputs["hy_conv_b"], np.float32).reshape(DEPTH, 6, 128).transpose(0, 2, 1))
    s["hy_b1_l"] = np.ascontiguousarray(np.asarray(inputs["hy_b1"], np.float32)[:, :, None])
    s["hy_b2_l"] = np.ascontiguousarray(np.asarray(inputs["hy_b2"], np.float32)[:, :, None])
    s["hy_fr_l"] = np.ascontiguousarray(np.asarray(inputs["hy_freq"], np.float32)[:, :, None])
    s["hy_skip_l"] = np.ascontiguousarray(np.asarray(inputs["hy_skip"], np.float32).reshape(DEPTH, 2, 128).transpose(0, 2, 1))
    s["diff_l"] = np.ascontiguousarray(np.stack([inputs["diff_lq1"], inputs["diff_lk1"], inputs["diff_lq2"],
                                                 inputs["diff_lk2"]], axis=1).astype(np.float32)[:, None])
    sub = np.asarray(inputs["diff_subln"], np.float32)
    s["subln_l"] = np.ascontiguousarray(np.stack([np.tile(sub[i], 2) for i in range(DEPTH)])[:, :, None])
    idx = np.arange(128) % 64
    idx_sw = (idx + 32) % 64
    for nm, key in (("gq_l", "gqa_qnorm"), ("gk_l", "gqa_knorm")):
        g = np.asarray(inputs[key], np.float32)
        s[nm] = np.ascontiguousarray(np.stack([np.stack([g[i][idx], g[i][idx_sw]], axis=-1) for i in range(DEPTH)]))
    return s


BIG_KEYS = ["w_ada", "w_in", "w_out", "hy_w1", "hy_w2", "hy_w3", "ffn_wg", "ffn_wu", "ffn_wd",
            "moe_router", "moe_wg", "moe_wu", "moe_wd"]


def build_program(depth=DEPTH, stop=None, debug=False):
    nc = bass.Bass("TRN2", target_bir_lowering=False)
    consts = _constants()
    np2dt = {np.dtype(np.float32): F32, np.dtype(ml_dtypes.bfloat16): BF16}

    def din(name, shape, dt=F32):
        return nc.dram_tensor(name, list(shape), dt, kind="ExternalInput").ap()

    shapes = {
        "x": [SEQ, D], "ctx": [NCX, D],
        "cvec": [128, 8, 2], "b_ada_l": [DEPTH, 128, 48], "g_mix_l": [DEPTH, 128, 8], "g_ffn_l": [DEPTH, 128, 8],
        "g_final_l": [128, 8], "hy_cw_l": [DEPTH, 128, 6, 3], "hy_cb_l": [DEPTH, 128, 6], "hy_b1_l": [DEPTH, 64, 1],
        "hy_b2_l": [DEPTH, 64, 1], "hy_fr_l": [DEPTH, 64, 1], "hy_skip_l": [DEPTH, 128, 2], "diff_l": [DEPTH, 1, 4, 32],
        "subln_l": [DEPTH, 128, 1], "gq_l": [DEPTH, 128, 2], "gk_l": [DEPTH, 128, 2],
        "w_ada": [DEPTH, D, 6 * D], "w_in": [DEPTH, D, IN_COLS], "w_out": [DEPTH, D, D],
        "hy_w1": [DEPTH, 33, 64], "hy_w2": [DEPTH, 64, 64], "hy_w3": [DEPTH, 64, 512],
        "ffn_wg": [2, D, DFF], "ffn_wu": [2, D, DFF], "ffn_wd": [2, DFF, D],
        "moe_router": [2, D, NEXP], "moe_wg": [2, NEXP, D, DFE], "moe_wu": [2, NEXP, D, DFE], "moe_wd": [2, NEXP, DFE, D],
    }

    class _Inputs(dict):
        def __missing__(self, key):
            if key in shapes:
                ap = din(key, shapes[key])
            else:
                v = consts[key]
                ap = din("k_" + key, v.shape, np2dt[v.dtype])
            self[key] = ap
            return ap

    I = _Inputs()
    out = nc.dram_tensor("out", [SEQ, D], F32, kind="ExternalOutput").ap()

    skind = "ExternalOutput" if debug else "Internal"

    def dscr(name, shape, dt):
        return nc.dram_tensor(name, list(shape), dt, kind=skind).ap()

    S = {}
    S["hT"] = dscr("s_hT", [128, NJ, NT], F32)
    S["zhy"] = dscr("s_zhy", [128, 6, NT], BF16)
    S["qd"] = dscr("s_qd", [128, 2, NT], BF16)
    S["qg"] = dscr("s_qg", [128, 4, NT], BF16)
    S["mg"] = dscr("s_mg", [128, NJ, NT], BF16)
    S["u2"] = dscr("s_u2", [128, NJ, NT], BF16)
    for k_ in range(2):
        S[f"hrev{k_}"] = dscr(f"s_hrev{k_}", [256, 2 * SEQ], BF16)
        S[f"hrevc{k_}"] = dscr(f"s_hrevc{k_}", [256, 2 * NCX], BF16)
    if debug:
        S["dbg_kv"] = dscr("s_dbgkv", [128, 12, NT], F32)

    B = {k: Buf(k) for k in list(S.keys())}
    Bin = Buf("inputs")
    Bout = Buf("out")

    with ExitStack() as st:
        fw = FW(nc, st)

        _uid = [0]

        def sb(name, shape, dt, stack=st):
            _uid[0] += 1
            return stack.enter_context(nc.sbuf_tensor(f"sb{_uid[0]}_{name}", list(shape), dt))

        ps_all = st.enter_context(nc.psum_tensor("ps_all", [128, 8, 512], F32))
        PB = [Buf(f"bank{k}", excl=True) for k in range(8)]

        def bank(k):
            return ps_all[:, k, :]

        def load_const(name, key, dt, q="sp"):
            a = consts[key]
            t = sb(name, a.shape, dt)
            b = Buf(name)
            fw.dma(q, t[:], I[key], reads=[Bin], writes=[b])
            return t, b

        ident_bf, Bident = load_const("ident_bf", "ident_bf", BF16)
        anti_bf, Banti = load_const("anti_bf", "anti_bf", BF16)
        ident_f, Bidentf = load_const("ident_f", "ident_f", F32)
        onesD, BonesD = load_const("onesD", "onesD_bf", BF16)
        bd64, Bbd64 = load_const("bd64", "bd64_bf", BF16)
        ones_f, Bonesf = load_const("ones_f", "ones_f", F32)
        sel_bf, Bsel = load_const("sel_bf", "sel_bf", BF16)
        dmask, Bdmask = load_const("dmask", "dmask", F32)
        ones64, Bones64 = load_const("ones64", "ones64_bf", BF16)
        cvec = sb("cvec", [128, 8, 2], F32)
        Bcvec = Buf("cvec")
        fw.dma("sp", cvec[:], I["cvec"], reads=[Bin], writes=[Bcvec])
        scb = sb("scb", [128, 8, 2], BF16)
        Bscb = Buf("scb")
        fw.op("act", lambda e: e.activation(out=scb[:], in_=cvec[:], func=AF.Silu), reads=[Bcvec], writes=[Bscb])
        gfin = sb("gfin", [128, 8], F32)
        Bgfin = Buf("gfin")
        fw.dma("sp", gfin[:], I["g_final_l"], reads=[Bin], writes=[Bgfin])

        if stop == "consts":
            fw.finish()
            return nc, list(I.keys())
        TBLK = [(0, NCX, 1)] + [(NCX + 512 * i, 512, 0) for i in range(SEQ // 512)]

        with ExitStack() as ph:
            xt = [sb(f"xt{i}", [128, D], F32, ph) for i in range(2)]
            Bxt = [Buf() for _ in range(2)]
            hst = [sb(f"hst{i}", [128, NJ, 512], F32, ph) for i in range(2)]
            Bhst = [Buf() for _ in range(2)]
            k = 0
            import os
            for bi, (t0, n, seg) in enumerate(TBLK[:int(os.environ.get('DEV_NBLK', '99'))]):
                hs, Bh = hst[bi % 2], Bhst[bi % 2]
                for tt in range(n // 128):
                    src = I["ctx"][tt * 128:(tt + 1) * 128, :] if seg else I["x"][t0 - NCX + tt * 128: t0 - NCX + (tt + 1) * 128, :]
                    x_, Bx_ = xt[k % 2], Bxt[k % 2]
                    fw.dma("sp", x_[:], src, reads=[Bin], writes=[Bx_])
                    for half in range(2):
                        pbk = 2 * (k % 2) + half
                        for jj in range(4):
                            j = half * 4 + jj
                            fw.op("pe", lambda e: e.matmul(ps_all[:, pbk, jj * 128:(jj + 1) * 128], lhsT=x_[:, j * 128:(j + 1) * 128], rhs=ident_f[:], start=True, stop=True),
                                  reads=[Bx_, Bidentf], writes=[PB[pbk]], inc=(jj == 3))
                        eng = "dve" if half == 0 else "act"
                        src_ps = ps_all[:, pbk, :].rearrange("p (j t) -> p j t", j=4)
                        dst = hs[:, half * 4:(half + 1) * 4, tt * 128:(tt + 1) * 128]
                        if eng == "dve":
                            fw.op("dve", lambda e: e.tensor_copy(out=dst, in_=src_ps), reads=[PB[pbk]], writes=[Bh])
                        else:
                            fw.op("act", lambda e: e.copy(out=dst, in_=src_ps), reads=[PB[pbk]], writes=[Bh])
                    k += 1
                fw.dma("sp", S["hT"][:, :, t0:t0 + n], hs[:, :, :n], reads=[Bh], writes=[B["hT"]])
            fw.barrier()

        if stop == "init":
            fw.finish()
            return nc, list(I.keys())

        def mm_group(out_ap, pairs, reads, wbuf, skip=False):
            n = len(pairs)
            for k, (l, r) in enumerate(pairs):
                fw.op("pe", lambda e: e.matmul(out_ap, lhsT=l, rhs=r, start=(k == 0), stop=(k == n - 1)),
                      reads=reads, writes=[wbuf], same=False, inc=(k == n - 1))

        def rstd_from_ps(ps_ap, rt_ap, Brt, PBk):
            fw.op("act", lambda e: e.activation(out=rt_ap, in_=ps_ap, func=AF.Sqrt, bias=EPS, scale=1.0), reads=[PBk], writes=[Brt])
            fw.op("dve", lambda e: e.reciprocal(out=rt_ap, in_=rt_ap), reads=[Brt], writes=[Brt])

        modsb = sb("modsb", [128, 48, 2], F32)
        Bmod = Buf("mod")
        prm = sb("prm", [128, 6, 8, 2], F32)
        Bprm = Buf("prm")
        gmix = sb("gmix", [128, 8], F32)
        gffn = sb("gffn", [128, 8], F32)
        bada = sb("bada", [128, 48], F32)
        Bsmall = Buf("small")
        gqk = sb("gqk", [128, 4], F32)
        lamt = sb("lamt", [128, 4], F32)
        Blam = Buf("lam")
        dl = sb("dl", [1, 4, 32], F32)
        dl2 = sb("dl2", [1, 8], F32)

        def layer_setup(i, ph):
            fw.dma("sp", gmix[:], I["g_mix_l"][i], reads=[Bin], writes=[Bsmall])
            fw.dma("sp", gffn[:], I["g_ffn_l"][i], reads=[Bin], writes=[Bsmall])
            fw.dma("sp", bada[:], I["b_ada_l"][i], reads=[Bin], writes=[Bsmall])
            fw.dma("sp", gqk[:, 0:2], I["gq_l"][i], reads=[Bin], writes=[Bsmall])
            fw.dma("sp", gqk[:, 2:4], I["gk_l"][i], reads=[Bin], writes=[Bsmall])
            fw.dma("sp", dl[:], I["diff_l"][i], reads=[Bin], writes=[Bsmall])
            fw.dma("sp", lamt[:, 1:2], I["subln_l"][i], reads=[Bin], writes=[Blam])
            wa = [sb(f"wada{k}", [128, 8, 1024], BF16, ph) for k in range(2)]
            Bwa = [Buf() for _ in range(2)]
            wsrc = I["w_ada"][i].rearrange("(j p) n -> p j n", p=128)
            for sidx in range(6):
                w_, Bw_ = wa[sidx % 2], Bwa[sidx % 2]
                fw.dma("pool", w_[:], wsrc[:, :, sidx * 1024:(sidx + 1) * 1024], reads=[Bin], writes=[Bw_])
                for jj in range(8):
                    col = (sidx * 8 + jj) * 2
                    mm_group(ps_all[:, 0, col:col + 2], [(w_[:, j, jj * 128:(jj + 1) * 128], scb[:, j, :]) for j in range(8)],
                             [Bw_, Bscb], PB[0])
            for n_ in range(2):
                fw.op("dve", lambda e: e.tensor_tensor(out=modsb[:, :, n_], in0=ps_all[:, 0, 0:96].rearrange("p (a b) -> p a b", b=2)[:, :, n_],
                                                       in1=bada[:], op=ALU.add), reads=[PB[0], Bsmall], writes=[Bmod])
            for (dst, g_, sc0, sh0, gt0) in ((0, gmix, 8, 0, 16), (3, gffn, 32, 24, 40)):
                for n_ in range(2):
                    fw.op("dve", lambda e: e.scalar_tensor_tensor(out=prm[:, dst, :, n_], in0=modsb[:, sc0:sc0 + 8, n_], scalar=1.0, in1=g_[:],
                                                                  op0=ALU.add, op1=ALU.mult), reads=[Bmod, Bsmall], writes=[Bprm])
                    fw.op("dve", lambda e: e.tensor_copy(out=prm[:, dst + 1, :, n_], in_=modsb[:, sh0:sh0 + 8, n_]), reads=[Bmod], writes=[Bprm])
                    fw.op("dve", lambda e: e.tensor_copy(out=prm[:, dst + 2, :, n_], in_=modsb[:, gt0:gt0 + 8, n_]), reads=[Bmod], writes=[Bprm])
            lam_init = 0.8 - 0.6 * math.exp(-0.3 * i)
            fw.op("dve", lambda e: e.tensor_tensor(out=dl[:, 0, :], in0=dl[:, 0, :], in1=dl[:, 1, :], op=ALU.mult), reads=[Bsmall], writes=[Bsmall])
            fw.op("dve", lambda e: e.tensor_tensor(out=dl[:, 2, :], in0=dl[:, 2, :], in1=dl[:, 3, :], op=ALU.mult), reads=[Bsmall], writes=[Bsmall])
            fw.op("dve", lambda e: e.reduce_sum(out=dl2[:, 0:1], in_=dl[:, 0, :], axis=AX.X), reads=[Bsmall], writes=[Bsmall])
            fw.op("dve", lambda e: e.reduce_sum(out=dl2[:, 1:2], in_=dl[:, 2, :], axis=AX.X), reads=[Bsmall], writes=[Bsmall])
            fw.op("act", lambda e: e.activation(out=dl2[:, 2:4], in_=dl2[:, 0:2], func=AF.Exp), reads=[Bsmall], writes=[Bsmall])
            fw.op("dve", lambda e: e.tensor_tensor(out=dl2[:, 4:5], in0=dl2[:, 3:4], in1=dl2[:, 2:3], op=ALU.subtract), reads=[Bsmall], writes=[Bsmall])
            fw.op("dve", lambda e: e.tensor_scalar(out=dl2[:, 4:5], in0=dl2[:, 4:5], scalar1=-lam_init, scalar2=None, op0=ALU.add), reads=[Bsmall], writes=[Bsmall])
            fw.op("pe", lambda e: e.matmul(ps_all[:, 1, 0:1], lhsT=ones_f[0:1, :], rhs=dl2[:, 4:5], start=True, stop=True),
                  reads=[Bsmall, Bonesf], writes=[PB[1]])
            fw.op("dve", lambda e: e.tensor_copy(out=lamt[:, 0:1], in_=ps_all[:, 1, 0:1]), reads=[PB[1]], writes=[Blam])
            fw.op("dve", lambda e: e.tensor_scalar(out=lamt[:, 1:2], in0=lamt[:, 1:2], scalar1=(1.0 - lam_init), scalar2=None, op0=ALU.mult), reads=[Blam], writes=[Blam])

        def norm_block(hblk, Bh, n, seg, a_idx, sq, Bsq, rt, Brt, tmp, Btmp, outs):
            fw.op("act", lambda e: e.activation(out=sq[:, :, :n], in_=hblk[:, :, :n], func=AF.Square), reads=[Bh], writes=[Bsq])
            mm_group(ps_all[:, 0, :n], [(onesD[:], sq[:, j, :n]) for j in range(8)], [BonesD, Bsq], PB[0])
            rstd_from_ps(ps_all[:, 0, :n], rt[:, :n], Brt, PB[0])
            for j in range(8):
                fw.op("dve", lambda e: e.tensor_tensor(out=tmp[:, :n], in0=hblk[:, j, :n], in1=rt[:, :n], op=ALU.mult), reads=[Bh, Brt], writes=[Btmp])
                for (ot, Bo_) in outs:
                    fw.op("dve", lambda e: e.tensor_scalar(out=ot[:, j, :n], in0=tmp[:, :n], scalar1=prm[:, a_idx, j, seg:seg + 1],
                                                           scalar2=prm[:, a_idx + 1, j, seg:seg + 1], op0=ALU.mult, op1=ALU.add),
                          reads=[Btmp, Bprm], writes=[Bo_])

        for li in range(depth):
            moe = (li % 2 == 1)
            with ExitStack() as kv:
                kd1 = sb("kd1", [128, 2, NT], BF16, kv)
                kd2 = sb("kd2", [128, 2, NT], BF16, kv)
                kgA = sb("kgA", [128, NT], BF16, kv)
                kgB = sb("kgB", [128, NT], BF16, kv)
                vd = sb("vd", [128, NT // 128, 4, 65], BF16, kv)
                vg = sb("vg", [128, NT // 128, 2, 65], BF16, kv)
                Bkd1, Bkd2, BkgA, BkgB, Bvd, Bvg = (Buf() for _ in range(6))
                fw.op("pool", lambda e: e.memset(vd[:, :, :, 64:65], 1.0), writes=[Bvd])
                fw.op("pool", lambda e: e.memset(vg[:, :, :, 64:65], 1.0), writes=[Bvg])
                with ExitStack() as ph:
                    layer_setup(li, ph)
                    fw.barrier()
                if stop == "setup":
                    if debug:
                        fw.dma("sp", S["dbg_kv"][:, 0, 0:96], modsb[:].rearrange("p a b -> p (a b)"), reads=[Bmod], writes=[Bout])
                        fw.dma("sp", S["dbg_kv"][:, 1, 0:96], prm[:].rearrange("p a b c -> p (a b c)"), reads=[Bprm], writes=[Bout])
                        fw.dma("sp", S["dbg_kv"][:, 2, 0:4], lamt[:], reads=[Blam], writes=[Bout])
                    fw.finish()
                    return nc, list(I.keys())

                with ExitStack() as ph:
                    win = sb("win", [128, 8, IN_COLS], BF16, ph)
                    Bwin = Buf("win")
                    wsrc = I["w_in"][li].rearrange("(j p) n -> p j n", p=128)
                    for c0 in range(0, IN_COLS, 768):
                        fw.dma("pool", win[:, :, c0:c0 + 768], wsrc[:, :, c0:c0 + 768], reads=[Bin], writes=[Bwin])
                    wx = sb("wx", [128, 8, 11 * 128], BF16, ph)
                    Bwx = Buf("wx")

                    def perm_copy(dst_c, src_col, hd, swap_heads=False, swap_halves=True):
                        half = hd // 2
                        ng = 128 // hd
                        for g in range(ng):
                            gs = (ng - 1 - g) if swap_heads else g
                            for h in range(2):
                                hs_ = (1 - h) if swap_halves else h
                                d0 = dst_c * 128 + g * hd + h * half
                                s0 = src_col + gs * hd + hs_ * half
                                fw.op("pool", lambda e: e.tensor_copy(out=wx[:, :, d0:d0 + half], in_=win[:, :, s0:s0 + half]),
                                      reads=[Bwin], writes=[Bwx])
                    for c in range(2):
                        perm_copy(0 + c, OFF_DQ + 128 * c, 32)
                        perm_copy(2 + c, OFF_DK + 128 * c, 32)
                    for c in range(4):
                        perm_copy(4 + c, OFF_GQ + 128 * c, 64)
                    perm_copy(8, OFF_GK, 64)
                    perm_copy(9, OFF_GK, 64, swap_heads=True, swap_halves=False)
                    perm_copy(10, OFF_GK, 64, swap_heads=True, swap_halves=True)

                    hblk = sb("hblk", [128, 8, 512], F32, ph)
                    Bhblk = Buf()
                    sq = sb("sq", [128, 8, 512], BF16, ph)
                    Bsq = Buf()
                    ub = sb("ub", [128, 8, 512], BF16, ph)
                    Bub = Buf()
                    rt = sb("rt", [128, 512], F32, ph)
                    Brt = Buf()
                    tmp = sb("tmp", [128, 512], F32, ph)
                    Btmp = Buf()
                    tabs = sb("tabs", [128, 4, 512], F32, ph)
                    Btabs = Buf()
                    t1 = sb("t1", [128, 512], F32, ph)
                    t2 = sb("t2", [128, 512], F32, ph)
                    t3 = sb("t3", [128, 512], F32, ph)
                    Bt1, Bt2, Bt3 = Buf(), Buf(), Buf()
                    rt2 = sb("rt2", [128, 512], F32, ph)
                    Brt2 = Buf()
                    sq2 = sb("sq2", [128, 512], BF16, ph)
                    Bsq2 = Buf()
                    zst = sb("zst", [128, 6, 512], BF16, ph)
                    Bzst = Buf()
                    qst = sb("qst", [128, 6, 512], BF16, ph)
                    Bqst = Buf()

                    def proj(pbk, wtile_fn, n):
                        mm_group(ps_all[:, pbk, :n], [(wtile_fn(j), ub[:, j, :n]) for j in range(8)], [Bwin, Bwx, Bub], PB[pbk])

                    import os
                    PARTS = os.environ.get('DEV_PARTS', 'nhdgv')
                    for (t0, n, seg) in TBLK[:int(os.environ.get('DEV_NBLK', '99'))]:
                        fw.dma("sp", hblk[:, :, :n], S["hT"][:, :, t0:t0 + n], reads=[B["hT"]], writes=[Bhblk])
                        for ti, key in enumerate(("cos_g", "sin_g", "cos_d", "sin_d")):
                            fw.dma("sp", tabs[:, ti, :n], I[key][:, t0:t0 + n], reads=[Bin], writes=[Btabs])
                        norm_block(hblk, Bhblk, n, seg, 0, sq, Bsq, rt, Brt, tmp, Btmp, [(ub, Bub)])
                        for c in range(6 if 'h' in PARTS else 0):
                            pbk = 2 + (c % 2)
                            proj(pbk, lambda j: win[:, j, c * 128:(c + 1) * 128], n)
                            if c % 2 == 0:
                                fw.op("act", lambda e: e.copy(out=zst[:, c, :n], in_=ps_all[:, pbk, :n]), reads=[PB[pbk]], writes=[Bzst])
                            else:
                                fw.op("dve", lambda e: e.tensor_copy(out=zst[:, c, :n], in_=ps_all[:, pbk, :n]), reads=[PB[pbk]], writes=[Bzst])
                        fw.dma("sp", S["zhy"][:, :, t0:t0 + n], zst[:, :, :n], reads=[Bzst], writes=[B["zhy"]])
                        for c in range(4 if 'd' in PARTS else 0):
                            isk = c >= 2
                            col = (OFF_DK if isk else OFF_DQ) + 128 * (c % 2)
                            xc = (2 if isk else 0) + (c % 2)
                            proj(4, lambda j: win[:, j, col:col + 128], n)
                            proj(5, lambda j: wx[:, j, xc * 128:(xc + 1) * 128], n)
                            fw.op("dve", lambda e: e.tensor_tensor(out=t1[:, :n], in0=ps_all[:, 4, :n], in1=tabs[:, 2, :n], op=ALU.mult), reads=[PB[4], Btabs], writes=[Bt1])
                            fw.op("dve", lambda e: e.tensor_tensor(out=t2[:, :n], in0=ps_all[:, 5, :n], in1=tabs[:, 3, :n], op=ALU.mult), reads=[PB[5], Btabs], writes=[Bt2])
                            if not isk:
                                fw.op("dve", lambda e: e.tensor_tensor(out=qst[:, c, :n], in0=t1[:, :n], in1=t2[:, :n], op=ALU.add), reads=[Bt1, Bt2], writes=[Bqst])
                            else:
                                fw.op("dve", lambda e: e.tensor_tensor(out=t3[:, :n], in0=t1[:, :n], in1=t2[:, :n], op=ALU.add), reads=[Bt1, Bt2], writes=[Bt3])
                                fw.op("dve", lambda e: e.tensor_scalar(out=kd1[:, c - 2, t0:t0 + n], in0=t3[:, :n], scalar1=dmask[:, 0:1], scalar2=None, op0=ALU.mult),
                                      reads=[Bt3, Bdmask], writes=[Bkd1])
                                fw.op("dve", lambda e: e.tensor_scalar(out=kd2[:, c - 2, t0:t0 + n], in0=t3[:, :n], scalar1=dmask[:, 1:2], scalar2=None, op0=ALU.mult),
                                      reads=[Bt3, Bdmask], writes=[Bkd2])
                        for c in range(6 if 'g' in PARTS else 0):
                            if c < 4:
                                wa_ = lambda j: win[:, j, OFF_GQ + c * 128: OFF_GQ + (c + 1) * 128]
                                wb_ = lambda j: wx[:, j, (4 + c) * 128:(5 + c) * 128]
                                g0 = 0
                            elif c == 4:
                                wa_ = lambda j: win[:, j, OFF_GK:OFF_GK + 128]
                                wb_ = lambda j: wx[:, j, 8 * 128:9 * 128]
                                g0 = 2
                            else:
                                wa_ = lambda j: wx[:, j, 9 * 128:10 * 128]
                                wb_ = lambda j: wx[:, j, 10 * 128:11 * 128]
                                g0 = 2
                            proj(4, wa_, n)
                            proj(5, wb_, n)
                            fw.op("act", lambda e: e.activation(out=sq2[:, :n], in_=ps_all[:, 4, :n], func=AF.Square), reads=[PB[4]], writes=[Bsq2])
                            fw.op("pe", lambda e: e.matmul(ps_all[:, 6, :n], lhsT=bd64[:], rhs=sq2[:, :n], start=True, stop=True), reads=[Bbd64, Bsq2], writes=[PB[6]])
                            rstd_from_ps(ps_all[:, 6, :n], rt2[:, :n], Brt2, PB[6])
                            fw.op("act", lambda e: e.activation(out=t1[:, :n], in_=ps_all[:, 4, :n], func=AF.Copy, scale=gqk[:, g0:g0 + 1]), reads=[PB[4], Bsmall], writes=[Bt1])
                            fw.op("act", lambda e: e.activation(out=t2[:, :n], in_=ps_all[:, 5, :n], func=AF.Copy, scale=gqk[:, g0 + 1:g0 + 2]), reads=[PB[5], Bsmall], writes=[Bt2])
                            fw.op("dve", lambda e: e.tensor_tensor(out=t1[:, :n], in0=t1[:, :n], in1=tabs[:, 0, :n], op=ALU.mult), reads=[Bt1, Btabs], writes=[Bt1])
                            fw.op("dve", lambda e: e.tensor_tensor(out=t2[:, :n], in0=t2[:, :n], in1=tabs[:, 1, :n], op=ALU.mult), reads=[Bt2, Btabs], writes=[Bt2])
                            fw.op("dve", lambda e: e.tensor_tensor(out=t3[:, :n], in0=t1[:, :n], in1=t2[:, :n], op=ALU.add), reads=[Bt1, Bt2], writes=[Bt3])
                            if c < 4:
                                dst, Bd = qst[:, 2 + c, :n], Bqst
                            elif c == 4:
                                dst, Bd = kgA[:, t0:t0 + n], BkgA
                            else:
                                dst, Bd = kgB[:, t0:t0 + n], BkgB
                            fw.op("dve", lambda e: e.tensor_tensor(out=dst, in0=t3[:, :n], in1=rt2[:, :n], op=ALU.mult), reads=[Bt3, Brt2], writes=[Bd])
                        fw.dma("sp", S["qd"][:, :, t0:t0 + n], qst[:, 0:2, :n], reads=[Bqst], writes=[B["qd"]])
                        fw.dma("sp", S["qg"][:, :, t0:t0 + n], qst[:, 2:6, :n], reads=[Bqst], writes=[B["qg"]])
                        for tt in range(n // 128 if 'v' in PARTS else 0):
                            kt = t0 // 128 + tt
                            pbk = 2 + (tt % 2)
                            if 'x' not in PARTS:
                                mm_group(ps_all[:, pbk, 0:256], [(ub[:, j, tt * 128:(tt + 1) * 128], win[:, j, OFF_DV:OFF_DV + 256]) for j in range(8)], [Bub, Bwin], PB[pbk])
                            if 'y' not in PARTS:
                                mm_group(ps_all[:, pbk, 256:384], [(ub[:, j, tt * 128:(tt + 1) * 128], win[:, j, OFF_GV:OFF_GV + 128]) for j in range(8)], [Bub, Bwin], PB[pbk])
                            if 'x' not in PARTS and 'z' not in PARTS:
                                fw.op("act", lambda e: e.copy(out=vd[:, kt, :, 0:64], in_=ps_all[:, pbk, 0:256].rearrange("p (h d) -> p h d", h=4)), reads=[PB[pbk]], writes=[Bvd])
                            if 'y' not in PARTS and 'z' not in PARTS:
                                fw.op("dve", lambda e: e.tensor_copy(out=vg[:, kt, :, 0:64], in_=ps_all[:, pbk, 256:384].rearrange("p (h d) -> p h d", h=2)), reads=[PB[pbk]], writes=[Bvg])
                    fw.barrier()
                if stop == "p1":
                    if debug:
                        with ExitStack() as ph:
                            dbg = sb("dbg", [128, 8840], F32, ph)
                            Bdbg = Buf()
                            for k_, (src_, Bs_) in enumerate(((kd1[:, 0, :], Bkd1), (kd1[:, 1, :], Bkd1), (kd2[:, 0, :], Bkd2), (kd2[:, 1, :], Bkd2), (kgA[:], BkgA), (kgB[:], BkgB))):
                                fw.op("dve", lambda e: e.tensor_copy(out=dbg[:, 0:NT], in_=src_), reads=[Bs_], writes=[Bdbg])
                                fw.dma("sp", S["dbg_kv"][:, k_, :], dbg[:, 0:NT], reads=[Bdbg], writes=[Bout])
                            fw.op("dve", lambda e: e.tensor_copy(out=dbg[:, 0:34 * 4 * 65], in_=vd[:].rearrange("p a b c -> p (a b c)")), reads=[Bvd], writes=[Bdbg])
                            fw.dma("sp", S["dbg_kv"][:, 6:9, :].rearrange("p a b -> p (a b)")[:, 0:8840], dbg[:, 0:8840], reads=[Bdbg], writes=[Bout])
                            fw.op("dve", lambda e: e.tensor_copy(out=dbg[:, 0:34 * 2 * 65], in_=vg[:].rearrange("p a b c -> p (a b c)")), reads=[Bvg], writes=[Bdbg])
                            fw.dma("sp", S["dbg_kv"][:, 9:11, :].rearrange("p a b -> p (a b)")[:, 0:4420], dbg[:, 0:4420], reads=[Bdbg], writes=[Bout])
                    fw.finish()
                    return nc, list(I.keys())

                with ExitStack() as ph:
                    qdb = [sb(f"qdb{k}", [128, 2, 512], BF16, ph) for k in range(2)]
                    qgb = [sb(f"qgb{k}", [128, 4, 512], BF16, ph) for k in range(2)]
                    Bqb = [Buf() for _ in range(2)]
                    pT = [sb(f"pT{k}", [128, 2, 512], BF16, ph) for k in range(3)]
                    BpT = [Buf() for _ in range(3)]
                    rec = [sb(f"rec{k}", [128, 512], F32, ph) for k in range(2)]
                    Brec = [Buf() for _ in range(2)]
                    tm = [sb(f"tm{k}", [128, 512], F32, ph) for k in range(2)]
                    Btm = [Buf() for _ in range(2)]
                    od = sb("od", [128, 512], F32, ph)
                    Bod = Buf()
                    sqd = sb("sqd", [128, 512], BF16, ph)
                    Bsqd = Buf()
                    rtd = sb("rtd", [128, 512], F32, ph)
                    Brtd = Buf()
                    of = [sb(f"of{k}", [128, 512], BF16, ph) for k in range(2)]
                    Bof = [Buf() for _ in range(2)]
                    cnt = {"pt": 0, "of": 0, "sp": 0, "pair": 0, "rec": 0}
                    deferred = []

                    def run_deferred(step=None):
                        while deferred and (step is None or deferred[0][0] <= step):
                            deferred.pop(0)[1]()

                    def attn_pair(kA_fn, kB_fn, q_t, ch, vA_fn, vB_fn, tiles, nq, scale):
                        pr = cnt["pair"] % 2
                        cnt["pair"] += 1
                        ob, db = (6, 7) if pr == 0 else (0, 1)
                        slots = []

                        def qk(i):
                            kt = tiles[i]
                            s_ = cnt["sp"] % 2
                            cnt["sp"] += 1
                            slots.append(s_)
                            fw.op("pe", lambda e: e.matmul(ps_all[:, 2 + 2 * s_, :nq], lhsT=kA_fn(kt), rhs=q_t[0:64, ch, :nq], start=True, stop=True),
                                  reads=[Bkd1, Bkd2, BkgA, BkgB] + Bqb, writes=[PB[2 + 2 * s_]], same=False, inc=False)
                            fw.op("pe", lambda e: e.matmul(ps_all[:, 3 + 2 * s_, :nq], lhsT=kB_fn(kt), rhs=q_t[64:128, ch, :nq], start=True, stop=True),
                                  reads=[Bkd1, Bkd2, BkgA, BkgB] + Bqb, writes=[PB[3 + 2 * s_]], same=False, inc=True)
                        qk(0)
                        nk = len(tiles)
                        for i, kt in enumerate(tiles):
                            if i + 1 < nk:
                                qk(i + 1)
                            s_ = slots[i]
                            pt_i = cnt["pt"] % 3
                            cnt["pt"] += 1
                            fw.op("act", lambda e: e.activation(out=pT[pt_i][:, :, :nq], in_=ps_all[:, 2 + 2 * s_:4 + 2 * s_, :nq], func=AF.Exp, scale=scale),
                                  reads=[PB[2 + 2 * s_], PB[3 + 2 * s_]], writes=[BpT[pt_i]])
                            first, last = (i == 0), (i == nk - 1)
                            for (bk_, lA, lB, Bl) in ((ob, vA_fn(kt), vB_fn(kt), [Bvd, Bvg]), (db, ones64[:, :], ones64[:, :], [Bones64])):
                                fw.op("pe", lambda e: e.matmul(ps_all[0:64, bk_, :nq], lhsT=lA, rhs=pT[pt_i][:, 0, :nq], start=first, stop=last,
                                                               skip_group_check=True, tile_position=(0, 0)),
                                      reads=Bl + [BpT[pt_i]], writes=[PB[bk_]], same=False, inc=False)
                                fw.op("pe", lambda e: e.matmul(ps_all[64:128, bk_, :nq], lhsT=lB, rhs=pT[pt_i][:, 1, :nq], start=first, stop=last,
                                                               skip_group_check=True, tile_position=(0, 64)),
                                      reads=Bl + [BpT[pt_i]], writes=[PB[bk_]], same=False, inc=(bk_ == db))
                            run_deferred(i)
                        run_deferred()
                        return ob, db

                    def recip_den(db, nq):
                        r_i = cnt["rec"] % 2
                        cnt["rec"] += 1
                        fw.op("dve", lambda e: e.reciprocal(out=rec[r_i][:, :nq], in_=ps_all[:, db, :nq]), reads=[PB[db]], writes=[Brec[r_i]])
                        return r_i

                    QBLK = [(0, NCX, [0, 1])] + [(NCX + 512 * i, 512, list(range(NT // 128))) for i in range(SEQ // 512)]
                    import os
                    QBLK = QBLK[:int(os.environ.get('DEV_NQB', '99'))]

                    def load_q(qi):
                        q0, nq, _ = QBLK[qi]
                        fw.dma("sp", qdb[qi % 2][:, :, :nq], S["qd"][:, :, q0:q0 + nq], reads=[B["qd"]], writes=[Bqb[qi % 2]])
                        fw.dma("sp", qgb[qi % 2][:, :, :nq], S["qg"][:, :, q0:q0 + nq], reads=[B["qg"]], writes=[Bqb[qi % 2]])
                    load_q(0)
                    for qi, (q0, nq, tiles) in enumerate(QBLK):
                        if qi + 1 < len(QBLK):
                            load_q(qi + 1)
                        qd_, qg_ = qdb[qi % 2], qgb[qi % 2]
                        for ch in range(2):
                            for m_ in range(2):
                                kk = kd1 if m_ == 0 else kd2
                                ob, db = attn_pair(lambda kt: kk[0:64, ch, kt * 128:(kt + 1) * 128], lambda kt: kk[64:128, ch, kt * 128:(kt + 1) * 128],
                                                   qd_, ch, lambda kt: vd[:, kt, 2 * ch, 0:64], lambda kt: vd[:, kt, 2 * ch + 1, 0:64], tiles, nq, 32 ** -0.5)
                                r_i = recip_den(db, nq)
                                if m_ == 1:
                                    fw.op("dve", lambda e: e.tensor_scalar(out=rec[r_i][:, :nq], in0=rec[r_i][:, :nq], scalar1=lamt[:, 0:1], scalar2=None, op0=ALU.mult),
                                          reads=[Brec[r_i], Blam], writes=[Brec[r_i]])
                                fw.op("dve", lambda e: e.tensor_tensor(out=tm[m_][:, :nq], in0=ps_all[:, ob, :nq], in1=rec[r_i][:, :nq], op=ALU.mult),
                                      reads=[PB[ob], Brec[r_i]], writes=[Btm[m_]])
                            fw.op("dve", lambda e: e.tensor_tensor(out=od[:, :nq], in0=tm[0][:, :nq], in1=tm[1][:, :nq], op=ALU.add), reads=[Btm[0], Btm[1]], writes=[Bod])

                            def stB(nq=nq):
                                fw.op("act", lambda e: e.activation(out=sqd[:, :nq], in_=od[:, :nq], func=AF.Square), reads=[Bod], writes=[Bsqd])

                            def stC(nq=nq, db=db):
                                fw.op("pe", lambda e: e.matmul(ps_all[:, db, :nq], lhsT=bd64[:], rhs=sqd[:, :nq], start=True, stop=True), reads=[Bsqd, Bbd64], writes=[PB[db]])

                            def stD(nq=nq, db=db, ch=ch, q0=q0):
                                rstd_from_ps(ps_all[:, db, :nq], rtd[:, :nq], Brtd, PB[db])
                                fw.op("dve", lambda e: e.tensor_tensor(out=od[:, :nq], in0=od[:, :nq], in1=rtd[:, :nq], op=ALU.mult), reads=[Bod, Brtd], writes=[Bod])
                                oi = cnt["of"] % 2
                                cnt["of"] += 1
                                fw.op("dve", lambda e: e.tensor_scalar(out=of[oi][:, :nq], in0=od[:, :nq], scalar1=lamt[:, 1:2], scalar2=None, op0=ALU.mult), reads=[Bod, Blam], writes=[Bof[oi]])
                                fw.dma("sp", S["mg"][:, 2 + ch, q0:q0 + nq], of[oi][:, :nq], reads=[Bof[oi]], writes=[B["mg"]])
                            deferred.extend([[3, stB], [5, stC], [8, stD]])
                        for ch in range(4):
                            g = ch // 2
                            kA = kgA if g == 0 else kgB
                            kB = kgA if g == 1 else kgB
                            ob, db = attn_pair(lambda kt: kA[0:64, kt * 128:(kt + 1) * 128], lambda kt: kB[64:128, kt * 128:(kt + 1) * 128],
                                               qg_, ch, lambda kt: vg[:, kt, g, 0:64], lambda kt: vg[:, kt, g, 0:64], tiles, nq, 64 ** -0.5)
                            r_i = recip_den(db, nq)
                            oi = cnt["of"] % 2
                            cnt["of"] += 1
                            fw.op("dve", lambda e: e.tensor_tensor(out=of[oi][:, :nq], in0=ps_all[:, ob, :nq], in1=rec[r_i][:, :nq], op=ALU.mult),
                                  reads=[PB[ob], Brec[r_i]], writes=[Bof[oi]])
                            fw.dma("sp", S["mg"][:, 4 + ch, q0:q0 + nq], of[oi][:, :nq], reads=[Bof[oi]], writes=[B["mg"]])
                    run_deferred()
                    fw.barrier()
            if stop == "p2":
                fw.finish()
                return nc, list(I.keys())

            with ExitStack() as ph:
                x0c = sb("x0c", [128, 2, NT], BF16, ph)
                v1 = sb("v1", [128, 2, NT], BF16, ph)
                Bx0c, Bv1 = Buf(), Buf()
                hsk = sb("hsk", [128, 2], F32, ph)
                Bhsk = Buf()
                fw.dma("sp", hsk[:], I["hy_skip_l"][li], reads=[Bin], writes=[Bhsk])
                def p3a_gen(lj, p3, bsin, bout):
                    w1 = sb("hw1", [33, 64], F32, p3)
                    w2 = sb("hw2", [64, 64], F32, p3)
                    w3 = sb("hw3", [64, 512], F32, p3)
                    hb = sb("hb", [64, 4], F32, p3)
                    Bhw = Buf()
                    fw.dma("sp", w1[:], I["hy_w1"][lj], reads=[Bin], writes=[Bhw])
                    fw.dma("sp", w2[:], I["hy_w2"][lj], reads=[Bin], writes=[Bhw])
                    fw.dma("sp", w3[:], I["hy_w3"][lj], reads=[Bin], writes=[Bhw])
                    fw.dma("sp", hb[:, 0:1], I["hy_b1_l"][lj], reads=[Bin], writes=[Bhw])
                    fw.dma("sp", hb[:, 1:2], I["hy_b2_l"][lj], reads=[Bin], writes=[Bhw])
                    fw.dma("sp", hb[:, 2:3], I["hy_fr_l"][lj], reads=[Bin], writes=[Bhw])
                    fw.op("dve", lambda e: e.tensor_scalar(out=hb[:, 0:2], in0=hb[:, 0:2], scalar1=hb[:, 2:3], scalar2=None, op0=ALU.mult), reads=[Bhw], writes=[Bhw])
                    ft = sb("ft", [33, 512], F32, p3)
                    dec = sb("dec", [128, 2, 512], F32, p3)
                    Bft, Bdec = Buf(), Buf()
                    pre = sb("pre", [64, 512], F32, p3)
                    mk = sb("mk", [64, 512], F32, p3)
                    act1 = sb("act1", [64, 512], F32, p3)
                    act2 = sb("act2", [64, 512], F32, p3)
                    Bpre, Bmk, Ba1, Ba2 = Buf(), Buf(), Buf(), Buf()
                    hrow = sb("hrow", [128, 2, 512], BF16, p3)
                    Bhrow = Buf()

                    def sin_layer(ps_ap, bcol, out_t, Bout_, PBk, n):
                        fw.op("dve", lambda e: e.tensor_scalar(out=pre[:, :n], in0=ps_ap, scalar1=hb[:, 2:3], scalar2=hb[:, bcol:bcol + 1], op0=ALU.mult, op1=ALU.add),
                              reads=[PBk, Bhw], writes=[Bpre])
                        fw.op("dve", lambda e: e.tensor_scalar(out=mk[:, :n], in0=pre[:, :n], scalar1=PI, scalar2=None, op0=ALU.is_gt), reads=[Bpre], writes=[Bmk])
                        fw.op("dve", lambda e: e.scalar_tensor_tensor(out=pre[:, :n], in0=mk[:, :n], scalar=-2 * PI, in1=pre[:, :n], op0=ALU.mult, op1=ALU.add), reads=[Bmk, Bpre], writes=[Bpre])
                        fw.op("dve", lambda e: e.tensor_scalar(out=mk[:, :n], in0=pre[:, :n], scalar1=-PI, scalar2=None, op0=ALU.is_lt), reads=[Bpre], writes=[Bmk])
                        fw.op("dve", lambda e: e.scalar_tensor_tensor(out=pre[:, :n], in0=mk[:, :n], scalar=2 * PI, in1=pre[:, :n], op0=ALU.mult, op1=ALU.add), reads=[Bmk, Bpre], writes=[Bpre])
                        fw.op("act", lambda e: e.activation(out=out_t[:, :n], in_=pre[:, :n], func=AF.Sin), reads=[Bpre], writes=[Bout_])

                    hs_ = lj % 2
                    for (L, kf, kfr, kd_, kdr, hkey) in ((SEQ, "featsT", "featsTr", "decT", "decTr", f"hrev{hs_}"), (NCX, "featsTc", "featsTcr", "decTc", "decTcr", f"hrevc{hs_}")):
                        hdst = S[hkey].rearrange("(c p) x -> p c x", p=128)
                        for direction in range(2):
                            fkey, dkey = (kfr, kdr) if direction == 0 else (kf, kd_)
                            colb = 0 if direction == 0 else 256
                            for p0 in range(0, L, 512):
                                n = min(512, L - p0)
                                fw.dma("sp", ft[:, :n], I[fkey][:, p0:p0 + n], reads=[Bin], writes=[Bft])
                                fw.dma("sp", dec[:, :, :n], I[dkey][:, :, p0:p0 + n], reads=[Bin], writes=[Bdec])
                                fw.op("pe", lambda e: e.matmul(ps_all[0:64, bsin[0], :n], lhsT=w1[:, :], rhs=ft[:, :n], start=True, stop=True), reads=[Bhw, Bft], writes=[PB[bsin[0]]])
                                sin_layer(ps_all[0:64, bsin[0], :n], 0, act1, Ba1, PB[bsin[0]], n)
                                yield
                                fw.op("pe", lambda e: e.matmul(ps_all[0:64, bsin[1], :n], lhsT=w2[:, :], rhs=act1[:, :n], start=True, stop=True), reads=[Bhw, Ba1], writes=[PB[bsin[1]]])
                                sin_layer(ps_all[0:64, bsin[1], :n], 1, act2, Ba2, PB[bsin[1]], n)
                                yield
                                for cc in range(2):
                                    fw.op("pe", lambda e: e.matmul(ps_all[:, bout[cc], :n], lhsT=w3[:, colb + cc * 128: colb + (cc + 1) * 128], rhs=act2[:, :n], start=True, stop=True),
                                          reads=[Bhw, Ba2], writes=[PB[bout[cc]]])
                                    fw.op("dve", lambda e: e.tensor_tensor(out=hrow[:, cc, :n], in0=ps_all[:, bout[cc], :n], in1=dec[:, cc, :n], op=ALU.mult), reads=[PB[bout[cc]], Bdec], writes=[Bhrow])
                                if direction == 0:
                                    fw.dma("sp", hdst[:, :, p0:p0 + n], hrow[:, :, :n], reads=[Bhrow], writes=[B[hkey]])
                                else:
                                    i0 = 1 if p0 == 0 else 0
                                    fw.dma("sp", hdst[:, :, L - 1 + p0 + i0: L - 1 + p0 + n], hrow[:, :, i0:n], reads=[Bhrow], writes=[B[hkey]])
                                yield

                if li == 0:
                    with ExitStack() as p3:
                        for _ in p3a_gen(0, p3, (0, 1), (2, 3)):
                            pass
                        fw.barrier()
                if stop == "p3a":
                    fw.finish()
                    return nc, list(I.keys())
                with ExitStack() as p3:
                    cw = sb("cw", [128, 6, 3], F32, p3)
                    cb = sb("cb", [128, 6], F32, p3)
                    Bcw = Buf()
                    fw.dma("sp", cw[:], I["hy_cw_l"][li], reads=[Bin], writes=[Bcw])
                    fw.dma("sp", cb[:], I["hy_cb_l"][li], reads=[Bin], writes=[Bcw])
                    zc = [sb(f"zc{k}", [128, NT], BF16, p3) for k in range(2)]
                    Bzc = [Buf() for _ in range(2)]
                    yA = sb("yA", [128, NT], F32, p3)
                    yB = sb("yB", [128, NT], F32, p3)
                    ByA, ByB = Buf(), Buf()
                    zi = 0
                    for cc in range(2):
                        for role, chunk in (("x1", 2 + cc), ("v", 4 + cc), ("x0", cc)):
                            z_, Bz_ = zc[zi % 2], Bzc[zi % 2]
                            zi += 1
                            fw.dma("sp", z_[:], S["zhy"][:, chunk, :], reads=[B["zhy"]], writes=[Bz_])
                            y_, By_ = (yA, ByA) if role == "x1" else (yB, ByB)
                            fw.op("dve", lambda e: e.tensor_scalar(out=y_[:], in0=z_[:], scalar1=cw[:, chunk, 1:2], scalar2=cb[:, chunk:chunk + 1], op0=ALU.mult, op1=ALU.add),
                                  reads=[Bz_, Bcw], writes=[By_])
                            for (s0, e0) in ((0, NCX), (NCX, NT)):
                                fw.op("dve", lambda e: e.scalar_tensor_tensor(out=y_[:, s0 + 1:e0], in0=z_[:, s0:e0 - 1], scalar=cw[:, chunk, 0:1], in1=y_[:, s0 + 1:e0], op0=ALU.mult, op1=ALU.add),
                                      reads=[Bz_, Bcw, By_], writes=[By_])
                                fw.op("dve", lambda e: e.scalar_tensor_tensor(out=y_[:, s0:e0 - 1], in0=z_[:, s0 + 1:e0], scalar=cw[:, chunk, 2:3], in1=y_[:, s0:e0 - 1], op0=ALU.mult, op1=ALU.add),
                                      reads=[Bz_, Bcw, By_], writes=[By_])
                            if role == "v":
                                fw.op("dve", lambda e: e.tensor_tensor(out=v1[:, cc, :], in0=yB[:], in1=yA[:], op=ALU.mult), reads=[ByA, ByB], writes=[Bv1])
                            elif role == "x0":
                                fw.op("act", lambda e: e.copy(out=x0c[:, cc, :], in_=yB[:]), reads=[ByB], writes=[Bx0c])
                    fw.barrier()
                with ExitStack() as p3:
                    NTL = NT // 128
                    Vt = sb("Vt", [128, 256, NTL], BF16, p3)
                    BVt = Buf()
                    Ysb = sb("Ysb", [128, NTL, 256], BF16, p3)
                    BYsb = Buf()
                    G = [sb(f"G{k}", [128, 8064], BF16, p3) for k in range(2)]
                    BG = [Buf() for _ in range(2)]
                    Gc = sb("Gc", [128, 16, 384], BF16, p3)
                    BGc = Buf()
                    hyo = sb("hyo", [128, 2, NT], BF16, p3)
                    Bhyo = Buf()
                    tmpf = sb("tmpf", [128, 512], F32, p3)
                    Btmpf = Buf()
                    gen3a = p3a_gen(li + 1, p3, (0, 1), (6, 7)) if li + 1 < depth else None
                    hkl, hkc = f"hrev{li % 2}", f"hrevc{li % 2}"
                    k4 = 0
                    for cc in range(2):
                        for j0 in range(0, NTL, 4):
                            nj = min(4, NTL - j0)
                            bk = k4 % 2
                            k4 += 1
                            for k in range(nj):
                                fw.op("pe", lambda e: e.matmul(ps_all[:, bk, k * 128:(k + 1) * 128], lhsT=v1[:, cc, (j0 + k) * 128:(j0 + k + 1) * 128], rhs=ident_bf[:], start=True, stop=True),
                                      reads=[Bv1, Bident], writes=[PB[bk]], same=False, inc=(k == nj - 1))
                            dstv = Vt[:, cc * 128:(cc + 1) * 128, j0:j0 + nj].rearrange("p c j -> p j c")
                            srcv = ps_all[:, bk, 0:nj * 128].rearrange("p (j c) -> p j c", j=nj)
                            if bk == 0:
                                fw.op("dve", lambda e: e.tensor_copy(out=dstv, in_=srcv), reads=[PB[bk]], writes=[BVt])
                            else:
                                fw.op("act", lambda e: e.copy(out=dstv, in_=srcv), reads=[PB[bk]], writes=[BVt])
                    import os
                    NCH = int(os.environ.get('DEV_NCH', '256'))
                    for c in range(NCH):
                        g_, Bg_ = G[c % 2], BG[c % 2]
                        src = bass.AP(S[hkl].tensor, c * 2 * SEQ, [[1, 128], [1, 8064]])
                        fw.dma("sp", g_[:], src, reads=[B[hkl]], writes=[Bg_])
                        bk = 2 + (c // 16) % 2
                        col0 = (c % 16) * 32
                        ds = [0] + [d for d in range(-31, 32) if d != 0]
                        for di, d in enumerate(ds):
                            o_d = SEQ - 128 - 128 * d
                            if d >= 0:
                                oc0, oc1, j0_ = d, 32, 0
                            else:
                                oc0, oc1, j0_ = 0, 32 + d, -d
                            nn = oc1 - oc0
                            last = (di == len(ds) - 1) and (c % 16 == 15 or c == NCH - 1)
                            fw.op("pe", lambda e: e.matmul(ps_all[:, bk, col0 + oc0:col0 + oc1], lhsT=g_[:, o_d:o_d + 128], rhs=Vt[:, c, 2 + j0_:2 + j0_ + nn],
                                                           start=(di == 0 and c % 16 == 0), stop=last, skip_group_check=True),
                                  reads=[Bg_, BVt], writes=[PB[bk]], same=False, inc=(di == len(ds) - 1))
                        if c % 16 == 15 or c == NCH - 1:
                            c0 = c - (c % 16)
                            ncg = c - c0 + 1
                            dsty = Ysb[:, 2:NTL, c0:c0 + ncg].rearrange("p i c -> p c i")
                            srcy = ps_all[:, bk, 0:ncg * 32].rearrange("p (c i) -> p c i", c=ncg)
                            if bk == 2:
                                fw.op("dve", lambda e: e.tensor_copy(out=dsty, in_=srcy), reads=[PB[bk]], writes=[BYsb])
                            else:
                                fw.op("act", lambda e: e.copy(out=dsty, in_=srcy), reads=[PB[bk]], writes=[BYsb])
                        if gen3a is not None and c % 4 == 3:
                            next(gen3a, None)
                    for c0 in range(0, min(NCH, 256), 16):
                        src = bass.AP(S[hkc].tensor, c0 * 2 * NCX, [[1, 128], [2 * NCX, 16], [1, 384]])
                        fw.dma("sp", Gc[:], src, reads=[B[hkc]], writes=[BGc])
                        bk = 4 + (c0 // 16) % 2
                        for cl in range(16):
                            c = c0 + cl
                            for di, d in enumerate((0, 1, -1)):
                                o_d = NCX - 128 - 128 * d
                                if d == 0:
                                    oc0, oc1, j0_ = 0, 2, 0
                                elif d == 1:
                                    oc0, oc1, j0_ = 1, 2, 0
                                else:
                                    oc0, oc1, j0_ = 0, 1, 1
                                nn = oc1 - oc0
                                last = (di == 2 and cl == 15)
                                fw.op("pe", lambda e: e.matmul(ps_all[:, bk, cl * 2 + oc0:cl * 2 + oc1], lhsT=Gc[:, cl, o_d:o_d + 128], rhs=Vt[:, c, j0_:j0_ + nn],
                                                               start=(di == 0 and cl == 0), stop=last, skip_group_check=True),
                                      reads=[BGc, BVt], writes=[PB[bk]], same=False, inc=last)
                        dsty = Ysb[:, 0:2, c0:c0 + 16].rearrange("p i c -> p c i")
                        srcy = ps_all[:, bk, 0:32].rearrange("p (c i) -> p c i", c=16)
                        fw.op("dve", lambda e: e.tensor_copy(out=dsty, in_=srcy), reads=[PB[bk]], writes=[BYsb])
                    if gen3a is not None:
                        for _ in gen3a:
                            pass
                    k4 = 0
                    for cc in range(2):
                        for i0 in range(0, NTL, 4):
                            ni = min(4, NTL - i0)
                            bk = 6 + k4 % 2
                            k4 += 1
                            for k in range(ni):
                                fw.op("pe", lambda e: e.matmul(ps_all[:, bk, k * 128:(k + 1) * 128], lhsT=Ysb[:, i0 + k, cc * 128:(cc + 1) * 128], rhs=anti_bf[:], start=True, stop=True),
                                      reads=[BYsb, Banti], writes=[PB[bk]], same=False, inc=(k == ni - 1))
                            tsl = slice(i0 * 128, (i0 + ni) * 128)
                            nn = ni * 128
                            fw.op("dve", lambda e: e.scalar_tensor_tensor(out=tmpf[:, :nn], in0=v1[:, cc, tsl], scalar=hsk[:, cc:cc + 1], in1=ps_all[:, bk, :nn], op0=ALU.mult, op1=ALU.add),
                                  reads=[Bv1, Bhsk, PB[bk]], writes=[Btmpf])
                            fw.op("dve", lambda e: e.tensor_tensor(out=hyo[:, cc, tsl], in0=tmpf[:, :nn], in1=x0c[:, cc, tsl], op=ALU.mult), reads=[Btmpf, Bx0c], writes=[Bhyo])
                    fw.dma("sp", S["mg"][:, 0:2, :], hyo[:], reads=[Bhyo], writes=[B["mg"]])
                    fw.barrier()
            if stop == "p3":
                fw.finish()
                return nc, list(I.keys())

            with ExitStack() as lf:
                gT = sb("gT", [8, NT], BF16, lf)
                BgT = Buf()
                with ExitStack() as ph:
                    wout = sb("wout", [128, 8, D], BF16, ph)
                    Bwout = Buf()
                    fw.dma("pool", wout[:], I["w_out"][li].rearrange("(j p) n -> p j n", p=128), reads=[Bin], writes=[Bwout])
                    mgb2 = [sb(f"mgb{k}", [128, 8, 512], BF16, ph) for k in range(2)]
                    Bmgb2 = [Buf() for _ in range(2)]
                    hblk2 = [sb(f"hblk4{k}", [128, 8, 512], F32, ph) for k in range(2)]
                    Bhblk2 = [Buf() for _ in range(2)]
                    sq = sb("sq4", [128, 8, 512], BF16, ph)
                    Bsq = Buf()
                    rt = sb("rt4", [128, 512], F32, ph)
                    Brt = Buf()
                    tmp = sb("tmp4", [128, 512], F32, ph)
                    Btmp = Buf()
                    u2b = sb("u2b", [128, 8, 512], BF16, ph)
                    Bu2b = Buf()
                    if moe:
                        u2f = sb("u2f", [128, 8, 512], F32, ph)
                        Bu2f = Buf()
                        rtr = sb("rtr", [128, 8, NEXP], F32, ph)
                        Brtr = Buf()
                        fw.dma("sp", rtr[:], I["moe_router"][li // 2].rearrange("(j p) n -> p j n", p=128), reads=[Bin], writes=[Brtr])
                        lg = sb("lg", [128, 8], F32, ph)
                        l2 = sb("l2", [128, 8], F32, ph)
                        eq1 = sb("eq1", [128, 8], F32, ph)
                        eq2 = sb("eq2", [128, 8], F32, ph)
                        gts = sb("gts", [128, 8], F32, ph)
                        mm_ = sb("mm_", [128, 8], F32, ph)
                        Brt_ = Buf()
                    for bi4, (t0, n, seg) in enumerate(TBLK):
                        mgb, Bmgb, hblk, Bhblk = mgb2[bi4 % 2], Bmgb2[bi4 % 2], hblk2[bi4 % 2], Bhblk2[bi4 % 2]
                        fw.dma("sp", mgb[:, :, :n], S["mg"][:, :, t0:t0 + n], reads=[B["mg"]], writes=[Bmgb])
                        fw.dma("sp", hblk[:, :, :n], S["hT"][:, :, t0:t0 + n], reads=[B["hT"]], writes=[Bhblk])
                        for dc in range(8):
                            bk = dc % 2
                            mm_group(ps_all[:, bk, :n], [(wout[:, j, dc * 128:(dc + 1) * 128], mgb[:, j, :n]) for j in range(8)], [Bwout, Bmgb], PB[bk])
                            fw.op("dve", lambda e: e.scalar_tensor_tensor(out=hblk[:, dc, :n], in0=ps_all[:, bk, :n], scalar=prm[:, 2, dc, seg:seg + 1], in1=hblk[:, dc, :n], op0=ALU.mult, op1=ALU.add),
                                  reads=[PB[bk], Bprm, Bhblk], writes=[Bhblk])
                        fw.dma("sp", S["hT"][:, :, t0:t0 + n], hblk[:, :, :n], reads=[Bhblk], writes=[B["hT"]])
                        outs = [(u2b, Bu2b)] + ([(u2f, Bu2f)] if moe else [])
                        norm_block(hblk, Bhblk, n, seg, 3, sq, Bsq, rt, Brt, tmp, Btmp, outs)
                        fw.dma("sp", S["u2"][:, :, t0:t0 + n], u2b[:, :, :n], reads=[Bu2b], writes=[B["u2"]])
                        if moe:
                            for tt in range(n // 128):
                                tok0 = t0 + tt * 128
                                mm_group(ps_all[:, 2, 0:8], [(u2f[:, j, tt * 128:(tt + 1) * 128], rtr[:, j, :]) for j in range(8)], [Bu2f, Brtr], PB[2])
                                fw.op("dve", lambda e: e.tensor_copy(out=lg[:], in_=ps_all[:, 2, 0:8]), reads=[PB[2]], writes=[Brt_])
                                fw.op("dve", lambda e: e.reduce_max(out=mm_[:, 0:1], in_=lg[:], axis=AX.X), reads=[Brt_], writes=[Brt_])
                                fw.op("dve", lambda e: e.tensor_scalar(out=eq1[:], in0=lg[:], scalar1=mm_[:, 0:1], scalar2=None, op0=ALU.is_equal), reads=[Brt_], writes=[Brt_])
                                fw.op("dve", lambda e: e.scalar_tensor_tensor(out=l2[:], in0=eq1[:], scalar=-1e30, in1=lg[:], op0=ALU.mult, op1=ALU.add), reads=[Brt_], writes=[Brt_])
                                fw.op("dve", lambda e: e.reduce_max(out=mm_[:, 1:2], in_=l2[:], axis=AX.X), reads=[Brt_], writes=[Brt_])
                                fw.op("dve", lambda e: e.tensor_scalar(out=eq2[:], in0=l2[:], scalar1=mm_[:, 1:2], scalar2=None, op0=ALU.is_equal), reads=[Brt_], writes=[Brt_])
                                fw.op("dve", lambda e: e.tensor_tensor(out=mm_[:, 2:3], in0=mm_[:, 1:2], in1=mm_[:, 0:1], op=ALU.subtract), reads=[Brt_], writes=[Brt_])
                                fw.op("act", lambda e: e.activation(out=mm_[:, 3:4], in_=mm_[:, 2:3], func=AF.Exp), reads=[Brt_], writes=[Brt_])
                                fw.op("dve", lambda e: e.tensor_scalar(out=mm_[:, 4:5], in0=mm_[:, 3:4], scalar1=1.0, scalar2=None, op0=ALU.add), reads=[Brt_], writes=[Brt_])
                                fw.op("dve", lambda e: e.reciprocal(out=mm_[:, 5:6], in_=mm_[:, 4:5]), reads=[Brt_], writes=[Brt_])
                                fw.op("dve", lambda e: e.tensor_tensor(out=mm_[:, 6:7], in0=mm_[:, 3:4], in1=mm_[:, 5:6], op=ALU.mult), reads=[Brt_], writes=[Brt_])
                                fw.op("dve", lambda e: e.tensor_scalar(out=gts[:], in0=eq1[:], scalar1=mm_[:, 5:6], scalar2=None, op0=ALU.mult), reads=[Brt_], writes=[Brt_])
                                fw.op("dve", lambda e: e.scalar_tensor_tensor(out=gts[:], in0=eq2[:], scalar=mm_[:, 6:7], in1=gts[:], op0=ALU.mult, op1=ALU.add), reads=[Brt_], writes=[Brt_])
                                fw.op("pe", lambda e: e.matmul(ps_all[0:8, 3, 0:128], lhsT=gts[:], rhs=ident_f[:], start=True, stop=True), reads=[Brt_, Bidentf], writes=[PB[3]])
                                fw.op("dve", lambda e: e.tensor_copy(out=gT[:, tok0:tok0 + 128], in_=ps_all[0:8, 3, 0:128]), reads=[PB[3]], writes=[BgT])
                    fw.barrier()
                if stop == "p4":
                    if debug and moe:
                        with ExitStack() as ph:
                            dbg = sb("dbg4", [8, NT], F32, ph)
                            Bdbg = Buf()
                            fw.op("dve", lambda e: e.tensor_copy(out=dbg[:], in_=gT[:]), reads=[BgT], writes=[Bdbg])
                            fw.dma("sp", S["dbg_kv"][0:8, 0, :], dbg[:], reads=[Bdbg], writes=[Bout])
                    fw.finish()
                    return nc, list(I.keys())

                with ExitStack() as ph:
                    SBW = NT // 4
                    subs = [(0, 512), (512, 512), (1024, SBW - 1024)]
                    if moe:
                        groups = [(e_, f0, 512) for e_ in range(NEXP) for f0 in range(0, DFE, 512)]
                    else:
                        groups = [(0, f0, min(512, DFF - f0)) for f0 in range(0, DFF, 512)]

                    def wviews(e_):
                        if moe:
                            return (I["moe_wg"][li // 2][e_].rearrange("(j p) f -> p j f", p=128),
                                    I["moe_wu"][li // 2][e_].rearrange("(j p) f -> p j f", p=128),
                                    I["moe_wd"][li // 2][e_].rearrange("(c p) d -> p c d", p=128))
                        return (I["ffn_wg"][li // 2].rearrange("(j p) f -> p j f", p=128),
                                I["ffn_wu"][li // 2].rearrange("(j p) f -> p j f", p=128),
                                I["ffn_wd"][li // 2].rearrange("(c p) d -> p c d", p=128))
                    wgb = [sb(f"wgb{k}", [128, 8, 512], BF16, ph) for k in range(2)]
                    wub = [sb(f"wub{k}", [128, 8, 512], BF16, ph) for k in range(2)]
                    wdb = [sb(f"wdb{k}", [128, 4, D], BF16, ph) for k in range(2)]
                    Bw = [Buf() for _ in range(2)]
                    u2k = sb("u2k", [128, 8, SBW], BF16, ph)
                    Bu2k = Buf()
                    acc = sb("acc", [128, 8, SBW], F32, ph)
                    Bacc = Buf()
                    Hb = [sb(f"Hb{k}", [128, 4, SBW], BF16, ph) for k in range(2)]
                    BH = [Buf() for _ in range(2)]
                    sgt = [sb(f"sgt{k}", [128, 512], BF16, ph) for k in range(2)]
                    Bsgt = [Buf() for _ in range(2)]
                    gbc = sb("gbc", [128, SBW], BF16, ph)
                    Bgbc = Buf()
                    hres = sb("hres", [128, SBW], F32, ph)
                    Bhres = Buf()
                    import os
                    NSB = int(os.environ.get('DEV_NSB', '4'))
                    NGR = int(os.environ.get('DEV_NGR', '999'))
                    groups = groups[:NGR]

                    Bwd = [Buf() for _ in range(2)]

                    def load_w(gi):
                        e_, f0, nf = groups[gi]
                        wg_, wu_, wd_ = wviews(e_)
                        k = gi % 2
                        fw.dma("pool", wgb[k][:, :, :nf], wg_[:, :, f0:f0 + nf], reads=[Bin], writes=[Bw[k]])
                        fw.dma("pool", wub[k][:, :, :nf], wu_[:, :, f0:f0 + nf], reads=[Bin], writes=[Bw[k]])

                    def load_wd(gi):
                        e_, f0, nf = groups[gi]
                        wg_, wu_, wd_ = wviews(e_)
                        k = gi % 2
                        fw.dma("pool", wdb[k][:, :nf // 128, :], wd_[:, f0 // 128:(f0 + nf) // 128, :], reads=[Bin], writes=[Bwd[k]])

                    for sbi in range(NSB):
                        t0 = sbi * SBW
                        fw.dma("sp", u2k[:], S["u2"][:, :, t0:t0 + SBW], reads=[B["u2"]], writes=[Bu2k])
                        fw.op("pool", lambda e: e.memset(acc[:], 0.0), writes=[Bacc])
                        load_w(0)
                        load_wd(0)
                        cnt5 = {"sg": 0, "ob": 0}

                        def phaseB_units(gi):
                            e_, f0, nf = groups[gi]
                            k = gi % 2
                            nfc = nf // 128
                            units = []
                            for dc in range(8):
                                for (s0, ns) in subs:
                                    units.append((k, nfc, dc, s0, ns))
                            return units

                        def emitB(unit):
                            k, nfc, dc, s0, ns = unit
                            ob = 6 + cnt5["ob"] % 2
                            cnt5["ob"] += 1
                            mm_group(ps_all[:, ob, :ns], [(wdb[k][:, fc2, dc * 128:(dc + 1) * 128], Hb[k][:, fc2, s0:s0 + ns]) for fc2 in range(nfc)], [Bwd[k], BH[k]], PB[ob])
                            fw.op("dve", lambda e: e.tensor_tensor(out=acc[:, dc, s0:s0 + ns], in0=acc[:, dc, s0:s0 + ns], in1=ps_all[:, ob, :ns], op=ALU.add),
                                  reads=[Bacc, PB[ob]], writes=[Bacc])

                        pendingB = []
                        cur_e = -1
                        for gi, (e_, f0, nf) in enumerate(groups):
                            if gi + 1 < len(groups):
                                load_w(gi + 1)
                            k = gi % 2
                            nfc = nf // 128
                            if moe and e_ != cur_e:
                                cur_e = e_
                                for (s0, ns) in subs:
                                    fw.op("pe", lambda e: e.matmul(ps_all[:, 1, :ns], lhsT=sel_bf[:, e_, :], rhs=gT[:, t0 + s0:t0 + s0 + ns], start=True, stop=True),
                                          reads=[Bsel, BgT], writes=[PB[1]])
                                    fw.op("act", lambda e: e.copy(out=gbc[:, s0:s0 + ns], in_=ps_all[:, 1, :ns]), reads=[PB[1]], writes=[Bgbc])
                            per_fc = (len(pendingB) + nfc - 1) // nfc if pendingB else 0
                            for fc in range(nfc):
                                for si, (s0, ns) in enumerate(subs):
                                    gbk, ubk = (2 + si, 4 + si) if si < 2 else (0, 1)
                                    if si == 2 and moe:
                                        gbk, ubk = 0, 0
                                    gcol = 0
                                    ucol = 0 if gbk != ubk else 64
                                    mm_group(ps_all[:, gbk, gcol:gcol + ns], [(wgb[k][:, j, fc * 128:(fc + 1) * 128], u2k[:, j, s0:s0 + ns]) for j in range(8)], [Bw[k], Bu2k], PB[gbk])
                                    mm_group(ps_all[:, ubk, ucol:ucol + ns], [(wub[k][:, j, fc * 128:(fc + 1) * 128], u2k[:, j, s0:s0 + ns]) for j in range(8)], [Bw[k], Bu2k], PB[ubk])
                                    sgi = cnt5["sg"] % 2
                                    cnt5["sg"] += 1
                                    fw.op("act", lambda e: e.activation(out=sgt[sgi][:, :ns], in_=ps_all[:, gbk, gcol:gcol + ns], func=AF.Silu), reads=[PB[gbk]], writes=[Bsgt[sgi]])
                                    if moe:
                                        fw.op("dve", lambda e: e.tensor_tensor(out=sgt[sgi][:, :ns], in0=sgt[sgi][:, :ns], in1=gbc[:, s0:s0 + ns], op=ALU.mult), reads=[Bsgt[sgi], Bgbc], writes=[Bsgt[sgi]])
                                    fw.op("dve", lambda e: e.tensor_tensor(out=Hb[k][:, fc, s0:s0 + ns], in0=sgt[sgi][:, :ns], in1=ps_all[:, ubk, ucol:ucol + ns], op=ALU.mult),
                                          reads=[Bsgt[sgi], PB[ubk]], writes=[BH[k]])
                                for _ in range(per_fc):
                                    if pendingB:
                                        emitB(pendingB.pop(0))
                            while pendingB:
                                emitB(pendingB.pop(0))
                            if gi + 1 < len(groups):
                                load_wd(gi + 1)
                            pendingB = phaseB_units(gi)
                        while pendingB:
                            emitB(pendingB.pop(0))
                        for dc in range(8):
                            fw.dma("sp", hres[:], S["hT"][:, dc, t0:t0 + SBW], reads=[B["hT"]], writes=[Bhres])
                            rngs = []
                            if t0 < NCX:
                                rngs.append((0, NCX - t0, 1))
                                rngs.append((NCX - t0, SBW, 0))
                            else:
                                rngs.append((0, SBW, 0))
                            for (c0, c1, seg) in rngs:
                                fw.op("dve", lambda e: e.scalar_tensor_tensor(out=hres[:, c0:c1], in0=acc[:, dc, c0:c1], scalar=prm[:, 5, dc, seg:seg + 1], in1=hres[:, c0:c1], op0=ALU.mult, op1=ALU.add),
                                      reads=[Bacc, Bprm, Bhres], writes=[Bhres])
                            fw.dma("sp", S["hT"][:, dc, t0:t0 + SBW], hres[:], reads=[Bhres], writes=[B["hT"]])
                    fw.barrier()
            if stop == f"l{li}":
                fw.finish()
                return nc, list(I.keys())

        with ExitStack() as ph:
            hblk = sb("hblkF", [128, 8, 512], F32, ph)
            Bhblk = Buf()
            sq = sb("sqF", [128, 8, 512], BF16, ph)
            Bsq = Buf()
            rt = sb("rtF", [128, 512], F32, ph)
            Brt = Buf()
            hn = sb("hnF", [128, 8, 512], F32, ph)
            Bhn = Buf()
            ot = [sb(f"otF{k}", [128, D], F32, ph) for k in range(2)]
            Bot = [Buf() for _ in range(2)]
            kk = 0
            for (t0, n, seg) in TBLK[1:]:
                fw.dma("sp", hblk[:, :, :n], S["hT"][:, :, t0:t0 + n], reads=[B["hT"]], writes=[Bhblk])
                fw.op("act", lambda e: e.activation(out=sq[:, :, :n], in_=hblk[:, :, :n], func=AF.Square), reads=[Bhblk], writes=[Bsq])
                mm_group(ps_all[:, 0, :n], [(onesD[:], sq[:, j, :n]) for j in range(8)], [BonesD, Bsq], PB[0])
                rstd_from_ps(ps_all[:, 0, :n], rt[:, :n], Brt, PB[0])
                for j in range(8):
                    fw.op("dve", lambda e: e.scalar_tensor_tensor(out=hn[:, j, :n], in0=hblk[:, j, :n], scalar=gfin[:, j:j + 1], in1=rt[:, :n], op0=ALU.mult, op1=ALU.mult),
                          reads=[Bhblk, Bgfin, Brt], writes=[Bhn])
                for tt in range(n // 128):
                    o_, Bo_ = ot[kk % 2], Bot[kk % 2]
                    for half in range(2):
                        bk = 2 + 2 * (kk % 2) + half
                        for jj in range(4):
                            j = half * 4 + jj
                            fw.op("pe", lambda e: e.matmul(ps_all[:, bk, jj * 128:(jj + 1) * 128], lhsT=hn[:, j, tt * 128:(tt + 1) * 128], rhs=ident_f[:], start=True, stop=True),
                                  reads=[Bhn, Bidentf], writes=[PB[bk]], same=False, inc=(jj == 3))
                        if half == 0:
                            fw.op("dve", lambda e: e.tensor_copy(out=o_[:, 0:512], in_=ps_all[:, bk, :]), reads=[PB[bk]], writes=[Bo_])
                        else:
                            fw.op("act", lambda e: e.copy(out=o_[:, 512:1024], in_=ps_all[:, bk, :]), reads=[PB[bk]], writes=[Bo_])
                    r0 = t0 - NCX + tt * 128
                    fw.dma("sp", out[r0:r0 + 128, :], o_[:], reads=[Bo_], writes=[Bout])
                    kk += 1
        fw.finish()
        return nc, list(I.keys())

    return nc, list(I.keys())


def _in_map(inputs, b, consts, names, small=None):
    small = small if small is not None else _layer_small(inputs, b)
    m = {}
    for k in names:
        if k == "x":
            m[k] = np.ascontiguousarray(inputs["x"][b], dtype=np.float32)
        elif k == "ctx":
            m[k] = np.ascontiguousarray(inputs["ctx"][b], dtype=np.float32)
        elif k in small:
            m[k] = small[k]
        elif k in consts:
            m["k_" + k] = consts[k]
        else:
            m[k] = np.ascontiguousarray(inputs[k], dtype=np.float32)
    return m


def kernel(**inputs):
    consts = _constants()
    nc, names = build_program()
    big = {k: np.ascontiguousarray(inputs[k], dtype=np.float32) for k in names if k in BIG_KEYS}
    in_maps = []
    for b in range(8):
        m = _in_map(inputs, b, consts, [k for k in names if k not in BIG_KEYS])
        m.update(big)
        in_maps.append(m)
    res = run_bass_kernel_spmd(nc, in_maps, core_ids=list(range(8)))
    return np.stack([np.asarray(r["out"], np.float32) for r in res.results], axis=0)
```

```python
import math
from contextlib import ExitStack

import numpy as np
import ml_dtypes
import concourse.bass as bass
import concourse.mybir as mybir
from concourse.bass_utils import run_bass_kernel_spmd

F32 = mybir.dt.float32
BF16 = mybir.dt.bfloat16
AF = mybir.ActivationFunctionType
ALU = mybir.AluOpType
AX = mybir.AxisListType

D = 1024
NJ = 8
SEQ = 4096
NCX = 256
NT = SEQ + NCX
DEPTH = 4
GRID_W = 64
EPS = 1e-6
DFF = 2816
DFE = 3584
NEXP = 8
OFF_DQ, OFF_DK, OFF_DV, OFF_GQ, OFF_GK, OFF_GV, IN_COLS = 768, 1024, 1280, 1536, 2048, 2176, 2304
PI = float(np.pi)


class Buf:
    __slots__ = ("name", "w", "r", "excl")

    def __init__(self, name="", excl=False):
        self.name = name
        self.w = None
        self.r = {}
        self.excl = excl


class _Eng:
    def __init__(self, name, eng, sem):
        self.name = name
        self.eng = eng
        self.sem = sem
        self.tick = 0
        self.pending = False
        self.seen = {}


class FW:
    NDMA = 32

    def __init__(self, nc, stack):
        self.nc = nc
        self.engs = {}
        for nm, e in (("pe", nc.tensor), ("dve", nc.vector), ("act", nc.scalar),
                      ("pool", nc.gpsimd), ("sp", nc.sync)):
            sem = stack.enter_context(nc.semaphore("s_" + nm))
            self.engs[nm] = _Eng(nm, e, sem)
        self.dma_sems = [stack.enter_context(nc.semaphore(f"s_dma{i}")) for i in range(self.NDMA)]
        self.dma_cnt = [0] * self.NDMA
        self.dma_next = 0
        self.n_inst = 0

    def _wait(self, E, prod, tick):
        if E.seen.get(prod, 0) >= tick:
            return
        if prod == E.name and tick > E.tick:
            return
        E.seen[prod] = tick
        if prod.startswith("dma"):
            sem = self.dma_sems[int(prod[3:])]
        else:
            sem = self.engs[prod].sem
        E.eng.wait_ge(sem, tick)

    def _deps(self, E, reads, writes, same):
        for b in reads:
            if b.w is not None and (same or b.w[0] != E.name):
                self._wait(E, *b.w)
            if b.excl:
                for p, t in b.r.items():
                    if p != E.name:
                        self._wait(E, p, t)
        for b in writes:
            if b.w is not None and (same or b.w[0] != E.name):
                self._wait(E, *b.w)
            for p, t in b.r.items():
                if p != E.name:
                    self._wait(E, p, t)

    @staticmethod
    def _mark(prod, tick, reads, writes):
        for b in reads:
            if b.r.get(prod, 0) < tick:
                b.r[prod] = tick
        for b in writes:
            b.w = (prod, tick)
            b.r = {}

    def op(self, engname, fn, reads=(), writes=(), same=True, inc=True):
        E = self.engs[engname]
        self._deps(E, reads, writes, same)
        ins = fn(E.eng)
        if inc:
            E.tick += 1
            E.pending = False
            ins.then_inc(E.sem, 1)
            self._mark(E.name, E.tick, reads, writes)
        else:
            E.pending = True
            self._mark(E.name, E.tick + 1, reads, writes)
        self.n_inst += 1
        return ins

    def dma(self, qname, out, in_, reads=(), writes=(), **kw):
        E = self.engs[qname]
        slot = self.dma_next
        self.dma_next = (self.dma_next + 1) % self.NDMA
        pname = f"dma{slot}"
        if self.dma_cnt[slot] > 0:
            self._wait(E, pname, 16 * self.dma_cnt[slot])
        self._deps(E, reads, writes, True)
        ins = E.eng.dma_start(out=out, in_=in_, **kw)
        self.dma_cnt[slot] += 1
        ins.then_inc(self.dma_sems[slot], 16)
        self._mark(pname, 16 * self.dma_cnt[slot], reads, writes)
        self.n_inst += 1
        return ins

    def _flush_pending(self):
        for nm, E in self.engs.items():
            if E.pending:
                E.tick += 1
                E.pending = False
                E.eng.nop().then_inc(E.sem, 1)

    def barrier(self):
        self._flush_pending()
        for nm, E in self.engs.items():
            for nm2, E2 in self.engs.items():
                if nm2 != nm and E2.tick:
                    self._wait(E, nm2, E2.tick)
            for s in range(self.NDMA):
                if self.dma_cnt[s]:
                    self._wait(E, f"dma{s}", 16 * self.dma_cnt[s])

    def finish(self):
        self._flush_pending()
        E = self.engs["sp"]
        for s in range(self.NDMA):
            if self.dma_cnt[s]:
                self._wait(E, f"dma{s}", 16 * self.dma_cnt[s])
        for nm, e in self.engs.items():
            if nm != "sp" and e.tick:
                self._wait(E, nm, e.tick)


def _rope_tables(head_dim):
    t = np.arange(SEQ)
    row = (t // GRID_W).astype(np.float32)
    col = (t % GRID_W).astype(np.float32)
    n = head_dim // 4
    inv = (10000.0 ** (-np.arange(n, dtype=np.float32) / n)).astype(np.float32)
    ang = np.concatenate([row[:, None] * inv, col[:, None] * inv], axis=-1).astype(np.float32)
    cos = np.cos(ang).astype(np.float32)
    sin = np.sin(ang).astype(np.float32)
    half = head_dim // 2
    C = np.ones((128, NT), np.float32)
    S = np.zeros((128, NT), np.float32)
    for p in range(128):
        dd = p % head_dim
        a = dd % half
        C[p, NCX:] = cos[:, a]
        S[p, NCX:] = -sin[:, a] if dd < half else sin[:, a]
    return C, S


def _hyena_tables(L):
    t = np.linspace(0.0, 1.0, L, dtype=np.float32)[:, None]
    bands = 16
    ang = (np.float32(2.0 * math.pi / L) * np.arange(L, dtype=np.float32)[:, None]
           * np.linspace(1e-4, bands - 1, bands, dtype=np.float32)).astype(np.float32)
    feats = np.concatenate([t, np.cos(ang), -np.sin(ang)], axis=-1).astype(np.float32)
    max_decay = math.log(1e-2) / 0.3
    min_decay = math.log(1e-2) / 1.5
    deltas = np.abs(np.linspace(min_decay, max_decay, 256, dtype=np.float32))
    decay = np.exp(-t * deltas).astype(np.float32)
    featsT = np.ascontiguousarray(feats.T)
    decT = np.ascontiguousarray(decay.T.reshape(2, 128, L).transpose(1, 0, 2))
    return featsT, np.ascontiguousarray(featsT[:, ::-1]), decT, np.ascontiguousarray(decT[:, :, ::-1])


def _pj(v, n=NJ):
    return np.ascontiguousarray(np.asarray(v, np.float32).reshape(n, 128).T)


_CONST_CACHE = {}


def _constants():
    if _CONST_CACHE:
        return _CONST_CACHE
    c = _CONST_CACHE
    c["cos_g"], c["sin_g"] = _rope_tables(64)
    c["cos_d"], c["sin_d"] = _rope_tables(32)
    c["featsT"], c["featsTr"], c["decT"], c["decTr"] = _hyena_tables(SEQ)
    c["featsTc"], c["featsTcr"], c["decTc"], c["decTcr"] = _hyena_tables(NCX)
    bf = ml_dtypes.bfloat16
    c["ident_bf"] = np.eye(128, dtype=np.float32).astype(bf)
    c["anti_bf"] = np.ascontiguousarray(np.eye(128, dtype=np.float32)[::-1]).astype(bf)
    c["ident_f"] = np.eye(128, dtype=np.float32)
    c["onesD_bf"] = np.full((128, 128), 1.0 / D, np.float32).astype(bf)
    bd = np.zeros((128, 128), np.float32)
    bd[:64, :64] = 1.0 / 64
    bd[64:, 64:] = 1.0 / 64
    c["bd64_bf"] = bd.astype(bf)
    c["ones_f"] = np.ones((128, 128), np.float32)
    sel = np.zeros((8, 8, 128), np.float32)
    for e in range(8):
        sel[e, e, :] = 1.0
    c["sel_bf"] = sel.astype(bf)
    m = np.zeros((128, 2), np.float32)
    for p in range(128):
        m[p, (p // 32) % 2] = 1.0
    c["dmask"] = m
    c["ones64_bf"] = np.ones((128, 64), np.float32).astype(bf)
    return c


def _layer_small(inputs, b):
    s = {}
    s["cvec"] = np.ascontiguousarray(np.stack([_pj(inputs["c"][b]), _pj(inputs["c_ctx"])], axis=-1))
    s["b_ada_l"] = np.ascontiguousarray(np.stack([_pj(inputs["b_ada"][i], 48) for i in range(DEPTH)]))
    s["g_mix_l"] = np.ascontiguousarray(np.stack([_pj(inputs["g_mix"][i]) for i in range(DEPTH)]))
    s["g_ffn_l"] = np.ascontiguousarray(np.stack([_pj(inputs["g_ffn"][i]) for i in range(DEPTH)]))
    s["g_final_l"] = _pj(inputs["g_final"])
    cw = np.asarray(inputs["hy_conv_w"], np.float32)
    s["hy_cw_l"] = np.ascontiguousarray(cw.reshape(DEPTH, 3, 6, 128).transpose(0, 3, 2, 1))
    s["hy_cb_l"] = np.ascontiguousarray(np.asarray(inputs["hy_conv_b"], np.float32).reshape(DEPTH, 6, 128).transpose(0, 2, 1))
    s["hy_b1_l"] = np.ascontiguousarray(np.asarray(inputs["hy_b1"], np.float32)[:, :, None])
    s["hy_b2_l"] = np.ascontiguousarray(np.asarray(inputs["hy_b2"], np.float32)[:, :, None])
    s["hy_fr_l"] = np.ascontiguousarray(np.asarray(inputs["hy_freq"], np.float32)[:, :, None])
    s["hy_skip_l"] = np.ascontiguousarray(np.asarray(inputs["hy_skip"], np.float32).reshape(DEPTH, 2, 128).transpose(0, 2, 1))
    s["diff_l"] = np.ascontiguousarray(np.stack([inputs["diff_lq1"], inputs["diff_lk1"], inputs["diff_lq2"],
                                                 inputs["diff_lk2"]], axis=1).astype(np.float32)[:, None])
    sub = np.asarray(inputs["diff_subln"], np.float32)
    s["subln_l"] = np.ascontiguousarray(np.stack([np.tile(sub[i], 2) for i in range(DEPTH)])[:, :, None])
    idx = np.arange(128) % 64
    idx_sw = (idx + 32) % 64
    for nm, key in (("gq_l", "gqa_qnorm"), ("gk_l", "gqa_knorm")):
        g = np.asarray(inputs[key], np.float32)
        s[nm] = np.ascontiguousarray(np.stack([np.stack([g[i][idx], g[i][idx_sw]], axis=-1) for i in range(DEPTH)]))
    return s


BIG_KEYS = ["w_ada", "w_in", "w_out", "hy_w1", "hy_w2", "hy_w3", "ffn_wg", "ffn_wu", "ffn_wd",
            "moe_router", "moe_wg", "moe_wu", "moe_wd"]


def build_program(depth=DEPTH, stop=None, debug=False):
    nc = bass.Bass("TRN2", target_bir_lowering=False)
    consts = _constants()
    np2dt = {np.dtype(np.float32): F32, np.dtype(ml_dtypes.bfloat16): BF16}

    def din(name, shape, dt=F32):
        return nc.dram_tensor(name, list(shape), dt, kind="ExternalInput").ap()

    shapes = {
        "x": [SEQ, D], "ctx": [NCX, D],
        "cvec": [128, 8, 2], "b_ada_l": [DEPTH, 128, 48], "g_mix_l": [DEPTH, 128, 8], "g_ffn_l": [DEPTH, 128, 8],
        "g_final_l": [128, 8], "hy_cw_l": [DEPTH, 128, 6, 3], "hy_cb_l": [DEPTH, 128, 6], "hy_b1_l": [DEPTH, 64, 1],
        "hy_b2_l": [DEPTH, 64, 1], "hy_fr_l": [DEPTH, 64, 1], "hy_skip_l": [DEPTH, 128, 2], "diff_l": [DEPTH, 1, 4, 32],
        "subln_l": [DEPTH, 128, 1], "gq_l": [DEPTH, 128, 2], "gk_l": [DEPTH, 128, 2],
        "w_ada": [DEPTH, D, 6 * D], "w_in": [DEPTH, D, IN_COLS], "w_out": [DEPTH, D, D],
        "hy_w1": [DEPTH, 33, 64], "hy_w2": [DEPTH, 64, 64], "hy_w3": [DEPTH, 64, 512],
        "ffn_wg": [2, D, DFF], "ffn_wu": [2, D, DFF], "ffn_wd": [2, DFF, D],
        "moe_router": [2, D, NEXP], "moe_wg": [2, NEXP, D, DFE], "moe_wu": [2, NEXP, D, DFE], "moe_wd": [2, NEXP, DFE, D],
    }

    class _Inputs(dict):
        def __missing__(self, key):
            if key in shapes:
                ap = din(key, shapes[key])
            else:
                v = consts[key]
                ap = din("k_" + key, v.shape, np2dt[v.dtype])
            self[key] = ap
            return ap

    I = _Inputs()
    out = nc.dram_tensor("out", [SEQ, D], F32, kind="ExternalOutput").ap()

    skind = "ExternalOutput" if debug else "Internal"

    def dscr(name, shape, dt):
        return nc.dram_tensor(name, list(shape), dt, kind=skind).ap()

    S = {}
    S["hT"] = dscr("s_hT", [128, NJ, NT], F32)
    S["zhy"] = dscr("s_zhy", [128, 6, NT], BF16)
    S["qd"] = dscr("s_qd", [128, 2, NT], BF16)
    S["qg"] = dscr("s_qg", [128, 4, NT], BF16)
    S["mg"] = dscr("s_mg", [128, NJ, NT], BF16)
    S["u2"] = dscr("s_u2", [128, NJ, NT], BF16)
    for k_ in range(2):
        S[f"hrev{k_}"] = dscr(f"s_hrev{k_}", [256, 2 * SEQ], BF16)
        S[f"hrevc{k_}"] = dscr(f"s_hrevc{k_}", [256, 2 * NCX], BF16)
    if debug:
        S["dbg_kv"] = dscr("s_dbgkv", [128, 12, NT], F32)

    B = {k: Buf(k) for k in list(S.keys())}
    Bin = Buf("inputs")
    Bout = Buf("out")

    with ExitStack() as st:
        fw = FW(nc, st)

        _uid = [0]

        def sb(name, shape, dt, stack=st):
            _uid[0] += 1
            return stack.enter_context(nc.sbuf_tensor(f"sb{_uid[0]}_{name}", list(shape), dt))

        ps_all = st.enter_context(nc.psum_tensor("ps_all", [128, 8, 512], F32))
        PB = [Buf(f"bank{k}", excl=True) for k in range(8)]

        def bank(k):
            return ps_all[:, k, :]

        def load_const(name, key, dt, q="sp"):
            a = consts[key]
            t = sb(name, a.shape, dt)
            b = Buf(name)
            fw.dma(q, t[:], I[key], reads=[Bin], writes=[b])
            return t, b

        ident_bf, Bident = load_const("ident_bf", "ident_bf", BF16)
        anti_bf, Banti = load_const("anti_bf", "anti_bf", BF16)
        ident_f, Bidentf = load_const("ident_f", "ident_f", F32)
        onesD, BonesD = load_const("onesD", "onesD_bf", BF16)
        bd64, Bbd64 = load_const("bd64", "bd64_bf", BF16)
        ones_f, Bonesf = load_const("ones_f", "ones_f", F32)
        sel_bf, Bsel = load_const("sel_bf", "sel_bf", BF16)
        dmask, Bdmask = load_const("dmask", "dmask", F32)
        ones64, Bones64 = load_const("ones64", "ones64_bf", BF16)
        cvec = sb("cvec", [128, 8, 2], F32)
        Bcvec = Buf("cvec")
        fw.dma("sp", cvec[:], I["cvec"], reads=[Bin], writes=[Bcvec])
        scb = sb("scb", [128, 8, 2], BF16)
        Bscb = Buf("scb")
        fw.op("act", lambda e: e.activation(out=scb[:], in_=cvec[:], func=AF.Silu), reads=[Bcvec], writes=[Bscb])
        gfin = sb("gfin", [128, 8], F32)
        Bgfin = Buf("gfin")
        fw.dma("sp", gfin[:], I["g_final_l"], reads=[Bin], writes=[Bgfin])

        if stop == "consts":
            fw.finish()
            return nc, list(I.keys())
        TBLK = [(0, NCX, 1)] + [(NCX + 512 * i, 512, 0) for i in range(SEQ // 512)]

        with ExitStack() as ph:
            xt = [sb(f"xt{i}", [128, D], F32, ph) for i in range(2)]
            Bxt = [Buf() for _ in range(2)]
            hst = [sb(f"hst{i}", [128, NJ, 512], F32, ph) for i in range(2)]
            Bhst = [Buf() for _ in range(2)]
            k = 0
            import os
            for bi, (t0, n, seg) in enumerate(TBLK[:int(os.environ.get('DEV_NBLK', '99'))]):
                hs, Bh = hst[bi % 2], Bhst[bi % 2]
                for tt in range(n // 128):
                    src = I["ctx"][tt * 128:(tt + 1) * 128, :] if seg else I["x"][t0 - NCX + tt * 128: t0 - NCX + (tt + 1) * 128, :]
                    x_, Bx_ = xt[k % 2], Bxt[k % 2]
                    fw.dma("sp", x_[:], src, reads=[Bin], writes=[Bx_])
                    for half in range(2):
                        pbk = 2 * (k % 2) + half
                        for jj in range(4):
                            j = half * 4 + jj
                            fw.op("pe", lambda e: e.matmul(ps_all[:, pbk, jj * 128:(jj + 1) * 128], lhsT=x_[:, j * 128:(j + 1) * 128], rhs=ident_f[:], start=True, stop=True),
                                  reads=[Bx_, Bidentf], writes=[PB[pbk]], inc=(jj == 3))
                        eng = "dve" if half == 0 else "act"
                        src_ps = ps_all[:, pbk, :].rearrange("p (j t) -> p j t", j=4)
                        dst = hs[:, half * 4:(half + 1) * 4, tt * 128:(tt + 1) * 128]
                        if eng == "dve":
                            fw.op("dve", lambda e: e.tensor_copy(out=dst, in_=src_ps), reads=[PB[pbk]], writes=[Bh])
                        else:
                            fw.op("act", lambda e: e.copy(out=dst, in_=src_ps), reads=[PB[pbk]], writes=[Bh])
                    k += 1
                fw.dma("sp", S["hT"][:, :, t0:t0 + n], hs[:, :, :n], reads=[Bh], writes=[B["hT"]])
            fw.barrier()

        if stop == "init":
            fw.finish()
            return nc, list(I.keys())

        def mm_group(out_ap, pairs, reads, wbuf, skip=False):
            n = len(pairs)
            for k, (l, r) in enumerate(pairs):
                fw.op("pe", lambda e: e.matmul(out_ap, lhsT=l, rhs=r, start=(k == 0), stop=(k == n - 1)),
                      reads=reads, writes=[wbuf], same=False, inc=(k == n - 1))

        def rstd_from_ps(ps_ap, rt_ap, Brt, PBk):
            fw.op("act", lambda e: e.activation(out=rt_ap, in_=ps_ap, func=AF.Sqrt, bias=EPS, scale=1.0), reads=[PBk], writes=[Brt])
            fw.op("dve", lambda e: e.reciprocal(out=rt_ap, in_=rt_ap), reads=[Brt], writes=[Brt])

        modsb = sb("modsb", [128, 48, 2], F32)
        Bmod = Buf("mod")
        prm = sb("prm", [128, 6, 8, 2], F32)
        Bprm = Buf("prm")
        gmix = sb("gmix", [128, 8], F32)
        gffn = sb("gffn", [128, 8], F32)
        bada = sb("bada", [128, 48], F32)
        Bsmall = Buf("small")
        gqk = sb("gqk", [128, 4], F32)
        lamt = sb("lamt", [128, 4], F32)
        Blam = Buf("lam")
        dl = sb("dl", [1, 4, 32], F32)
        dl2 = sb("dl2", [1, 8], F32)

        def layer_setup(i, ph):
            fw.dma("sp", gmix[:], I["g_mix_l"][i], reads=[Bin], writes=[Bsmall])
            fw.dma("sp", gffn[:], I["g_ffn_l"][i], reads=[Bin], writes=[Bsmall])
            fw.dma("sp", bada[:], I["b_ada_l"][i], reads=[Bin], writes=[Bsmall])
            fw.dma("sp", gqk[:, 0:2], I["gq_l"][i], reads=[Bin], writes=[Bsmall])
            fw.dma("sp", gqk[:, 2:4], I["gk_l"][i], reads=[Bin], writes=[Bsmall])
            fw.dma("sp", dl[:], I["diff_l"][i], reads=[Bin], writes=[Bsmall])
            fw.dma("sp", lamt[:, 1:2], I["subln_l"][i], reads=[Bin], writes=[Blam])
            wa = [sb(f"wada{k}", [128, 8, 1024], BF16, ph) for k in range(2)]
            Bwa = [Buf() for _ in range(2)]
            wsrc = I["w_ada"][i].rearrange("(j p) n -> p j n", p=128)
            for sidx in range(6):
                w_, Bw_ = wa[sidx % 2], Bwa[sidx % 2]
                fw.dma("pool", w_[:], wsrc[:, :, sidx * 1024:(sidx + 1) * 1024], reads=[Bin], writes=[Bw_])
                for jj in range(8):
                    col = (sidx * 8 + jj) * 2
                    mm_group(ps_all[:, 0, col:col + 2], [(w_[:, j, jj * 128:(jj + 1) * 128], scb[:, j, :]) for j in range(8)],
                             [Bw_, Bscb], PB[0])
            for n_ in range(2):
                fw.op("dve", lambda e: e.tensor_tensor(out=modsb[:, :, n_], in0=ps_all[:, 0, 0:96].rearrange("p (a b) -> p a b", b=2)[:, :, n_],
                                                       in1=bada[:], op=ALU.add), reads=[PB[0], Bsmall], writes=[Bmod])
            for (dst, g_, sc0, sh0, gt0) in ((0, gmix, 8, 0, 16), (3, gffn, 32, 24, 40)):
                for n_ in range(2):
                    fw.op("dve", lambda e: e.scalar_tensor_tensor(out=prm[:, dst, :, n_], in0=modsb[:, sc0:sc0 + 8, n_], scalar=1.0, in1=g_[:],
                                                                  op0=ALU.add, op1=ALU.mult), reads=[Bmod, Bsmall], writes=[Bprm])
                    fw.op("dve", lambda e: e.tensor_copy(out=prm[:, dst + 1, :, n_], in_=modsb[:, sh0:sh0 + 8, n_]), reads=[Bmod], writes=[Bprm])
                    fw.op("dve", lambda e: e.tensor_copy(out=prm[:, dst + 2, :, n_], in_=modsb[:, gt0:gt0 + 8, n_]), reads=[Bmod], writes=[Bprm])
            lam_init = 0.8 - 0.6 * math.exp(-0.3 * i)
            fw.op("dve", lambda e: e.tensor_tensor(out=dl[:, 0, :], in0=dl[:, 0, :], in1=dl[:, 1, :], op=ALU.mult), reads=[Bsmall], writes=[Bsmall])
            fw.op("dve", lambda e: e.tensor_tensor(out=dl[:, 2, :], in0=dl[:, 2, :], in1=dl[:, 3, :], op=ALU.mult), reads=[Bsmall], writes=[Bsmall])
            fw.op("dve", lambda e: e.reduce_sum(out=dl2[:, 0:1], in_=dl[:, 0, :], axis=AX.X), reads=[Bsmall], writes=[Bsmall])
            fw.op("dve", lambda e: e.reduce_sum(out=dl2[:, 1:2], in_=dl[:, 2, :], axis=AX.X), reads=[Bsmall], writes=[Bsmall])
            fw.op("act", lambda e: e.activation(out=dl2[:, 2:4], in_=dl2[:, 0:2], func=AF.Exp), reads=[Bsmall], writes=[Bsmall])
            fw.op("dve", lambda e: e.tensor_tensor(out=dl2[:, 4:5], in0=dl2[:, 3:4], in1=dl2[:, 2:3], op=ALU.subtract), reads=[Bsmall], writes=[Bsmall])
            fw.op("dve", lambda e: e.tensor_scalar(out=dl2[:, 4:5], in0=dl2[:, 4:5], scalar1=-lam_init, scalar2=None, op0=ALU.add), reads=[Bsmall], writes=[Bsmall])
            fw.op("pe", lambda e: e.matmul(ps_all[:, 1, 0:1], lhsT=ones_f[0:1, :], rhs=dl2[:, 4:5], start=True, stop=True),
                  reads=[Bsmall, Bonesf], writes=[PB[1]])
            fw.op("dve", lambda e: e.tensor_copy(out=lamt[:, 0:1], in_=ps_all[:, 1, 0:1]), reads=[PB[1]], writes=[Blam])
            fw.op("dve", lambda e: e.tensor_scalar(out=lamt[:, 1:2], in0=lamt[:, 1:2], scalar1=(1.0 - lam_init), scalar2=None, op0=ALU.mult), reads=[Blam], writes=[Blam])

        def norm_block(hblk, Bh, n, seg, a_idx, sq, Bsq, rt, Brt, tmp, Btmp, outs):
            fw.op("act", lambda e: e.activation(out=sq[:, :, :n], in_=hblk[:, :, :n], func=AF.Square), reads=[Bh], writes=[Bsq])
            mm_group(ps_all[:, 0, :n], [(onesD[:], sq[:, j, :n]) for j in range(8)], [BonesD, Bsq], PB[0])
            rstd_from_ps(ps_all[:, 0, :n], rt[:, :n], Brt, PB[0])
            for j in range(8):
                fw.op("dve", lambda e: e.tensor_tensor(out=tmp[:, :n], in0=hblk[:, j, :n], in1=rt[:, :n], op=ALU.mult), reads=[Bh, Brt], writes=[Btmp])
                for (ot, Bo_) in outs:
                    fw.op("dve", lambda e: e.tensor_scalar(out=ot[:, j, :n], in0=tmp[:, :n], scalar1=prm[:, a_idx, j, seg:seg + 1],
                                                           scalar2=prm[:, a_idx + 1, j, seg:seg + 1], op0=ALU.mult, op1=ALU.add),
                          reads=[Btmp, Bprm], writes=[Bo_])

        for li in range(depth):
            moe = (li % 2 == 1)
            with ExitStack() as kv:
                kd1 = sb("kd1", [128, 2, NT], BF16, kv)
                kd2 = sb("kd2", [128, 2, NT], BF16, kv)
                kgA = sb("kgA", [128, NT], BF16, kv)
                kgB = sb("kgB", [128, NT], BF16, kv)
                vd = sb("vd", [128, NT // 128, 4, 65], BF16, kv)
                vg = sb("vg", [128, NT // 128, 2, 65], BF16, kv)
                Bkd1, Bkd2, BkgA, BkgB, Bvd, Bvg = (Buf() for _ in range(6))
                fw.op("pool", lambda e: e.memset(vd[:, :, :, 64:65], 1.0), writes=[Bvd])
                fw.op("pool", lambda e: e.memset(vg[:, :, :, 64:65], 1.0), writes=[Bvg])
                with ExitStack() as ph:
                    layer_setup(li, ph)
                    fw.barrier()
                if stop == "setup":
                    if debug:
                        fw.dma("sp", S["dbg_kv"][:, 0, 0:96], modsb[:].rearrange("p a b -> p (a b)"), reads=[Bmod], writes=[Bout])
                        fw.dma("sp", S["dbg_kv"][:, 1, 0:96], prm[:].rearrange("p a b c -> p (a b c)"), reads=[Bprm], writes=[Bout])
                        fw.dma("sp", S["dbg_kv"][:, 2, 0:4], lamt[:], reads=[Blam], writes=[Bout])
                    fw.finish()
                    return nc, list(I.keys())

                with ExitStack() as ph:
                    win = sb("win", [128, 8, IN_COLS], BF16, ph)
                    Bwin = Buf("win")
                    wsrc = I["w_in"][li].rearrange("(j p) n -> p j n", p=128)
                    for c0 in range(0, IN_COLS, 768):
                        fw.dma("pool", win[:, :, c0:c0 + 768], wsrc[:, :, c0:c0 + 768], reads=[Bin], writes=[Bwin])
                    wx = sb("wx", [128, 8, 11 * 128], BF16, ph)
                    Bwx = Buf("wx")

                    def perm_copy(dst_c, src_col, hd, swap_heads=False, swap_halves=True):
                        half = hd // 2
                        ng = 128 // hd
                        for g in range(ng):
                            gs = (ng - 1 - g) if swap_heads else g
                            for h in range(2):
                                hs_ = (1 - h) if swap_halves else h
                                d0 = dst_c * 128 + g * hd + h * half
                                s0 = src_col + gs * hd + hs_ * half
                                fw.op("pool", lambda e: e.tensor_copy(out=wx[:, :, d0:d0 + half], in_=win[:, :, s0:s0 + half]),
                                      reads=[Bwin], writes=[Bwx])
                    for c in range(2):
                        perm_copy(0 + c, OFF_DQ + 128 * c, 32)
                        perm_copy(2 + c, OFF_DK + 128 * c, 32)
                    for c in range(4):
                        perm_copy(4 + c, OFF_GQ + 128 * c, 64)
                    perm_copy(8, OFF_GK, 64)
                    perm_copy(9, OFF_GK, 64, swap_heads=True, swap_halves=False)
                    perm_copy(10, OFF_GK, 64, swap_heads=True, swap_halves=True)

                    hblk = sb("hblk", [128, 8, 512], F32, ph)
                    Bhblk = Buf()
                    sq = sb("sq", [128, 8, 512], BF16, ph)
                    Bsq = Buf()
                    ub = sb("ub", [128, 8, 512], BF16, ph)
                    Bub = Buf()
                    rt = sb("rt", [128, 512], F32, ph)
                    Brt = Buf()
                    tmp = sb("tmp", [128, 512], F32, ph)
                    Btmp = Buf()
                    tabs = sb("tabs", [128, 4, 512], F32, ph)
                    Btabs = Buf()
                    t1 = sb("t1", [128, 512], F32, ph)
                    t2 = sb("t2", [128, 512], F32, ph)
                    t3 = sb("t3", [128, 512], F32, ph)
                    Bt1, Bt2, Bt3 = Buf(), Buf(), Buf()
                    rt2 = sb("rt2", [128, 512], F32, ph)
                    Brt2 = Buf()
                    sq2 = sb("sq2", [128, 512], BF16, ph)
                    Bsq2 = Buf()
                    zst = sb("zst", [128, 6, 512], BF16, ph)
                    Bzst = Buf()
                    qst = sb("qst", [128, 6, 512], BF16, ph)
                    Bqst = Buf()

                    def proj(pbk, wtile_fn, n):
                        mm_group(ps_all[:, pbk, :n], [(wtile_fn(j), ub[:, j, :n]) for j in range(8)], [Bwin, Bwx, Bub], PB[pbk])

                    import os
                    PARTS = os.environ.get('DEV_PARTS', 'nhdgv')
                    for (t0, n, seg) in TBLK[:int(os.environ.get('DEV_NBLK', '99'))]:
                        fw.dma("sp", hblk[:, :, :n], S["hT"][:, :, t0:t0 + n], reads=[B["hT"]], writes=[Bhblk])
                        for ti, key in enumerate(("cos_g", "sin_g", "cos_d", "sin_d")):
                            fw.dma("sp", tabs[:, ti, :n], I[key][:, t0:t0 + n], reads=[Bin], writes=[Btabs])
                        norm_block(hblk, Bhblk, n, seg, 0, sq, Bsq, rt, Brt, tmp, Btmp, [(ub, Bub)])
                        for c in range(6 if 'h' in PARTS else 0):
                            pbk = 2 + (c % 2)
                            proj(pbk, lambda j: win[:, j, c * 128:(c + 1) * 128], n)
                            if c % 2 == 0:
                                fw.op("act", lambda e: e.copy(out=zst[:, c, :n], in_=ps_all[:, pbk, :n]), reads=[PB[pbk]], writes=[Bzst])
                            else:
                                fw.op("dve", lambda e: e.tensor_copy(out=zst[:, c, :n], in_=ps_all[:, pbk, :n]), reads=[PB[pbk]], writes=[Bzst])
                        fw.dma("sp", S["zhy"][:, :, t0:t0 + n], zst[:, :, :n], reads=[Bzst], writes=[B["zhy"]])
                        for c in range(4 if 'd' in PARTS else 0):
                            isk = c >= 2
                            col = (OFF_DK if isk else OFF_DQ) + 128 * (c % 2)
                            xc = (2 if isk else 0) + (c % 2)
                            proj(4, lambda j: win[:, j, col:col + 128], n)
                            proj(5, lambda j: wx[:, j, xc * 128:(xc + 1) * 128], n)
                            fw.op("dve", lambda e: e.tensor_tensor(out=t1[:, :n], in0=ps_all[:, 4, :n], in1=tabs[:, 2, :n], op=ALU.mult), reads=[PB[4], Btabs], writes=[Bt1])
                            fw.op("dve", lambda e: e.tensor_tensor(out=t2[:, :n], in0=ps_all[:, 5, :n], in1=tabs[:, 3, :n], op=ALU.mult), reads=[PB[5], Btabs], writes=[Bt2])
                            if not isk:
                                fw.op("dve", lambda e: e.tensor_tensor(out=qst[:, c, :n], in0=t1[:, :n], in1=t2[:, :n], op=ALU.add), reads=[Bt1, Bt2], writes=[Bqst])
                            else:
                                fw.op("dve", lambda e: e.tensor_tensor(out=t3[:, :n], in0=t1[:, :n], in1=t2[:, :n], op=ALU.add), reads=[Bt1, Bt2], writes=[Bt3])
                                fw.op("dve", lambda e: e.tensor_scalar(out=kd1[:, c - 2, t0:t0 + n], in0=t3[:, :n], scalar1=dmask[:, 0:1], scalar2=None, op0=ALU.mult),
                                      reads=[Bt3, Bdmask], writes=[Bkd1])
                                fw.op("dve", lambda e: e.tensor_scalar(out=kd2[:, c - 2, t0:t0 + n], in0=t3[:, :n], scalar1=dmask[:, 1:2], scalar2=None, op0=ALU.mult),
                                      reads=[Bt3, Bdmask], writes=[Bkd2])
                        for c in range(6 if 'g' in PARTS else 0):
                            if c < 4:
                                wa_ = lambda j: win[:, j, OFF_GQ + c * 128: OFF_GQ + (c + 1) * 128]
                                wb_ = lambda j: wx[:, j, (4 + c) * 128:(5 + c) * 128]
                                g0 = 0
                            elif c == 4:
                                wa_ = lambda j: win[:, j, OFF_GK:OFF_GK + 128]
                                wb_ = lambda j: wx[:, j, 8 * 128:9 * 128]
                                g0 = 2
                            else:
                                wa_ = lambda j: wx[:, j, 9 * 128:10 * 128]
                                wb_ = lambda j: wx[:, j, 10 * 128:11 * 128]
                                g0 = 2
                            proj(4, wa_, n)
                            proj(5, wb_, n)
                            fw.op("act", lambda e: e.activation(out=sq2[:, :n], in_=ps_all[:, 4, :n], func=AF.Square), reads=[PB[4]], writes=[Bsq2])
                            fw.op("pe", lambda e: e.matmul(ps_all[:, 6, :n], lhsT=bd64[:], rhs=sq2[:, :n], start=True, stop=True), reads=[Bbd64, Bsq2], writes=[PB[6]])
                            rstd_from_ps(ps_all[:, 6, :n], rt2[:, :n], Brt2, PB[6])
                            fw.op("act", lambda e: e.activation(out=t1[:, :n], in_=ps_all[:, 4, :n], func=AF.Copy, scale=gqk[:, g0:g0 + 1]), reads=[PB[4], Bsmall], writes=[Bt1])
                            fw.op("act", lambda e: e.activation(out=t2[:, :n], in_=ps_all[:, 5, :n], func=AF.Copy, scale=gqk[:, g0 + 1:g0 + 2]), reads=[PB[5], Bsmall], writes=[Bt2])
                            fw.op("dve", lambda e: e.tensor_tensor(out=t1[:, :n], in0=t1[:, :n], in1=tabs[:, 0, :n], op=ALU.mult), reads=[Bt1, Btabs], writes=[Bt1])
                            fw.op("dve", lambda e: e.tensor_tensor(out=t2[:, :n], in0=t2[:, :n], in1=tabs[:, 1, :n], op=ALU.mult), reads=[Bt2, Btabs], writes=[Bt2])
                            fw.op("dve", lambda e: e.tensor_tensor(out=t3[:, :n], in0=t1[:, :n], in1=t2[:, :n], op=ALU.add), reads=[Bt1, Bt2], writes=[Bt3])
                            if c < 4:
                                dst, Bd = qst[:, 2 + c, :n], Bqst
                            elif c == 4:
                                dst, Bd = kgA[:, t0:t0 + n], BkgA
                            else:
                                dst, Bd = kgB[:, t0:t0 + n], BkgB
                            fw.op("dve", lambda e: e.tensor_tensor(out=dst, in0=t3[:, :n], in1=rt2[:, :n], op=ALU.mult), reads=[Bt3, Brt2], writes=[Bd])
                        fw.dma("sp", S["qd"][:, :, t0:t0 + n], qst[:, 0:2, :n], reads=[Bqst], writes=[B["qd"]])
                        fw.dma("sp", S["qg"][:, :, t0:t0 + n], qst[:, 2:6, :n], reads=[Bqst], writes=[B["qg"]])
                        for tt in range(n // 128 if 'v' in PARTS else 0):
                            kt = t0 // 128 + tt
                            pbk = 2 + (tt % 2)
                            if 'x' not in PARTS:
                                mm_group(ps_all[:, pbk, 0:256], [(ub[:, j, tt * 128:(tt + 1) * 128], win[:, j, OFF_DV:OFF_DV + 256]) for j in range(8)], [Bub, Bwin], PB[pbk])
                            if 'y' not in PARTS:
                                mm_group(ps_all[:, pbk, 256:384], [(ub[:, j, tt * 128:(tt + 1) * 128], win[:, j, OFF_GV:OFF_GV + 128]) for j in range(8)], [Bub, Bwin], PB[pbk])
                            if 'x' not in PARTS and 'z' not in PARTS:
                                fw.op("act", lambda e: e.copy(out=vd[:, kt, :, 0:64], in_=ps_all[:, pbk, 0:256].rearrange("p (h d) -> p h d", h=4)), reads=[PB[pbk]], writes=[Bvd])
                            if 'y' not in PARTS and 'z' not in PARTS:
                                fw.op("dve", lambda e: e.tensor_copy(out=vg[:, kt, :, 0:64], in_=ps_all[:, pbk, 256:384].rearrange("p (h d) -> p h d", h=2)), reads=[PB[pbk]], writes=[Bvg])
                    fw.barrier()
                if stop == "p1":
                    if debug:
                        with ExitStack() as ph:
                            dbg = sb("dbg", [128, 8840], F32, ph)
                            Bdbg = Buf()
                            for k_, (src_, Bs_) in enumerate(((kd1[:, 0, :], Bkd1), (kd1[:, 1, :], Bkd1), (kd2[:, 0, :], Bkd2), (kd2[:, 1, :], Bkd2), (kgA[:], BkgA), (kgB[:], BkgB))):
                                fw.op("dve", lambda e: e.tensor_copy(out=dbg[:, 0:NT], in_=src_), reads=[Bs_], writes=[Bdbg])
                                fw.dma("sp", S["dbg_kv"][:, k_, :], dbg[:, 0:NT], reads=[Bdbg], writes=[Bout])
                            fw.op("dve", lambda e: e.tensor_copy(out=dbg[:, 0:34 * 4 * 65], in_=vd[:].rearrange("p a b c -> p (a b c)")), reads=[Bvd], writes=[Bdbg])
                            fw.dma("sp", S["dbg_kv"][:, 6:9, :].rearrange("p a b -> p (a b)")[:, 0:8840], dbg[:, 0:8840], reads=[Bdbg], writes=[Bout])
                            fw.op("dve", lambda e: e.tensor_copy(out=dbg[:, 0:34 * 2 * 65], in_=vg[:].rearrange("p a b c -> p (a b c)")), reads=[Bvg], writes=[Bdbg])
                            fw.dma("sp", S["dbg_kv"][:, 9:11, :].rearrange("p a b -> p (a b)")[:, 0:4420], dbg[:, 0:4420], reads=[Bdbg], writes=[Bout])
                    fw.finish()
                    return nc, list(I.keys())

                with ExitStack() as ph:
                    qdb = [sb(f"qdb{k}", [128, 2, 512], BF16, ph) for k in range(2)]
                    qgb = [sb(f"qgb{k}", [128, 4, 512], BF16, ph) for k in range(2)]
                    Bqb = [Buf() for _ in range(2)]
                    pT = [sb(f"pT{k}", [128, 2, 512], BF16, ph) for k in range(3)]
                    BpT = [Buf() for _ in range(3)]
                    rec = [sb(f"rec{k}", [128, 512], F32, ph) for k in range(2)]
                    Brec = [Buf() for _ in range(2)]
                    tm = [sb(f"tm{k}", [128, 512], F32, ph) for k in range(2)]
                    Btm = [Buf() for _ in range(2)]
                    od = sb("od", [128, 512], F32, ph)
                    Bod = Buf()
                    sqd = sb("sqd", [128, 512], BF16, ph)
                    Bsqd = Buf()
                    rtd = sb("rtd", [128, 512], F32, ph)
                    Brtd = Buf()
                    of = [sb(f"of{k}", [128, 512], BF16, ph) for k in range(2)]
                    Bof = [Buf() for _ in range(2)]
                    cnt = {"pt": 0, "of": 0, "sp": 0, "pair": 0, "rec": 0}
                    deferred = []

                    def run_deferred(step=None):
                        while deferred and (step is None or deferred[0][0] <= step):
                            deferred.pop(0)[1]()

                    def attn_pair(kA_fn, kB_fn, q_t, ch, vA_fn, vB_fn, tiles, nq, scale):
                        pr = cnt["pair"] % 2
                        cnt["pair"] += 1
                        ob, db = (6, 7) if pr == 0 else (0, 1)
                        slots = []

                        def qk(i):
                            kt = tiles[i]
                            s_ = cnt["sp"] % 2
                            cnt["sp"] += 1
                            slots.append(s_)
                            fw.op("pe", lambda e: e.matmul(ps_all[:, 2 + 2 * s_, :nq], lhsT=kA_fn(kt), rhs=q_t[0:64, ch, :nq], start=True, stop=True),
                                  reads=[Bkd1, Bkd2, BkgA, BkgB] + Bqb, writes=[PB[2 + 2 * s_]], same=False, inc=False)
                            fw.op("pe", lambda e: e.matmul(ps_all[:, 3 + 2 * s_, :nq], lhsT=kB_fn(kt), rhs=q_t[64:128, ch, :nq], start=True, stop=True),
                                  reads=[Bkd1, Bkd2, BkgA, BkgB] + Bqb, writes=[PB[3 + 2 * s_]], same=False, inc=True)
                        qk(0)
                        nk = len(tiles)
                        for i, kt in enumerate(tiles):
                            if i + 1 < nk:
                                qk(i + 1)
                            s_ = slots[i]
                            pt_i = cnt["pt"] % 3
                            cnt["pt"] += 1
                            fw.op("act", lambda e: e.activation(out=pT[pt_i][:, :, :nq], in_=ps_all[:, 2 + 2 * s_:4 + 2 * s_, :nq], func=AF.Exp, scale=scale),
                                  reads=[PB[2 + 2 * s_], PB[3 + 2 * s_]], writes=[BpT[pt_i]])
                            first, last = (i == 0), (i == nk - 1)
                            for (bk_, lA, lB, Bl) in ((ob, vA_fn(kt), vB_fn(kt), [Bvd, Bvg]), (db, ones64[:, :], ones64[:, :], [Bones64])):
                                fw.op("pe", lambda e: e.matmul(ps_all[0:64, bk_, :nq], lhsT=lA, rhs=pT[pt_i][:, 0, :nq], start=first, stop=last,
                                                               skip_group_check=True, tile_position=(0, 0)),
                                      reads=Bl + [BpT[pt_i]], writes=[PB[bk_]], same=False, inc=False)
                                fw.op("pe", lambda e: e.matmul(ps_all[64:128, bk_, :nq], lhsT=lB, rhs=pT[pt_i][:, 1, :nq], start=first, stop=last,
                                                               skip_group_check=True, tile_position=(0, 64)),
                                      reads=Bl + [BpT[pt_i]], writes=[PB[bk_]], same=False, inc=(bk_ == db))
                            run_deferred(i)
                        run_deferred()
                        return ob, db

                    def recip_den(db, nq):
                        r_i = cnt["rec"] % 2
                        cnt["rec"] += 1
                        fw.op("dve", lambda e: e.reciprocal(out=rec[r_i][:, :nq], in_=ps_all[:, db, :nq]), reads=[PB[db]], writes=[Brec[r_i]])
                        return r_i

                    QBLK = [(0, NCX, [0, 1])] + [(NCX + 512 * i, 512, list(range(NT // 128))) for i in range(SEQ // 512)]
                    import os
                    QBLK = QBLK[:int(os.environ.get('DEV_NQB', '99'))]

                    def load_q(qi):
                        q0, nq, _ = QBLK[qi]
                        fw.dma("sp", qdb[qi % 2][:, :, :nq], S["qd"][:, :, q0:q0 + nq], reads=[B["qd"]], writes=[Bqb[qi % 2]])
                        fw.dma("sp", qgb[qi % 2][:, :, :nq], S["qg"][:, :, q0:q0 + nq], reads=[B["qg"]], writes=[Bqb[qi % 2]])
                    load_q(0)
                    for qi, (q0, nq, tiles) in enumerate(QBLK):
                        if qi + 1 < len(QBLK):
                            load_q(qi + 1)
                        qd_, qg_ = qdb[qi % 2], qgb[qi % 2]
                        for ch in range(2):
                            for m_ in range(2):
                                kk = kd1 if m_ == 0 else kd2
                                ob, db = attn_pair(lambda kt: kk[0:64, ch, kt * 128:(kt + 1) * 128], lambda kt: kk[64:128, ch, kt * 128:(kt + 1) * 128],
                                                   qd_, ch, lambda kt: vd[:, kt, 2 * ch, 0:64], lambda kt: vd[:, kt, 2 * ch + 1, 0:64], tiles, nq, 32 ** -0.5)
                                r_i = recip_den(db, nq)
                                if m_ == 1:
                                    fw.op("dve", lambda e: e.tensor_scalar(out=rec[r_i][:, :nq], in0=rec[r_i][:, :nq], scalar1=lamt[:, 0:1], scalar2=None, op0=ALU.mult),
                                          reads=[Brec[r_i], Blam], writes=[Brec[r_i]])
                                fw.op("dve", lambda e: e.tensor_tensor(out=tm[m_][:, :nq], in0=ps_all[:, ob, :nq], in1=rec[r_i][:, :nq], op=ALU.mult),
                                      reads=[PB[ob], Brec[r_i]], writes=[Btm[m_]])
                            fw.op("dve", lambda e: e.tensor_tensor(out=od[:, :nq], in0=tm[0][:, :nq], in1=tm[1][:, :nq], op=ALU.add), reads=[Btm[0], Btm[1]], writes=[Bod])

                            def stB(nq=nq):
                                fw.op("act", lambda e: e.activation(out=sqd[:, :nq], in_=od[:, :nq], func=AF.Square), reads=[Bod], writes=[Bsqd])

                            def stC(nq=nq, db=db):
                                fw.op("pe", lambda e: e.matmul(ps_all[:, db, :nq], lhsT=bd64[:], rhs=sqd[:, :nq], start=True, stop=True), reads=[Bsqd, Bbd64], writes=[PB[db]])

                            def stD(nq=nq, db=db, ch=ch, q0=q0):
                                rstd_from_ps(ps_all[:, db, :nq], rtd[:, :nq], Brtd, PB[db])
                                fw.op("dve", lambda e: e.tensor_tensor(out=od[:, :nq], in0=od[:, :nq], in1=rtd[:, :nq], op=ALU.mult), reads=[Bod, Brtd], writes=[Bod])
                                oi = cnt["of"] % 2
                                cnt["of"] += 1
                                fw.op("dve", lambda e: e.tensor_scalar(out=of[oi][:, :nq], in0=od[:, :nq], scalar1=lamt[:, 1:2], scalar2=None, op0=ALU.mult), reads=[Bod, Blam], writes=[Bof[oi]])
                                fw.dma("sp", S["mg"][:, 2 + ch, q0:q0 + nq], of[oi][:, :nq], reads=[Bof[oi]], writes=[B["mg"]])
                            deferred.extend([[3, stB], [5, stC], [8, stD]])
                        for ch in range(4):
                            g = ch // 2
                            kA = kgA if g == 0 else kgB
                            kB = kgA if g == 1 else kgB
                            ob, db = attn_pair(lambda kt: kA[0:64, kt * 128:(kt + 1) * 128], lambda kt: kB[64:128, kt * 128:(kt + 1) * 128],
                                               qg_, ch, lambda kt: vg[:, kt, g, 0:64], lambda kt: vg[:, kt, g, 0:64], tiles, nq, 64 ** -0.5)
                            r_i = recip_den(db, nq)
                            oi = cnt["of"] % 2
                            cnt["of"] += 1
                            fw.op("dve", lambda e: e.tensor_tensor(out=of[oi][:, :nq], in0=ps_all[:, ob, :nq], in1=rec[r_i][:, :nq], op=ALU.mult),
                                  reads=[PB[ob], Brec[r_i]], writes=[Bof[oi]])
                            fw.dma("sp", S["mg"][:, 4 + ch, q0:q0 + nq], of[oi][:, :nq], reads=[Bof[oi]], writes=[B["mg"]])
                    run_deferred()
                    fw.barrier()
            if stop == "p2":
                fw.finish()
                return nc, list(I.keys())

            with ExitStack() as ph:
                x0c = sb("x0c", [128, 2, NT], BF16, ph)
                v1 = sb("v1", [128, 2, NT], BF16, ph)
                Bx0c, Bv1 = Buf(), Buf()
                hsk = sb("hsk", [128, 2], F32, ph)
                Bhsk = Buf()
                fw.dma("sp", hsk[:], I["hy_skip_l"][li], reads=[Bin], writes=[Bhsk])
                def p3a_gen(lj, p3, bsin, bout):
                    w1 = sb("hw1", [33, 64], F32, p3)
                    w2 = sb("hw2", [64, 64], F32, p3)
                    w3 = sb("hw3", [64, 512], F32, p3)
                    hb = sb("hb", [64, 4], F32, p3)
                    Bhw = Buf()
                    fw.dma("sp", w1[:], I["hy_w1"][lj], reads=[Bin], writes=[Bhw])
                    fw.dma("sp", w2[:], I["hy_w2"][lj], reads=[Bin], writes=[Bhw])
                    fw.dma("sp", w3[:], I["hy_w3"][lj], reads=[Bin], writes=[Bhw])
                    fw.dma("sp", hb[:, 0:1], I["hy_b1_l"][lj], reads=[Bin], writes=[Bhw])
                    fw.dma("sp", hb[:, 1:2], I["hy_b2_l"][lj], reads=[Bin], writes=[Bhw])
                    fw.dma("sp", hb[:, 2:3], I["hy_fr_l"][lj], reads=[Bin], writes=[Bhw])
                    fw.op("dve", lambda e: e.tensor_scalar(out=hb[:, 0:2], in0=hb[:, 0:2], scalar1=hb[:, 2:3], scalar2=None, op0=ALU.mult), reads=[Bhw], writes=[Bhw])
                    ft = sb("ft", [33, 512], F32, p3)
                    dec = sb("dec", [128, 2, 512], F32, p3)
                    Bft, Bdec = Buf(), Buf()
                    pre = sb("pre", [64, 512], F32, p3)
                    mk = sb("mk", [64, 512], F32, p3)
                    act1 = sb("act1", [64, 512], F32, p3)
                    act2 = sb("act2", [64, 512], F32, p3)
                    Bpre, Bmk, Ba1, Ba2 = Buf(), Buf(), Buf(), Buf()
                    hrow = sb("hrow", [128, 2, 512], BF16, p3)
                    Bhrow = Buf()

                    def sin_layer(ps_ap, bcol, out_t, Bout_, PBk, n):
                        fw.op("dve", lambda e: e.tensor_scalar(out=pre[:, :n], in0=ps_ap, scalar1=hb[:, 2:3], scalar2=hb[:, bcol:bcol + 1], op0=ALU.mult, op1=ALU.add),
                              reads=[PBk, Bhw], writes=[Bpre])
                        fw.op("dve", lambda e: e.tensor_scalar(out=mk[:, :n], in0=pre[:, :n], scalar1=PI, scalar2=None, op0=ALU.is_gt), reads=[Bpre], writes=[Bmk])
                        fw.op("dve", lambda e: e.scalar_tensor_tensor(out=pre[:, :n], in0=mk[:, :n], scalar=-2 * PI, in1=pre[:, :n], op0=ALU.mult, op1=ALU.add), reads=[Bmk, Bpre], writes=[Bpre])
                        fw.op("dve", lambda e: e.tensor_scalar(out=mk[:, :n], in0=pre[:, :n], scalar1=-PI, scalar2=None, op0=ALU.is_lt), reads=[Bpre], writes=[Bmk])
                        fw.op("dve", lambda e: e.scalar_tensor_tensor(out=pre[:, :n], in0=mk[:, :n], scalar=2 * PI, in1=pre[:, :n], op0=ALU.mult, op1=ALU.add), reads=[Bmk, Bpre], writes=[Bpre])
                        fw.op("act", lambda e: e.activation(out=out_t[:, :n], in_=pre[:, :n], func=AF.Sin), reads=[Bpre], writes=[Bout_])

                    hs_ = lj % 2
                    for (L, kf, kfr, kd_, kdr, hkey) in ((SEQ, "featsT", "featsTr", "decT", "decTr", f"hrev{hs_}"), (NCX, "featsTc", "featsTcr", "decTc", "decTcr", f"hrevc{hs_}")):
                        hdst = S[hkey].rearrange("(c p) x -> p c x", p=128)
                        for direction in range(2):
                            fkey, dkey = (kfr, kdr) if direction == 0 else (kf, kd_)
                            colb = 0 if direction == 0 else 256
                            for p0 in range(0, L, 512):
                                n = min(512, L - p0)
                                fw.dma("sp", ft[:, :n], I[fkey][:, p0:p0 + n], reads=[Bin], writes=[Bft])
                                fw.dma("sp", dec[:, :, :n], I[dkey][:, :, p0:p0 + n], reads=[Bin], writes=[Bdec])
                                fw.op("pe", lambda e: e.matmul(ps_all[0:64, bsin[0], :n], lhsT=w1[:, :], rhs=ft[:, :n], start=True, stop=True), reads=[Bhw, Bft], writes=[PB[bsin[0]]])
                                sin_layer(ps_all[0:64, bsin[0], :n], 0, act1, Ba1, PB[bsin[0]], n)
                                yield
                                fw.op("pe", lambda e: e.matmul(ps_all[0:64, bsin[1], :n], lhsT=w2[:, :], rhs=act1[:, :n], start=True, stop=True), reads=[Bhw, Ba1], writes=[PB[bsin[1]]])
                                sin_layer(ps_all[0:64, bsin[1], :n], 1, act2, Ba2, PB[bsin[1]], n)
                                yield
                                for cc in range(2):
                                    fw.op("pe", lambda e: e.matmul(ps_all[:, bout[cc], :n], lhsT=w3[:, colb + cc * 128: colb + (cc + 1) * 128], rhs=act2[:, :n], start=True, stop=True),
                                          reads=[Bhw, Ba2], writes=[PB[bout[cc]]])
                                    fw.op("dve", lambda e: e.tensor_tensor(out=hrow[:, cc, :n], in0=ps_all[:, bout[cc], :n], in1=dec[:, cc, :n], op=ALU.mult), reads=[PB[bout[cc]], Bdec], writes=[Bhrow])
                                if direction == 0:
                                    fw.dma("sp", hdst[:, :, p0:p0 + n], hrow[:, :, :n], reads=[Bhrow], writes=[B[hkey]])
                                else:
                                    i0 = 1 if p0 == 0 else 0
                                    fw.dma("sp", hdst[:, :, L - 1 + p0 + i0: L - 1 + p0 + n], hrow[:, :, i0:n], reads=[Bhrow], writes=[B[hkey]])
                                yield

                if li == 0:
                    with ExitStack() as p3:
                        for _ in p3a_gen(0, p3, (0, 1), (2, 3)):
                            pass
                        fw.barrier()
                if stop == "p3a":
                    fw.finish()
                    return nc, list(I.keys())
                with ExitStack() as p3:
                    cw = sb("cw", [128, 6, 3], F32, p3)
                    cb = sb("cb", [128, 6], F32, p3)
                    Bcw = Buf()
                    fw.dma("sp", cw[:], I["hy_cw_l"][li], reads=[Bin], writes=[Bcw])
                    fw.dma("sp", cb[:], I["hy_cb_l"][li], reads=[Bin], writes=[Bcw])
                    zc = [sb(f"zc{k}", [128, NT], BF16, p3) for k in range(2)]
                    Bzc = [Buf() for _ in range(2)]
                    yA = sb("yA", [128, NT], F32, p3)
                    yB = sb("yB", [128, NT], F32, p3)
                    ByA, ByB = Buf(), Buf()
                    zi = 0
                    for cc in range(2):
                        for role, chunk in (("x1", 2 + cc), ("v", 4 + cc), ("x0", cc)):
                            z_, Bz_ = zc[zi % 2], Bzc[zi % 2]
                            zi += 1
                            fw.dma("sp", z_[:], S["zhy"][:, chunk, :], reads=[B["zhy"]], writes=[Bz_])
                            y_, By_ = (yA, ByA) if role == "x1" else (yB, ByB)
                            fw.op("dve", lambda e: e.tensor_scalar(out=y_[:], in0=z_[:], scalar1=cw[:, chunk, 1:2], scalar2=cb[:, chunk:chunk + 1], op0=ALU.mult, op1=ALU.add),
                                  reads=[Bz_, Bcw], writes=[By_])
                            for (s0, e0) in ((0, NCX), (NCX, NT)):
                                fw.op("dve", lambda e: e.scalar_tensor_tensor(out=y_[:, s0 + 1:e0], in0=z_[:, s0:e0 - 1], scalar=cw[:, chunk, 0:1], in1=y_[:, s0 + 1:e0], op0=ALU.mult, op1=ALU.add),
                                      reads=[Bz_, Bcw, By_], writes=[By_])
                                fw.op("dve", lambda e: e.scalar_tensor_tensor(out=y_[:, s0:e0 - 1], in0=z_[:, s0 + 1:e0], scalar=cw[:, chunk, 2:3], in1=y_[:, s0:e0 - 1], op0=ALU.mult, op1=ALU.add),
                                      reads=[Bz_, Bcw, By_], writes=[By_])
                            if role == "v":
                                fw.op("dve", lambda e: e.tensor_tensor(out=v1[:, cc, :], in0=yB[:], in1=yA[:], op=ALU.mult), reads=[ByA, ByB], writes=[Bv1])
                            elif role == "x0":
                                fw.op("act", lambda e: e.copy(out=x0c[:, cc, :], in_=yB[:]), reads=[ByB], writes=[Bx0c])
                    fw.barrier()
                with ExitStack() as p3:
                    NTL = NT // 128
                    Vt = sb("Vt", [128, 256, NTL], BF16, p3)
                    BVt = Buf()
                    Ysb = sb("Ysb", [128, NTL, 256], BF16, p3)
                    BYsb = Buf()
                    G = [sb(f"G{k}", [128, 8064], BF16, p3) for k in range(2)]
                    BG = [Buf() for _ in range(2)]
                    Gc = sb("Gc", [128, 16, 384], BF16, p3)
                    BGc = Buf()
                    hyo = sb("hyo", [128, 2, NT], BF16, p3)
                    Bhyo = Buf()
                    tmpf = sb("tmpf", [128, 512], F32, p3)
                    Btmpf = Buf()
                    gen3a = p3a_gen(li + 1, p3, (0, 1), (6, 7)) if li + 1 < depth else None
                    hkl, hkc = f"hrev{li % 2}", f"hrevc{li % 2}"
                    k4 = 0
                    for cc in range(2):
                        for j0 in range(0, NTL, 4):
                            nj = min(4, NTL - j0)
                            bk = k4 % 2
                            k4 += 1
                            for k in range(nj):
                                fw.op("pe", lambda e: e.matmul(ps_all[:, bk, k * 128:(k + 1) * 128], lhsT=v1[:, cc, (j0 + k) * 128:(j0 + k + 1) * 128], rhs=ident_bf[:], start=True, stop=True),
                                      reads=[Bv1, Bident], writes=[PB[bk]], same=False, inc=(k == nj - 1))
                            dstv = Vt[:, cc * 128:(cc + 1) * 128, j0:j0 + nj].rearrange("p c j -> p j c")
                            srcv = ps_all[:, bk, 0:nj * 128].rearrange("p (j c) -> p j c", j=nj)
                            if bk == 0:
                                fw.op("dve", lambda e: e.tensor_copy(out=dstv, in_=srcv), reads=[PB[bk]], writes=[BVt])
                            else:
                                fw.op("act", lambda e: e.copy(out=dstv, in_=srcv), reads=[PB[bk]], writes=[BVt])
                    import os
                    NCH = int(os.environ.get('DEV_NCH', '256'))
                    for c in range(NCH):
                        g_, Bg_ = G[c % 2], BG[c % 2]
                        src = bass.AP(S[hkl].tensor, c * 2 * SEQ, [[1, 128], [1, 8064]])
                        fw.dma("sp", g_[:], src, reads=[B[hkl]], writes=[Bg_])
                        bk = 2 + (c // 16) % 2
                        col0 = (c % 16) * 32
                        ds = [0] + [d for d in range(-31, 32) if d != 0]
                        for di, d in enumerate(ds):
                            o_d = SEQ - 128 - 128 * d
                            if d >= 0:
                                oc0, oc1, j0_ = d, 32, 0
                            else:
                                oc0, oc1, j0_ = 0, 32 + d, -d
                            nn = oc1 - oc0
                            last = (di == len(ds) - 1) and (c % 16 == 15 or c == NCH - 1)
                            fw.op("pe", lambda e: e.matmul(ps_all[:, bk, col0 + oc0:col0 + oc1], lhsT=g_[:, o_d:o_d + 128], rhs=Vt[:, c, 2 + j0_:2 + j0_ + nn],
                                                           start=(di == 0 and c % 16 == 0), stop=last, skip_group_check=True),
                                  reads=[Bg_, BVt], writes=[PB[bk]], same=False, inc=(di == len(ds) - 1))
                        if c % 16 == 15 or c == NCH - 1:
                            c0 = c - (c % 16)
                            ncg = c - c0 + 1
                            dsty = Ysb[:, 2:NTL, c0:c0 + ncg].rearrange("p i c -> p c i")
                            srcy = ps_all[:, bk, 0:ncg * 32].rearrange("p (c i) -> p c i", c=ncg)
                            if bk == 2:
                                fw.op("dve", lambda e: e.tensor_copy(out=dsty, in_=srcy), reads=[PB[bk]], writes=[BYsb])
                            else:
                                fw.op("act", lambda e: e.copy(out=dsty, in_=srcy), reads=[PB[bk]], writes=[BYsb])
                        if gen3a is not None and c % 4 == 3:
                            next(gen3a, None)
                    for c0 in range(0, min(NCH, 256), 16):
                        src = bass.AP(S[hkc].tensor, c0 * 2 * NCX, [[1, 128], [2 * NCX, 16], [1, 384]])
                        fw.dma("sp", Gc[:], src, reads=[B[hkc]], writes=[BGc])
                        bk = 4 + (c0 // 16) % 2
                        for cl in range(16):
                            c = c0 + cl
                            for di, d in enumerate((0, 1, -1)):
                                o_d = NCX - 128 - 128 * d
                                if d == 0:
                                    oc0, oc1, j0_ = 0, 2, 0
                                elif d == 1:
                                    oc0, oc1, j0_ = 1, 2, 0
                                else:
                                    oc0, oc1, j0_ = 0, 1, 1
                                nn = oc1 - oc0
                                last = (di == 2 and cl == 15)
                                fw.op("pe", lambda e: e.matmul(ps_all[:, bk, cl * 2 + oc0:cl * 2 + oc1], lhsT=Gc[:, cl, o_d:o_d + 128], rhs=Vt[:, c, j0_:j0_ + nn],
                                                               start=(di == 0 and cl == 0), stop=last, skip_group_check=True),
                                      reads=[BGc, BVt], writes=[PB[bk]], same=False, inc=last)
                        dsty = Ysb[:, 0:2, c0:c0 + 16].rearrange("p i c -> p c i")
                        srcy = ps_all[:, bk, 0:32].rearrange("p (c i) -> p c i", c=16)
                        fw.op("dve", lambda e: e.tensor_copy(out=dsty, in_=srcy), reads=[PB[bk]], writes=[BYsb])
                    if gen3a is not None:
                        for _ in gen3a:
                            pass
                    k4 = 0
                    for cc in range(2):
                        for i0 in range(0, NTL, 4):
                            ni = min(4, NTL - i0)
                            bk = 6 + k4 % 2
                            k4 += 1
                            for k in range(ni):
                                fw.op("pe", lambda e: e.matmul(ps_all[:, bk, k * 128:(k + 1) * 128], lhsT=Ysb[:, i0 + k, cc * 128:(cc + 1) * 128], rhs=anti_bf[:], start=True, stop=True),
                                      reads=[BYsb, Banti], writes=[PB[bk]], same=False, inc=(k == ni - 1))
                            tsl = slice(i0 * 128, (i0 + ni) * 128)
                            nn = ni * 128
                            fw.op("dve", lambda e: e.scalar_tensor_tensor(out=tmpf[:, :nn], in0=v1[:, cc, tsl], scalar=hsk[:, cc:cc + 1], in1=ps_all[:, bk, :nn], op0=ALU.mult, op1=ALU.add),
                                  reads=[Bv1, Bhsk, PB[bk]], writes=[Btmpf])
                            fw.op("dve", lambda e: e.tensor_tensor(out=hyo[:, cc, tsl], in0=tmpf[:, :nn], in1=x0c[:, cc, tsl], op=ALU.mult), reads=[Btmpf, Bx0c], writes=[Bhyo])
                    fw.dma("sp", S["mg"][:, 0:2, :], hyo[:], reads=[Bhyo], writes=[B["mg"]])
                    fw.barrier()
            if stop == "p3":
                fw.finish()
                return nc, list(I.keys())

            with ExitStack() as lf:
                gT = sb("gT", [8, NT], BF16, lf)
                BgT = Buf()
                with ExitStack() as ph:
                    wout = sb("wout", [128, 8, D], BF16, ph)
                    Bwout = Buf()
                    fw.dma("pool", wout[:], I["w_out"][li].rearrange("(j p) n -> p j n", p=128), reads=[Bin], writes=[Bwout])
                    mgb2 = [sb(f"mgb{k}", [128, 8, 512], BF16, ph) for k in range(2)]
                    Bmgb2 = [Buf() for _ in range(2)]
                    hblk2 = [sb(f"hblk4{k}", [128, 8, 512], F32, ph) for k in range(2)]
                    Bhblk2 = [Buf() for _ in range(2)]
                    sq = sb("sq4", [128, 8, 512], BF16, ph)
                    Bsq = Buf()
                    rt = sb("rt4", [128, 512], F32, ph)
                    Brt = Buf()
                    tmp = sb("tmp4", [128, 512], F32, ph)
                    Btmp = Buf()
                    u2b = sb("u2b", [128, 8, 512], BF16, ph)
                    Bu2b = Buf()
                    if moe:
                        u2f = sb("u2f", [128, 8, 512], F32, ph)
                        Bu2f = Buf()
                        rtr = sb("rtr", [128, 8, NEXP], F32, ph)
                        Brtr = Buf()
                        fw.dma("sp", rtr[:], I["moe_router"][li // 2].rearrange("(j p) n -> p j n", p=128), reads=[Bin], writes=[Brtr])
                        lg = sb("lg", [128, 8], F32, ph)
                        l2 = sb("l2", [128, 8], F32, ph)
                        eq1 = sb("eq1", [128, 8], F32, ph)
                        eq2 = sb("eq2", [128, 8], F32, ph)
                        gts = sb("gts", [128, 8], F32, ph)
                        mm_ = sb("mm_", [128, 8], F32, ph)
                        Brt_ = Buf()
                    for bi4, (t0, n, seg) in enumerate(TBLK):
                        mgb, Bmgb, hblk, Bhblk = mgb2[bi4 % 2], Bmgb2[bi4 % 2], hblk2[bi4 % 2], Bhblk2[bi4 % 2]
                        fw.dma("sp", mgb[:, :, :n], S["mg"][:, :, t0:t0 + n], reads=[B["mg"]], writes=[Bmgb])
                        fw.dma("sp", hblk[:, :, :n], S["hT"][:, :, t0:t0 + n], reads=[B["hT"]], writes=[Bhblk])
                        for dc in range(8):
                            bk = dc % 2
                            mm_group(ps_all[:, bk, :n], [(wout[:, j, dc * 128:(dc + 1) * 128], mgb[:, j, :n]) for j in range(8)], [Bwout, Bmgb], PB[bk])
                            fw.op("dve", lambda e: e.scalar_tensor_tensor(out=hblk[:, dc, :n], in0=ps_all[:, bk, :n], scalar=prm[:, 2, dc, seg:seg + 1], in1=hblk[:, dc, :n], op0=ALU.mult, op1=ALU.add),
                                  reads=[PB[bk], Bprm, Bhblk], writes=[Bhblk])
                        fw.dma("sp", S["hT"][:, :, t0:t0 + n], hblk[:, :, :n], reads=[Bhblk], writes=[B["hT"]])
                        outs = [(u2b, Bu2b)] + ([(u2f, Bu2f)] if moe else [])
                        norm_block(hblk, Bhblk, n, seg, 3, sq, Bsq, rt, Brt, tmp, Btmp, outs)
                        fw.dma("sp", S["u2"][:, :, t0:t0 + n], u2b[:, :, :n], reads=[Bu2b], writes=[B["u2"]])
                        if moe:
                            for tt in range(n // 128):
                                tok0 = t0 + tt * 128
                                mm_group(ps_all[:, 2, 0:8], [(u2f[:, j, tt * 128:(tt + 1) * 128], rtr[:, j, :]) for j in range(8)], [Bu2f, Brtr], PB[2])
                                fw.op("dve", lambda e: e.tensor_copy(out=lg[:], in_=ps_all[:, 2, 0:8]), reads=[PB[2]], writes=[Brt_])
                                fw.op("dve", lambda e: e.reduce_max(out=mm_[:, 0:1], in_=lg[:], axis=AX.X), reads=[Brt_], writes=[Brt_])
                                fw.op("dve", lambda e: e.tensor_scalar(out=eq1[:], in0=lg[:], scalar1=mm_[:, 0:1], scalar2=None, op0=ALU.is_equal), reads=[Brt_], writes=[Brt_])
                                fw.op("dve", lambda e: e.scalar_tensor_tensor(out=l2[:], in0=eq1[:], scalar=-1e30, in1=lg[:], op0=ALU.mult, op1=ALU.add), reads=[Brt_], writes=[Brt_])
                                fw.op("dve", lambda e: e.reduce_max(out=mm_[:, 1:2], in_=l2[:], axis=AX.X), reads=[Brt_], writes=[Brt_])
                                fw.op("dve", lambda e: e.tensor_scalar(out=eq2[:], in0=l2[:], scalar1=mm_[:, 1:2], scalar2=None, op0=ALU.is_equal), reads=[Brt_], writes=[Brt_])
                                fw.op("dve", lambda e: e.tensor_tensor(out=mm_[:, 2:3], in0=mm_[:, 1:2], in1=mm_[:, 0:1], op=ALU.subtract), reads=[Brt_], writes=[Brt_])
                                fw.op("act", lambda e: e.activation(out=mm_[:, 3:4], in_=mm_[:, 2:3], func=AF.Exp), reads=[Brt_], writes=[Brt_])
                                fw.op("dve", lambda e: e.tensor_scalar(out=mm_[:, 4:5], in0=mm_[:, 3:4], scalar1=1.0, scalar2=None, op0=ALU.add), reads=[Brt_], writes=[Brt_])
                                fw.op("dve", lambda e: e.reciprocal(out=mm_[:, 5:6], in_=mm_[:, 4:5]), reads=[Brt_], writes=[Brt_])
                                fw.op("dve", lambda e: e.tensor_tensor(out=mm_[:, 6:7], in0=mm_[:, 3:4], in1=mm_[:, 5:6], op=ALU.mult), reads=[Brt_], writes=[Brt_])
                                fw.op("dve", lambda e: e.tensor_scalar(out=gts[:], in0=eq1[:], scalar1=mm_[:, 5:6], scalar2=None, op0=ALU.mult), reads=[Brt_], writes=[Brt_])
                                fw.op("dve", lambda e: e.scalar_tensor_tensor(out=gts[:], in0=eq2[:], scalar=mm_[:, 6:7], in1=gts[:], op0=ALU.mult, op1=ALU.add), reads=[Brt_], writes=[Brt_])
                                fw.op("pe", lambda e: e.matmul(ps_all[0:8, 3, 0:128], lhsT=gts[:], rhs=ident_f[:], start=True, stop=True), reads=[Brt_, Bidentf], writes=[PB[3]])
                                fw.op("dve", lambda e: e.tensor_copy(out=gT[:, tok0:tok0 + 128], in_=ps_all[0:8, 3, 0:128]), reads=[PB[3]], writes=[BgT])
                    fw.barrier()
                if stop == "p4":
                    if debug and moe:
                        with ExitStack() as ph:
                            dbg = sb("dbg4", [8, NT], F32, ph)
                            Bdbg = Buf()
                            fw.op("dve", lambda e: e.tensor_copy(out=dbg[:], in_=gT[:]), reads=[BgT], writes=[Bdbg])
                            fw.dma("sp", S["dbg_kv"][0:8, 0, :], dbg[:], reads=[Bdbg], writes=[Bout])
                    fw.finish()
                    return nc, list(I.keys())

                with ExitStack() as ph:
                    SBL = [(0, 1536), (1536, 1536), (3072, NT - 3072)]
                    SBW = 1536
                    if moe:
                        groups = [(e_, f0, 512) for e_ in range(NEXP) for f0 in range(0, DFE, 512)]
                    else:
                        groups = [(0, f0, min(512, DFF - f0)) for f0 in range(0, DFF, 512)]

                    def wviews(e_):
                        if moe:
                            return (I["moe_wg"][li // 2][e_].rearrange("(j p) f -> p j f", p=128),
                                    I["moe_wu"][li // 2][e_].rearrange("(j p) f -> p j f", p=128),
                                    I["moe_wd"][li // 2][e_].rearrange("(c p) d -> p c d", p=128))
                        return (I["ffn_wg"][li // 2].rearrange("(j p) f -> p j f", p=128),
                                I["ffn_wu"][li // 2].rearrange("(j p) f -> p j f", p=128),
                                I["ffn_wd"][li // 2].rearrange("(c p) d -> p c d", p=128))
                    wgb = [sb(f"wgb{k}", [128, 8, 512], BF16, ph) for k in range(2)]
                    wub = [sb(f"wub{k}", [128, 8, 512], BF16, ph) for k in range(2)]
                    wdb = [sb(f"wdb{k}", [128, 4, D], BF16, ph) for k in range(2)]
                    Bw = [Buf() for _ in range(2)]
                    u2k = sb("u2k", [128, 8, SBW], BF16, ph)
                    Bu2k = Buf()
                    acc = sb("acc", [128, 8, SBW], F32, ph)
                    Bacc = Buf()
                    Hb = [sb(f"Hb{k}", [128, 4, SBW], BF16, ph) for k in range(2)]
                    BH = [Buf() for _ in range(2)]
                    sgt = [sb(f"sgt{k}", [128, 512], BF16, ph) for k in range(2)]
                    Bsgt = [Buf() for _ in range(2)]
                    gbc = sb("gbc", [128, SBW], BF16, ph)
                    Bgbc = Buf()
                    hres = sb("hres", [128, SBW], F32, ph)
                    Bhres = Buf()
                    import os
                    NSB = int(os.environ.get('DEV_NSB', '3'))
                    NGR = int(os.environ.get('DEV_NGR', '999'))
                    groups = groups[:NGR]

                    Bwd = [Buf() for _ in range(2)]

                    def load_w(gi):
                        e_, f0, nf = groups[gi]
                        wg_, wu_, wd_ = wviews(e_)
                        k = gi % 2
                        fw.dma("pool", wgb[k][:, :, :nf], wg_[:, :, f0:f0 + nf], reads=[Bin], writes=[Bw[k]])
                        fw.dma("pool", wub[k][:, :, :nf], wu_[:, :, f0:f0 + nf], reads=[Bin], writes=[Bw[k]])

                    def load_wd(gi):
                        e_, f0, nf = groups[gi]
                        wg_, wu_, wd_ = wviews(e_)
                        k = gi % 2
                        fw.dma("pool", wdb[k][:, :nf // 128, :], wd_[:, f0 // 128:(f0 + nf) // 128, :], reads=[Bin], writes=[Bwd[k]])

                    for sbi in range(NSB):
                        t0, sw = SBL[sbi]
                        subs = [(s0, min(512, sw - s0)) for s0 in range(0, sw, 512)]
                        fw.dma("sp", u2k[:, :, :sw], S["u2"][:, :, t0:t0 + sw], reads=[B["u2"]], writes=[Bu2k])
                        fw.op("pool", lambda e: e.memset(acc[:], 0.0), writes=[Bacc])
                        load_w(0)
                        load_wd(0)
                        cnt5 = {"sg": 0, "ob": 0}

                        def phaseB_units(gi):
                            e_, f0, nf = groups[gi]
                            k = gi % 2
                            nfc = nf // 128
                            units = []
                            for dc in range(8):
                                for (s0, ns) in subs:
                                    units.append((k, nfc, dc, s0, ns))
                            return units

                        def emitB(unit):
                            k, nfc, dc, s0, ns = unit
                            ob = 6 + cnt5["ob"] % 2
                            cnt5["ob"] += 1
                            mm_group(ps_all[:, ob, :ns], [(wdb[k][:, fc2, dc * 128:(dc + 1) * 128], Hb[k][:, fc2, s0:s0 + ns]) for fc2 in range(nfc)], [Bwd[k], BH[k]], PB[ob])
                            fw.op("dve", lambda e: e.tensor_tensor(out=acc[:, dc, s0:s0 + ns], in0=acc[:, dc, s0:s0 + ns], in1=ps_all[:, ob, :ns], op=ALU.add),
                                  reads=[Bacc, PB[ob]], writes=[Bacc])

                        pendingB = []
                        cur_e = -1
                        for gi, (e_, f0, nf) in enumerate(groups):
                            if gi + 1 < len(groups):
                                load_w(gi + 1)
                            k = gi % 2
                            nfc = nf // 128
                            if moe and e_ != cur_e:
                                cur_e = e_
                                for (s0, ns) in subs:
                                    fw.op("pe", lambda e: e.matmul(ps_all[:, 7, :ns], lhsT=sel_bf[:, e_, :], rhs=gT[:, t0 + s0:t0 + s0 + ns], start=True, stop=True),
                                          reads=[Bsel, BgT], writes=[PB[7]])
                                    fw.op("act", lambda e: e.copy(out=gbc[:, s0:s0 + ns], in_=ps_all[:, 7, :ns]), reads=[PB[7]], writes=[Bgbc])
                            per_fc = (len(pendingB) + nfc - 1) // nfc if pendingB else 0
                            for fc in range(nfc):
                                for si, (s0, ns) in enumerate(subs):
                                    gbk, ubk = (2 + si, 4 + si) if si < 2 else (0, 1)
                                    gcol = 0
                                    ucol = 0
                                    mm_group(ps_all[:, gbk, gcol:gcol + ns], [(wgb[k][:, j, fc * 128:(fc + 1) * 128], u2k[:, j, s0:s0 + ns]) for j in range(8)], [Bw[k], Bu2k], PB[gbk])
                                    mm_group(ps_all[:, ubk, ucol:ucol + ns], [(wub[k][:, j, fc * 128:(fc + 1) * 128], u2k[:, j, s0:s0 + ns]) for j in range(8)], [Bw[k], Bu2k], PB[ubk])
                                    sgi = cnt5["sg"] % 2
                                    cnt5["sg"] += 1
                                    fw.op("act", lambda e: e.activation(out=sgt[sgi][:, :ns], in_=ps_all[:, gbk, gcol:gcol + ns], func=AF.Silu), reads=[PB[gbk]], writes=[Bsgt[sgi]])
                                    if moe:
                                        fw.op("dve", lambda e: e.tensor_tensor(out=sgt[sgi][:, :ns], in0=sgt[sgi][:, :ns], in1=gbc[:, s0:s0 + ns], op=ALU.mult), reads=[Bsgt[sgi], Bgbc], writes=[Bsgt[sgi]])
                                    fw.op("dve", lambda e: e.tensor_tensor(out=Hb[k][:, fc, s0:s0 + ns], in0=sgt[sgi][:, :ns], in1=ps_all[:, ubk, ucol:ucol + ns], op=ALU.mult),
                                          reads=[Bsgt[sgi], PB[ubk]], writes=[BH[k]])
                                for _ in range(per_fc):
                                    if pendingB:
                                        emitB(pendingB.pop(0))
                            while pendingB:
                                emitB(pendingB.pop(0))
                            if gi + 1 < len(groups):
                                load_wd(gi + 1)
                            pendingB = phaseB_units(gi)
                        while pendingB:
                            emitB(pendingB.pop(0))
                        for dc in range(8):
                            fw.dma("sp", hres[:, :sw], S["hT"][:, dc, t0:t0 + sw], reads=[B["hT"]], writes=[Bhres])
                            rngs = []
                            if t0 < NCX:
                                rngs.append((0, NCX - t0, 1))
                                rngs.append((NCX - t0, sw, 0))
                            else:
                                rngs.append((0, sw, 0))
                            for (c0, c1, seg) in rngs:
                                fw.op("dve", lambda e: e.scalar_tensor_tensor(out=hres[:, c0:c1], in0=acc[:, dc, c0:c1], scalar=prm[:, 5, dc, seg:seg + 1], in1=hres[:, c0:c1], op0=ALU.mult, op1=ALU.add),
                                      reads=[Bacc, Bprm, Bhres], writes=[Bhres])
                            fw.dma("sp", S["hT"][:, dc, t0:t0 + sw], hres[:, :sw], reads=[Bhres], writes=[B["hT"]])
                    fw.barrier()
            if stop == f"l{li}":
                fw.finish()
                return nc, list(I.keys())

        with ExitStack() as ph:
            hblk = sb("hblkF", [128, 8, 512], F32, ph)
            Bhblk = Buf()
            sq = sb("sqF", [128, 8, 512], BF16, ph)
            Bsq = Buf()
            rt = sb("rtF", [128, 512], F32, ph)
            Brt = Buf()
            hn = sb("hnF", [128, 8, 512], F32, ph)
            Bhn = Buf()
            ot = [sb(f"otF{k}", [128, D], F32, ph) for k in range(2)]
            Bot = [Buf() for _ in range(2)]
            kk = 0
            for (t0, n, seg) in TBLK[1:]:
                fw.dma("sp", hblk[:, :, :n], S["hT"][:, :, t0:t0 + n], reads=[B["hT"]], writes=[Bhblk])
                fw.op("act", lambda e: e.activation(out=sq[:, :, :n], in_=hblk[:, :, :n], func=AF.Square), reads=[Bhblk], writes=[Bsq])
                mm_group(ps_all[:, 0, :n], [(onesD[:], sq[:, j, :n]) for j in range(8)], [BonesD, Bsq], PB[0])
                rstd_from_ps(ps_all[:, 0, :n], rt[:, :n], Brt, PB[0])
                for j in range(8):
                    fw.op("dve", lambda e: e.scalar_tensor_tensor(out=hn[:, j, :n], in0=hblk[:, j, :n], scalar=gfin[:, j:j + 1], in1=rt[:, :n], op0=ALU.mult, op1=ALU.mult),
                          reads=[Bhblk, Bgfin, Brt], writes=[Bhn])
                for tt in range(n // 128):
                    o_, Bo_ = ot[kk % 2], Bot[kk % 2]
                    for half in range(2):
                        bk = 2 + 2 * (kk % 2) + half
                        for jj in range(4):
                            j = half * 4 + jj
                            fw.op("pe", lambda e: e.matmul(ps_all[:, bk, jj * 128:(jj + 1) * 128], lhsT=hn[:, j, tt * 128:(tt + 1) * 128], rhs=ident_f[:], start=True, stop=True),
                                  reads=[Bhn, Bidentf], writes=[PB[bk]], same=False, inc=(jj == 3))
                        if half == 0:
                            fw.op("dve", lambda e: e.tensor_copy(out=o_[:, 0:512], in_=ps_all[:, bk, :]), reads=[PB[bk]], writes=[Bo_])
                        else:
                            fw.op("act", lambda e: e.copy(out=o_[:, 512:1024], in_=ps_all[:, bk, :]), reads=[PB[bk]], writes=[Bo_])
                    r0 = t0 - NCX + tt * 128
                    fw.dma("sp", out[r0:r0 + 128, :], o_[:], reads=[Bo_], writes=[Bout])
                    kk += 1
        fw.finish()
        return nc, list(I.keys())

    return nc, list(I.keys())


def _in_map(inputs, b, consts, names, small=None):
    small = small if small is not None else _layer_small(inputs, b)
    m = {}
    for k in names:
        if k == "x":
            m[k] = np.ascontiguousarray(inputs["x"][b], dtype=np.float32)
        elif k == "ctx":
            m[k] = np.ascontiguousarray(inputs["ctx"][b], dtype=np.float32)
        elif k in small:
            m[k] = small[k]
        elif k in consts:
            m["k_" + k] = consts[k]
        else:
            m[k] = np.ascontiguousarray(inputs[k], dtype=np.float32)
    return m


def kernel(**inputs):
    consts = _constants()
    nc, names = build_program()
    big = {k: np.ascontiguousarray(inputs[k], dtype=np.float32) for k in names if k in BIG_KEYS}
    in_maps = []
    for b in range(8):
        m = _in_map(inputs, b, consts, [k for k in names if k not in BIG_KEYS])
        m.update(big)
        in_maps.append(m)
    res = run_bass_kernel_spmd(nc, in_maps, core_ids=list(range(8)))
    return np.stack([np.asarray(r["out"], np.float32) for r in res.results], axis=0)
```

```python
import math
from contextlib import ExitStack

import numpy as np
import ml_dtypes
import concourse.bass as bass
import concourse.mybir as mybir
from concourse.bass_utils import run_bass_kernel_spmd

F32 = mybir.dt.float32
BF16 = mybir.dt.bfloat16
AF = mybir.ActivationFunctionType
ALU = mybir.AluOpType
AX = mybir.AxisListType

D = 1024
NJ = 8
SEQ = 4096
NCX = 256
NT = SEQ + NCX
DEPTH = 4
GRID_W = 64
EPS = 1e-6
DFF = 2816
DFE = 3584
NEXP = 8
OFF_DQ, OFF_DK, OFF_DV, OFF_GQ, OFF_GK, OFF_GV, IN_COLS = 768, 1024, 1280, 1536, 2048, 2176, 2304
PI = float(np.pi)


class Buf:
    __slots__ = ("name", "w", "r", "excl")

    def __init__(self, name="", excl=False):
        self.name = name
        self.w = None
        self.r = {}
        self.excl = excl


class _Eng:
    def __init__(self, name, eng, sem):
        self.name = name
        self.eng = eng
        self.sem = sem
        self.tick = 0
        self.pending = False
        self.seen = {}


class FW:
    NDMA = 32

    def __init__(self, nc, stack):
        self.nc = nc
        self.engs = {}
        for nm, e in (("pe", nc.tensor), ("dve", nc.vector), ("act", nc.scalar),
                      ("pool", nc.gpsimd), ("sp", nc.sync)):
            sem = stack.enter_context(nc.semaphore("s_" + nm))
            self.engs[nm] = _Eng(nm, e, sem)
        self.dma_sems = [stack.enter_context(nc.semaphore(f"s_dma{i}")) for i in range(self.NDMA)]
        self.dma_cnt = [0] * self.NDMA
        self.dma_next = 0
        self.n_inst = 0

    def _wait(self, E, prod, tick):
        if E.seen.get(prod, 0) >= tick:
            return
        if prod == E.name and tick > E.tick:
            return
        E.seen[prod] = tick
        if prod.startswith("dma"):
            sem = self.dma_sems[int(prod[3:])]
        else:
            sem = self.engs[prod].sem
        E.eng.wait_ge(sem, tick)

    def _deps(self, E, reads, writes, same):
        for b in reads:
            if b.w is not None and (same or b.w[0] != E.name):
                self._wait(E, *b.w)
            if b.excl:
                for p, t in b.r.items():
                    if p != E.name:
                        self._wait(E, p, t)
        for b in writes:
            if b.w is not None and (same or b.w[0] != E.name):
                self._wait(E, *b.w)
            for p, t in b.r.items():
                if p != E.name:
                    self._wait(E, p, t)

    @staticmethod
    def _mark(prod, tick, reads, writes):
        for b in reads:
            if b.r.get(prod, 0) < tick:
                b.r[prod] = tick
        for b in writes:
            b.w = (prod, tick)
            b.r = {}

    def op(self, engname, fn, reads=(), writes=(), same=True, inc=True):
        E = self.engs[engname]
        self._deps(E, reads, writes, same)
        ins = fn(E.eng)
        if inc:
            E.tick += 1
            E.pending = False
            ins.then_inc(E.sem, 1)
            self._mark(E.name, E.tick, reads, writes)
        else:
            E.pending = True
            self._mark(E.name, E.tick + 1, reads, writes)
        self.n_inst += 1
        return ins

    def dma(self, qname, out, in_, reads=(), writes=(), **kw):
        E = self.engs[qname]
        slot = self.dma_next
        self.dma_next = (self.dma_next + 1) % self.NDMA
        pname = f"dma{slot}"
        if self.dma_cnt[slot] > 0:
            self._wait(E, pname, 16 * self.dma_cnt[slot])
        self._deps(E, reads, writes, True)
        ins = E.eng.dma_start(out=out, in_=in_, **kw)
        self.dma_cnt[slot] += 1
        ins.then_inc(self.dma_sems[slot], 16)
        self._mark(pname, 16 * self.dma_cnt[slot], reads, writes)
        self.n_inst += 1
        return ins

    def _flush_pending(self):
        for nm, E in self.engs.items():
            if E.pending:
                E.tick += 1
                E.pending = False
                E.eng.nop().then_inc(E.sem, 1)

    def barrier(self):
        self._flush_pending()
        for nm, E in self.engs.items():
            for nm2, E2 in self.engs.items():
                if nm2 != nm and E2.tick:
                    self._wait(E, nm2, E2.tick)
            for s in range(self.NDMA):
                if self.dma_cnt[s]:
                    self._wait(E, f"dma{s}", 16 * self.dma_cnt[s])

    def finish(self):
        self._flush_pending()
        E = self.engs["sp"]
        for s in range(self.NDMA):
            if self.dma_cnt[s]:
                self._wait(E, f"dma{s}", 16 * self.dma_cnt[s])
        for nm, e in self.engs.items():
            if nm != "sp" and e.tick:
                self._wait(E, nm, e.tick)


def _rope_tables(head_dim):
    t = np.arange(SEQ)
    row = (t // GRID_W).astype(np.float32)
    col = (t % GRID_W).astype(np.float32)
    n = head_dim // 4
    inv = (10000.0 ** (-np.arange(n, dtype=np.float32) / n)).astype(np.float32)
    ang = np.concatenate([row[:, None] * inv, col[:, None] * inv], axis=-1).astype(np.float32)
    cos = np.cos(ang).astype(np.float32)
    sin = np.sin(ang).astype(np.float32)
    half = head_dim // 2
    C = np.ones((128, NT), np.float32)
    S = np.zeros((128, NT), np.float32)
    for p in range(128):
        dd = p % head_dim
        a = dd % half
        C[p, NCX:] = cos[:, a]
        S[p, NCX:] = -sin[:, a] if dd < half else sin[:, a]
    return C, S


def _hyena_tables(L):
    t = np.linspace(0.0, 1.0, L, dtype=np.float32)[:, None]
    bands = 16
    ang = (np.float32(2.0 * math.pi / L) * np.arange(L, dtype=np.float32)[:, None]
           * np.linspace(1e-4, bands - 1, bands, dtype=np.float32)).astype(np.float32)
    feats = np.concatenate([t, np.cos(ang), -np.sin(ang)], axis=-1).astype(np.float32)
    max_decay = math.log(1e-2) / 0.3
    min_decay = math.log(1e-2) / 1.5
    deltas = np.abs(np.linspace(min_decay, max_decay, 256, dtype=np.float32))
    decay = np.exp(-t * deltas).astype(np.float32)
    featsT = np.ascontiguousarray(feats.T)
    decT = np.ascontiguousarray(decay.T.reshape(2, 128, L).transpose(1, 0, 2))
    return featsT, np.ascontiguousarray(featsT[:, ::-1]), decT, np.ascontiguousarray(decT[:, :, ::-1])


def _pj(v, n=NJ):
    return np.ascontiguousarray(np.asarray(v, np.float32).reshape(n, 128).T)


_CONST_CACHE = {}


def _constants():
    if _CONST_CACHE:
        return _CONST_CACHE
    c = _CONST_CACHE
    c["cos_g"], c["sin_g"] = _rope_tables(64)
    c["cos_d"], c["sin_d"] = _rope_tables(32)
    c["featsT"], c["featsTr"], c["decT"], c["decTr"] = _hyena_tables(SEQ)
    c["featsTc"], c["featsTcr"], c["decTc"], c["decTcr"] = _hyena_tables(NCX)
    bf = ml_dtypes.bfloat16
    c["ident_bf"] = np.eye(128, dtype=np.float32).astype(bf)
    c["anti_bf"] = np.ascontiguousarray(np.eye(128, dtype=np.float32)[::-1]).astype(bf)
    c["ident_f"] = np.eye(128, dtype=np.float32)
    c["onesD_bf"] = np.full((128, 128), 1.0 / D, np.float32).astype(bf)
    bd = np.zeros((128, 128), np.float32)
    bd[:64, :64] = 1.0 / 64
    bd[64:, 64:] = 1.0 / 64
    c["bd64_bf"] = bd.astype(bf)
    c["ones_f"] = np.ones((128, 128), np.float32)
    sel = np.zeros((8, 8, 128), np.float32)
    for e in range(8):
        sel[e, e, :] = 1.0
    c["sel_bf"] = sel.astype(bf)
    m = np.zeros((128, 2), np.float32)
    for p in range(128):
        m[p, (p // 32) % 2] = 1.0
    c["dmask"] = m
    c["ones64_bf"] = np.ones((128, 64), np.float32).astype(bf)
    return c


def _layer_small(inputs, b):
    s = {}
    s["cvec"] = np.ascontiguousarray(np.stack([_pj(inputs["c"][b]), _pj(inputs["c_ctx"])], axis=-1))
    s["b_ada_l"] = np.ascontiguousarray(np.stack([_pj(inputs["b_ada"][i], 48) for i in range(DEPTH)]))
    s["g_mix_l"] = np.ascontiguousarray(np.stack([_pj(inputs["g_mix"][i]) for i in range(DEPTH)]))
    s["g_ffn_l"] = np.ascontiguousarray(np.stack([_pj(inputs["g_ffn"][i]) for i in range(DEPTH)]))
    s["g_final_l"] = _pj(inputs["g_final"])
    cw = np.asarray(inputs["hy_conv_w"], np.float32)
    s["hy_cw_l"] = np.ascontiguousarray(cw.reshape(DEPTH, 3, 6, 128).transpose(0, 3, 2, 1))
    s["hy_cb_l"] = np.ascontiguousarray(np.asarray(inputs["hy_conv_b"], np.float32).reshape(DEPTH, 6, 128).transpose(0, 2, 1))
    s["hy_b1_l"] = np.ascontiguousarray(np.asarray(inputs["hy_b1"], np.float32)[:, :, None])
    s["hy_b2_l"] = np.ascontiguousarray(np.asarray(inputs["hy_b2"], np.float32)[:, :, None])
    s["hy_fr_l"] = np.ascontiguousarray(np.asarray(inputs["hy_freq"], np.float32)[:, :, None])
    s["hy_skip_l"] = np.ascontiguousarray(np.asarray(inputs["hy_skip"], np.float32).reshape(DEPTH, 2, 128).transpose(0, 2, 1))
    s["diff_l"] = np.ascontiguousarray(np.stack([inputs["diff_lq1"], inputs["diff_lk1"], inputs["diff_lq2"],
                                                 inputs["diff_lk2"]], axis=1).astype(np.float32)[:, None])
    sub = np.asarray(inputs["diff_subln"], np.float32)
    s["subln_l"] = np.ascontiguousarray(np.stack([np.tile(sub[i], 2) for i in range(DEPTH)])[:, :, None])
    idx = np.arange(128) % 64
    idx_sw = (idx + 32) % 64
    for nm, key in (("gq_l", "gqa_qnorm"), ("gk_l", "gqa_knorm")):
        g = np.asarray(inputs[key], np.float32)
        s[nm] = np.ascontiguousarray(np.stack([np.stack([g[i][idx], g[i][idx_sw]], axis=-1) for i in range(DEPTH)]))
    return s


BIG_KEYS = ["w_ada", "w_in", "w_out", "hy_w1", "hy_w2", "hy_w3", "ffn_wg", "ffn_wu", "ffn_wd",
            "moe_router", "moe_wg", "moe_wu", "moe_wd"]


def build_program(depth=DEPTH, stop=None, debug=False):
    nc = bass.Bass("TRN2", target_bir_lowering=False)
    consts = _constants()
    np2dt = {np.dtype(np.float32): F32, np.dtype(ml_dtypes.bfloat16): BF16}

    def din(name, shape, dt=F32):
        return nc.dram_tensor(name, list(shape), dt, kind="ExternalInput").ap()

    shapes = {
        "x": [SEQ, D], "ctx": [NCX, D],
        "cvec": [128, 8, 2], "b_ada_l": [DEPTH, 128, 48], "g_mix_l": [DEPTH, 128, 8], "g_ffn_l": [DEPTH, 128, 8],
        "g_final_l": [128, 8], "hy_cw_l": [DEPTH, 128, 6, 3], "hy_cb_l": [DEPTH, 128, 6], "hy_b1_l": [DEPTH, 64, 1],
        "hy_b2_l": [DEPTH, 64, 1], "hy_fr_l": [DEPTH, 64, 1], "hy_skip_l": [DEPTH, 128, 2], "diff_l": [DEPTH, 1, 4, 32],
        "subln_l": [DEPTH, 128, 1], "gq_l": [DEPTH, 128, 2], "gk_l": [DEPTH, 128, 2],
        "w_ada": [DEPTH, D, 6 * D], "w_in": [DEPTH, D, IN_COLS], "w_out": [DEPTH, D, D],
        "hy_w1": [DEPTH, 33, 64], "hy_w2": [DEPTH, 64, 64], "hy_w3": [DEPTH, 64, 512],
        "ffn_wg": [2, D, DFF], "ffn_wu": [2, D, DFF], "ffn_wd": [2, DFF, D],
        "moe_router": [2, D, NEXP], "moe_wg": [2, NEXP, D, DFE], "moe_wu": [2, NEXP, D, DFE], "moe_wd": [2, NEXP, DFE, D],
    }

    class _Inputs(dict):
        def __missing__(self, key):
            if key in shapes:
                ap = din(key, shapes[key])
            else:
                v = consts[key]
                ap = din("k_" + key, v.shape, np2dt[v.dtype])
            self[key] = ap
            return ap

    I = _Inputs()
    out = nc.dram_tensor("out", [SEQ, D], F32, kind="ExternalOutput").ap()

    skind = "ExternalOutput" if debug else "Internal"

    def dscr(name, shape, dt):
        return nc.dram_tensor(name, list(shape), dt, kind=skind).ap()

    S = {}
    S["hT"] = dscr("s_hT", [128, NJ, NT], F32)
    S["zhy"] = dscr("s_zhy", [128, 6, NT], BF16)
    S["qd"] = dscr("s_qd", [128, 2, NT], BF16)
    S["qg"] = dscr("s_qg", [128, 4, NT], BF16)
    S["mg"] = dscr("s_mg", [128, NJ, NT], BF16)
    S["u2"] = dscr("s_u2", [128, NJ, NT], BF16)
    for k_ in range(2):
        S[f"hrev{k_}"] = dscr(f"s_hrev{k_}", [256, 2 * SEQ], BF16)
        S[f"hrevc{k_}"] = dscr(f"s_hrevc{k_}", [256, 2 * NCX], BF16)
    if debug:
        S["dbg_kv"] = dscr("s_dbgkv", [128, 12, NT], F32)

    B = {k: Buf(k) for k in list(S.keys())}
    Bin = Buf("inputs")
    Bout = Buf("out")

    with ExitStack() as st:
        fw = FW(nc, st)

        _uid = [0]

        def sb(name, shape, dt, stack=st):
            _uid[0] += 1
            return stack.enter_context(nc.sbuf_tensor(f"sb{_uid[0]}_{name}", list(shape), dt))

        ps_all = st.enter_context(nc.psum_tensor("ps_all", [128, 8, 512], F32))
        PB = [Buf(f"bank{k}", excl=True) for k in range(8)]

        def bank(k):
            return ps_all[:, k, :]

        def load_const(name, key, dt, q="sp"):
            a = consts[key]
            t = sb(name, a.shape, dt)
            b = Buf(name)
            fw.dma(q, t[:], I[key], reads=[Bin], writes=[b])
            return t, b

        ident_bf, Bident = load_const("ident_bf", "ident_bf", BF16)
        anti_bf, Banti = load_const("anti_bf", "anti_bf", BF16)
        ident_f, Bidentf = load_const("ident_f", "ident_f", F32)
        onesD, BonesD = load_const("onesD", "onesD_bf", BF16)
        bd64, Bbd64 = load_const("bd64", "bd64_bf", BF16)
        ones_f, Bonesf = load_const("ones_f", "ones_f", F32)
        sel_bf, Bsel = load_const("sel_bf", "sel_bf", BF16)
        dmask, Bdmask = load_const("dmask", "dmask", F32)
        ones64, Bones64 = load_const("ones64", "ones64_bf", BF16)
        cvec = sb("cvec", [128, 8, 2], F32)
        Bcvec = Buf("cvec")
        fw.dma("sp", cvec[:], I["cvec"], reads=[Bin], writes=[Bcvec])
        scb = sb("scb", [128, 8, 2], BF16)
        Bscb = Buf("scb")
        fw.op("act", lambda e: e.activation(out=scb[:], in_=cvec[:], func=AF.Silu), reads=[Bcvec], writes=[Bscb])
        gfin = sb("gfin", [128, 8], F32)
        Bgfin = Buf("gfin")
        fw.dma("sp", gfin[:], I["g_final_l"], reads=[Bin], writes=[Bgfin])

        if stop == "consts":
            fw.finish()
            return nc, list(I.keys())
        TBLK = [(0, NCX, 1)] + [(NCX + 512 * i, 512, 0) for i in range(SEQ // 512)]

        with ExitStack() as ph:
            xt = [sb(f"xt{i}", [128, D], F32, ph) for i in range(2)]
            Bxt = [Buf() for _ in range(2)]
            hst = [sb(f"hst{i}", [128, NJ, 512], F32, ph) for i in range(2)]
            Bhst = [Buf() for _ in range(2)]
            k = 0
            import os
            for bi, (t0, n, seg) in enumerate(TBLK[:int(os.environ.get('DEV_NBLK', '99'))]):
                hs, Bh = hst[bi % 2], Bhst[bi % 2]
                for tt in range(n // 128):
                    src = I["ctx"][tt * 128:(tt + 1) * 128, :] if seg else I["x"][t0 - NCX + tt * 128: t0 - NCX + (tt + 1) * 128, :]
                    x_, Bx_ = xt[k % 2], Bxt[k % 2]
                    fw.dma("sp", x_[:], src, reads=[Bin], writes=[Bx_])
                    for half in range(2):
                        pbk = 2 * (k % 2) + half
                        for jj in range(4):
                            j = half * 4 + jj
                            fw.op("pe", lambda e: e.matmul(ps_all[:, pbk, jj * 128:(jj + 1) * 128], lhsT=x_[:, j * 128:(j + 1) * 128], rhs=ident_f[:], start=True, stop=True),
                                  reads=[Bx_, Bidentf], writes=[PB[pbk]], inc=(jj == 3))
                        eng = "dve" if half == 0 else "act"
                        src_ps = ps_all[:, pbk, :].rearrange("p (j t) -> p j t", j=4)
                        dst = hs[:, half * 4:(half + 1) * 4, tt * 128:(tt + 1) * 128]
                        if eng == "dve":
                            fw.op("dve", lambda e: e.tensor_copy(out=dst, in_=src_ps), reads=[PB[pbk]], writes=[Bh])
                        else:
                            fw.op("act", lambda e: e.copy(out=dst, in_=src_ps), reads=[PB[pbk]], writes=[Bh])
                    k += 1
                fw.dma("sp", S["hT"][:, :, t0:t0 + n], hs[:, :, :n], reads=[Bh], writes=[B["hT"]])
            fw.barrier()

        if stop == "init":
            fw.finish()
            return nc, list(I.keys())

        def mm_group(out_ap, pairs, reads, wbuf, skip=False):
            n = len(pairs)
            for k, (l, r) in enumerate(pairs):
                fw.op("pe", lambda e: e.matmul(out_ap, lhsT=l, rhs=r, start=(k == 0), stop=(k == n - 1)),
                      reads=reads, writes=[wbuf], same=False, inc=(k == n - 1))

        def rstd_from_ps(ps_ap, rt_ap, Brt, PBk):
            fw.op("act", lambda e: e.activation(out=rt_ap, in_=ps_ap, func=AF.Sqrt, bias=EPS, scale=1.0), reads=[PBk], writes=[Brt])
            fw.op("dve", lambda e: e.reciprocal(out=rt_ap, in_=rt_ap), reads=[Brt], writes=[Brt])

        modsb = sb("modsb", [128, 48, 2], F32)
        Bmod = Buf("mod")
        prm = sb("prm", [128, 6, 8, 2], F32)
        Bprm = Buf("prm")
        gmix = sb("gmix", [128, 8], F32)
        gffn = sb("gffn", [128, 8], F32)
        bada = sb("bada", [128, 48], F32)
        Bsmall = Buf("small")
        gqk = sb("gqk", [128, 4], F32)
        lamt = sb("lamt", [128, 4], F32)
        Blam = Buf("lam")
        dl = sb("dl", [1, 4, 32], F32)
        dl2 = sb("dl2", [1, 8], F32)

        def layer_setup(i, ph):
            fw.dma("sp", gmix[:], I["g_mix_l"][i], reads=[Bin], writes=[Bsmall])
            fw.dma("sp", gffn[:], I["g_ffn_l"][i], reads=[Bin], writes=[Bsmall])
            fw.dma("sp", bada[:], I["b_ada_l"][i], reads=[Bin], writes=[Bsmall])
            fw.dma("sp", gqk[:, 0:2], I["gq_l"][i], reads=[Bin], writes=[Bsmall])
            fw.dma("sp", gqk[:, 2:4], I["gk_l"][i], reads=[Bin], writes=[Bsmall])
            fw.dma("sp", dl[:], I["diff_l"][i], reads=[Bin], writes=[Bsmall])
            fw.dma("sp", lamt[:, 1:2], I["subln_l"][i], reads=[Bin], writes=[Blam])
            wa = [sb(f"wada{k}", [128, 8, 1024], BF16, ph) for k in range(2)]
            Bwa = [Buf() for _ in range(2)]
            wsrc = I["w_ada"][i].rearrange("(j p) n -> p j n", p=128)
            for sidx in range(6):
                w_, Bw_ = wa[sidx % 2], Bwa[sidx % 2]
                fw.dma("pool", w_[:], wsrc[:, :, sidx * 1024:(sidx + 1) * 1024], reads=[Bin], writes=[Bw_])
                for jj in range(8):
                    col = (sidx * 8 + jj) * 2
                    mm_group(ps_all[:, 0, col:col + 2], [(w_[:, j, jj * 128:(jj + 1) * 128], scb[:, j, :]) for j in range(8)],
                             [Bw_, Bscb], PB[0])
            for n_ in range(2):
                fw.op("dve", lambda e: e.tensor_tensor(out=modsb[:, :, n_], in0=ps_all[:, 0, 0:96].rearrange("p (a b) -> p a b", b=2)[:, :, n_],
                                                       in1=bada[:], op=ALU.add), reads=[PB[0], Bsmall], writes=[Bmod])
            for (dst, g_, sc0, sh0, gt0) in ((0, gmix, 8, 0, 16), (3, gffn, 32, 24, 40)):
                for n_ in range(2):
                    fw.op("dve", lambda e: e.scalar_tensor_tensor(out=prm[:, dst, :, n_], in0=modsb[:, sc0:sc0 + 8, n_], scalar=1.0, in1=g_[:],
                                                                  op0=ALU.add, op1=ALU.mult), reads=[Bmod, Bsmall], writes=[Bprm])
                    fw.op("dve", lambda e: e.tensor_copy(out=prm[:, dst + 1, :, n_], in_=modsb[:, sh0:sh0 + 8, n_]), reads=[Bmod], writes=[Bprm])
                    fw.op("dve", lambda e: e.tensor_copy(out=prm[:, dst + 2, :, n_], in_=modsb[:, gt0:gt0 + 8, n_]), reads=[Bmod], writes=[Bprm])
            lam_init = 0.8 - 0.6 * math.exp(-0.3 * i)
            fw.op("dve", lambda e: e.tensor_tensor(out=dl[:, 0, :], in0=dl[:, 0, :], in1=dl[:, 1, :], op=ALU.mult), reads=[Bsmall], writes=[Bsmall])
            fw.op("dve", lambda e: e.tensor_tensor(out=dl[:, 2, :], in0=dl[:, 2, :], in1=dl[:, 3, :], op=ALU.mult), reads=[Bsmall], writes=[Bsmall])
            fw.op("dve", lambda e: e.reduce_sum(out=dl2[:, 0:1], in_=dl[:, 0, :], axis=AX.X), reads=[Bsmall], writes=[Bsmall])
            fw.op("dve", lambda e: e.reduce_sum(out=dl2[:, 1:2], in_=dl[:, 2, :], axis=AX.X), reads=[Bsmall], writes=[Bsmall])
            fw.op("act", lambda e: e.activation(out=dl2[:, 2:4], in_=dl2[:, 0:2], func=AF.Exp), reads=[Bsmall], writes=[Bsmall])
            fw.op("dve", lambda e: e.tensor_tensor(out=dl2[:, 4:5], in0=dl2[:, 3:4], in1=dl2[:, 2:3], op=ALU.subtract), reads=[Bsmall], writes=[Bsmall])
            fw.op("dve", lambda e: e.tensor_scalar(out=dl2[:, 4:5], in0=dl2[:, 4:5], scalar1=-lam_init, scalar2=None, op0=ALU.add), reads=[Bsmall], writes=[Bsmall])
            fw.op("pe", lambda e: e.matmul(ps_all[:, 1, 0:1], lhsT=ones_f[0:1, :], rhs=dl2[:, 4:5], start=True, stop=True),
                  reads=[Bsmall, Bonesf], writes=[PB[1]])
            fw.op("dve", lambda e: e.tensor_copy(out=lamt[:, 0:1], in_=ps_all[:, 1, 0:1]), reads=[PB[1]], writes=[Blam])
            fw.op("dve", lambda e: e.tensor_scalar(out=lamt[:, 1:2], in0=lamt[:, 1:2], scalar1=(1.0 - lam_init), scalar2=None, op0=ALU.mult), reads=[Blam], writes=[Blam])

        def norm_block(hblk, Bh, n, seg, a_idx, sq, Bsq, rt, Brt, tmp, Btmp, outs):
            fw.op("act", lambda e: e.activation(out=sq[:, :, :n], in_=hblk[:, :, :n], func=AF.Square), reads=[Bh], writes=[Bsq])
            mm_group(ps_all[:, 0, :n], [(onesD[:], sq[:, j, :n]) for j in range(8)], [BonesD, Bsq], PB[0])
            rstd_from_ps(ps_all[:, 0, :n], rt[:, :n], Brt, PB[0])
            for j in range(8):
                fw.op("dve", lambda e: e.tensor_tensor(out=tmp[:, :n], in0=hblk[:, j, :n], in1=rt[:, :n], op=ALU.mult), reads=[Bh, Brt], writes=[Btmp])
                for (ot, Bo_) in outs:
                    fw.op("dve", lambda e: e.tensor_scalar(out=ot[:, j, :n], in0=tmp[:, :n], scalar1=prm[:, a_idx, j, seg:seg + 1],
                                                           scalar2=prm[:, a_idx + 1, j, seg:seg + 1], op0=ALU.mult, op1=ALU.add),
                          reads=[Btmp, Bprm], writes=[Bo_])

        for li in range(depth):
            moe = (li % 2 == 1)
            last = (li == DEPTH - 1)
            with ExitStack() as kv:
                kd1 = sb("kd1", [128, 2, NT], BF16, kv)
                kd2 = sb("kd2", [128, 2, NT], BF16, kv)
                kgA = sb("kgA", [128, NT], BF16, kv)
                kgB = sb("kgB", [128, NT], BF16, kv)
                vd = sb("vd", [128, NT // 128, 4, 65], BF16, kv)
                vg = sb("vg", [128, NT // 128, 2, 65], BF16, kv)
                Bkd1, Bkd2, BkgA, BkgB, Bvd, Bvg = (Buf() for _ in range(6))
                fw.op("pool", lambda e: e.memset(vd[:, :, :, 64:65], 1.0), writes=[Bvd])
                fw.op("pool", lambda e: e.memset(vg[:, :, :, 64:65], 1.0), writes=[Bvg])
                with ExitStack() as ph:
                    layer_setup(li, ph)
                    fw.barrier()
                if stop == "setup":
                    if debug:
                        fw.dma("sp", S["dbg_kv"][:, 0, 0:96], modsb[:].rearrange("p a b -> p (a b)"), reads=[Bmod], writes=[Bout])
                        fw.dma("sp", S["dbg_kv"][:, 1, 0:96], prm[:].rearrange("p a b c -> p (a b c)"), reads=[Bprm], writes=[Bout])
                        fw.dma("sp", S["dbg_kv"][:, 2, 0:4], lamt[:], reads=[Blam], writes=[Bout])
                    fw.finish()
                    return nc, list(I.keys())

                with ExitStack() as ph:
                    win = sb("win", [128, 8, IN_COLS], BF16, ph)
                    Bwin = Buf("win")
                    wsrc = I["w_in"][li].rearrange("(j p) n -> p j n", p=128)
                    for c0 in range(0, IN_COLS, 768):
                        fw.dma("pool", win[:, :, c0:c0 + 768], wsrc[:, :, c0:c0 + 768], reads=[Bin], writes=[Bwin])
                    wx = sb("wx", [128, 8, 11 * 128], BF16, ph)
                    Bwx = Buf("wx")

                    def perm_copy(dst_c, src_col, hd, swap_heads=False, swap_halves=True):
                        half = hd // 2
                        ng = 128 // hd
                        for g in range(ng):
                            gs = (ng - 1 - g) if swap_heads else g
                            for h in range(2):
                                hs_ = (1 - h) if swap_halves else h
                                d0 = dst_c * 128 + g * hd + h * half
                                s0 = src_col + gs * hd + hs_ * half
                                fw.op("pool", lambda e: e.tensor_copy(out=wx[:, :, d0:d0 + half], in_=win[:, :, s0:s0 + half]),
                                      reads=[Bwin], writes=[Bwx])
                    for c in range(2):
                        perm_copy(0 + c, OFF_DQ + 128 * c, 32)
                        perm_copy(2 + c, OFF_DK + 128 * c, 32)
                    for c in range(4):
                        perm_copy(4 + c, OFF_GQ + 128 * c, 64)
                    perm_copy(8, OFF_GK, 64)
                    perm_copy(9, OFF_GK, 64, swap_heads=True, swap_halves=False)
                    perm_copy(10, OFF_GK, 64, swap_heads=True, swap_halves=True)

                    hblk = sb("hblk", [128, 8, 512], F32, ph)
                    Bhblk = Buf()
                    sq = sb("sq", [128, 8, 512], BF16, ph)
                    Bsq = Buf()
                    ub = sb("ub", [128, 8, 512], BF16, ph)
                    Bub = Buf()
                    rt = sb("rt", [128, 512], F32, ph)
                    Brt = Buf()
                    tmp = sb("tmp", [128, 512], F32, ph)
                    Btmp = Buf()
                    tabs = sb("tabs", [128, 4, 512], F32, ph)
                    Btabs = Buf()
                    t1 = sb("t1", [128, 512], F32, ph)
                    t2 = sb("t2", [128, 512], F32, ph)
                    t3 = sb("t3", [128, 512], F32, ph)
                    Bt1, Bt2, Bt3 = Buf(), Buf(), Buf()
                    rt2 = sb("rt2", [128, 512], F32, ph)
                    Brt2 = Buf()
                    sq2 = sb("sq2", [128, 512], BF16, ph)
                    Bsq2 = Buf()
                    zst = sb("zst", [128, 6, 512], BF16, ph)
                    Bzst = Buf()
                    qst = sb("qst", [128, 6, 512], BF16, ph)
                    Bqst = Buf()

                    def proj(pbk, wtile_fn, n):
                        mm_group(ps_all[:, pbk, :n], [(wtile_fn(j), ub[:, j, :n]) for j in range(8)], [Bwin, Bwx, Bub], PB[pbk])

                    import os
                    PARTS = os.environ.get('DEV_PARTS', 'nhdgv')
                    for (t0, n, seg) in TBLK[:int(os.environ.get('DEV_NBLK', '99'))]:
                        fw.dma("sp", hblk[:, :, :n], S["hT"][:, :, t0:t0 + n], reads=[B["hT"]], writes=[Bhblk])
                        for ti, key in enumerate(("cos_g", "sin_g", "cos_d", "sin_d")):
                            fw.dma("sp", tabs[:, ti, :n], I[key][:, t0:t0 + n], reads=[Bin], writes=[Btabs])
                        norm_block(hblk, Bhblk, n, seg, 0, sq, Bsq, rt, Brt, tmp, Btmp, [(ub, Bub)])
                        for c in range(6 if 'h' in PARTS else 0):
                            pbk = 2 + (c % 2)
                            proj(pbk, lambda j: win[:, j, c * 128:(c + 1) * 128], n)
                            if c % 2 == 0:
                                fw.op("act", lambda e: e.copy(out=zst[:, c, :n], in_=ps_all[:, pbk, :n]), reads=[PB[pbk]], writes=[Bzst])
                            else:
                                fw.op("dve", lambda e: e.tensor_copy(out=zst[:, c, :n], in_=ps_all[:, pbk, :n]), reads=[PB[pbk]], writes=[Bzst])
                        fw.dma("sp", S["zhy"][:, :, t0:t0 + n], zst[:, :, :n], reads=[Bzst], writes=[B["zhy"]])
                        for c in range(4 if 'd' in PARTS else 0):
                            isk = c >= 2
                            col = (OFF_DK if isk else OFF_DQ) + 128 * (c % 2)
                            xc = (2 if isk else 0) + (c % 2)
                            proj(4, lambda j: win[:, j, col:col + 128], n)
                            proj(5, lambda j: wx[:, j, xc * 128:(xc + 1) * 128], n)
                            fw.op("dve", lambda e: e.tensor_tensor(out=t1[:, :n], in0=ps_all[:, 4, :n], in1=tabs[:, 2, :n], op=ALU.mult), reads=[PB[4], Btabs], writes=[Bt1])
                            fw.op("dve", lambda e: e.tensor_tensor(out=t2[:, :n], in0=ps_all[:, 5, :n], in1=tabs[:, 3, :n], op=ALU.mult), reads=[PB[5], Btabs], writes=[Bt2])
                            if not isk:
                                fw.op("dve", lambda e: e.tensor_tensor(out=qst[:, c, :n], in0=t1[:, :n], in1=t2[:, :n], op=ALU.add), reads=[Bt1, Bt2], writes=[Bqst])
                            else:
                                fw.op("dve", lambda e: e.tensor_tensor(out=t3[:, :n], in0=t1[:, :n], in1=t2[:, :n], op=ALU.add), reads=[Bt1, Bt2], writes=[Bt3])
                                fw.op("dve", lambda e: e.tensor_scalar(out=kd1[:, c - 2, t0:t0 + n], in0=t3[:, :n], scalar1=dmask[:, 0:1], scalar2=None, op0=ALU.mult),
                                      reads=[Bt3, Bdmask], writes=[Bkd1])
                                fw.op("dve", lambda e: e.tensor_scalar(out=kd2[:, c - 2, t0:t0 + n], in0=t3[:, :n], scalar1=dmask[:, 1:2], scalar2=None, op0=ALU.mult),
                                      reads=[Bt3, Bdmask], writes=[Bkd2])
                        for c in range(6 if 'g' in PARTS else 0):
                            if c < 4:
                                wa_ = lambda j: win[:, j, OFF_GQ + c * 128: OFF_GQ + (c + 1) * 128]
                                wb_ = lambda j: wx[:, j, (4 + c) * 128:(5 + c) * 128]
                                g0 = 0
                            elif c == 4:
                                wa_ = lambda j: win[:, j, OFF_GK:OFF_GK + 128]
                                wb_ = lambda j: wx[:, j, 8 * 128:9 * 128]
                                g0 = 2
                            else:
                                wa_ = lambda j: wx[:, j, 9 * 128:10 * 128]
                                wb_ = lambda j: wx[:, j, 10 * 128:11 * 128]
                                g0 = 2
                            proj(4, wa_, n)
                            proj(5, wb_, n)
                            fw.op("act", lambda e: e.activation(out=sq2[:, :n], in_=ps_all[:, 4, :n], func=AF.Square), reads=[PB[4]], writes=[Bsq2])
                            fw.op("pe", lambda e: e.matmul(ps_all[:, 6, :n], lhsT=bd64[:], rhs=sq2[:, :n], start=True, stop=True), reads=[Bbd64, Bsq2], writes=[PB[6]])
                            rstd_from_ps(ps_all[:, 6, :n], rt2[:, :n], Brt2, PB[6])
                            fw.op("act", lambda e: e.activation(out=t1[:, :n], in_=ps_all[:, 4, :n], func=AF.Copy, scale=gqk[:, g0:g0 + 1]), reads=[PB[4], Bsmall], writes=[Bt1])
                            fw.op("act", lambda e: e.activation(out=t2[:, :n], in_=ps_all[:, 5, :n], func=AF.Copy, scale=gqk[:, g0 + 1:g0 + 2]), reads=[PB[5], Bsmall], writes=[Bt2])
                            fw.op("dve", lambda e: e.tensor_tensor(out=t1[:, :n], in0=t1[:, :n], in1=tabs[:, 0, :n], op=ALU.mult), reads=[Bt1, Btabs], writes=[Bt1])
                            fw.op("dve", lambda e: e.tensor_tensor(out=t2[:, :n], in0=t2[:, :n], in1=tabs[:, 1, :n], op=ALU.mult), reads=[Bt2, Btabs], writes=[Bt2])
                            fw.op("dve", lambda e: e.tensor_tensor(out=t3[:, :n], in0=t1[:, :n], in1=t2[:, :n], op=ALU.add), reads=[Bt1, Bt2], writes=[Bt3])
                            if c < 4:
                                dst, Bd = qst[:, 2 + c, :n], Bqst
                            elif c == 4:
                                dst, Bd = kgA[:, t0:t0 + n], BkgA
                            else:
                                dst, Bd = kgB[:, t0:t0 + n], BkgB
                            fw.op("dve", lambda e: e.tensor_tensor(out=dst, in0=t3[:, :n], in1=rt2[:, :n], op=ALU.mult), reads=[Bt3, Brt2], writes=[Bd])
                        fw.dma("sp", S["qd"][:, :, t0:t0 + n], qst[:, 0:2, :n], reads=[Bqst], writes=[B["qd"]])
                        fw.dma("sp", S["qg"][:, :, t0:t0 + n], qst[:, 2:6, :n], reads=[Bqst], writes=[B["qg"]])
                        for tt in range(n // 128 if 'v' in PARTS else 0):
                            kt = t0 // 128 + tt
                            pbk = 2 + (tt % 2)
                            if 'x' not in PARTS:
                                mm_group(ps_all[:, pbk, 0:256], [(ub[:, j, tt * 128:(tt + 1) * 128], win[:, j, OFF_DV:OFF_DV + 256]) for j in range(8)], [Bub, Bwin], PB[pbk])
                            if 'y' not in PARTS:
                                mm_group(ps_all[:, pbk, 256:384], [(ub[:, j, tt * 128:(tt + 1) * 128], win[:, j, OFF_GV:OFF_GV + 128]) for j in range(8)], [Bub, Bwin], PB[pbk])
                            if 'x' not in PARTS and 'z' not in PARTS:
                                fw.op("act", lambda e: e.copy(out=vd[:, kt, :, 0:64], in_=ps_all[:, pbk, 0:256].rearrange("p (h d) -> p h d", h=4)), reads=[PB[pbk]], writes=[Bvd])
                            if 'y' not in PARTS and 'z' not in PARTS:
                                fw.op("dve", lambda e: e.tensor_copy(out=vg[:, kt, :, 0:64], in_=ps_all[:, pbk, 256:384].rearrange("p (h d) -> p h d", h=2)), reads=[PB[pbk]], writes=[Bvg])
                    fw.barrier()
                if stop == "p1":
                    if debug:
                        with ExitStack() as ph:
                            dbg = sb("dbg", [128, 8840], F32, ph)
                            Bdbg = Buf()
                            for k_, (src_, Bs_) in enumerate(((kd1[:, 0, :], Bkd1), (kd1[:, 1, :], Bkd1), (kd2[:, 0, :], Bkd2), (kd2[:, 1, :], Bkd2), (kgA[:], BkgA), (kgB[:], BkgB))):
                                fw.op("dve", lambda e: e.tensor_copy(out=dbg[:, 0:NT], in_=src_), reads=[Bs_], writes=[Bdbg])
                                fw.dma("sp", S["dbg_kv"][:, k_, :], dbg[:, 0:NT], reads=[Bdbg], writes=[Bout])
                            fw.op("dve", lambda e: e.tensor_copy(out=dbg[:, 0:34 * 4 * 65], in_=vd[:].rearrange("p a b c -> p (a b c)")), reads=[Bvd], writes=[Bdbg])
                            fw.dma("sp", S["dbg_kv"][:, 6:9, :].rearrange("p a b -> p (a b)")[:, 0:8840], dbg[:, 0:8840], reads=[Bdbg], writes=[Bout])
                            fw.op("dve", lambda e: e.tensor_copy(out=dbg[:, 0:34 * 2 * 65], in_=vg[:].rearrange("p a b c -> p (a b c)")), reads=[Bvg], writes=[Bdbg])
                            fw.dma("sp", S["dbg_kv"][:, 9:11, :].rearrange("p a b -> p (a b)")[:, 0:4420], dbg[:, 0:4420], reads=[Bdbg], writes=[Bout])
                    fw.finish()
                    return nc, list(I.keys())

                with ExitStack() as ph:
                    qdb = [sb(f"qdb{k}", [128, 2, 512], BF16, ph) for k in range(2)]
                    qgb = [sb(f"qgb{k}", [128, 4, 512], BF16, ph) for k in range(2)]
                    Bqb = [Buf() for _ in range(2)]
                    pT = [sb(f"pT{k}", [128, 2, 512], BF16, ph) for k in range(3)]
                    BpT = [Buf() for _ in range(3)]
                    rec = [sb(f"rec{k}", [128, 512], F32, ph) for k in range(2)]
                    Brec = [Buf() for _ in range(2)]
                    tm = [sb(f"tm{k}", [128, 512], F32, ph) for k in range(2)]
                    Btm = [Buf() for _ in range(2)]
                    od = sb("od", [128, 512], F32, ph)
                    Bod = Buf()
                    sqd = sb("sqd", [128, 512], BF16, ph)
                    Bsqd = Buf()
                    rtd = sb("rtd", [128, 512], F32, ph)
                    Brtd = Buf()
                    of = [sb(f"of{k}", [128, 512], BF16, ph) for k in range(2)]
                    Bof = [Buf() for _ in range(2)]
                    cnt = {"pt": 0, "of": 0, "sp": 0, "pair": 0, "rec": 0}
                    deferred = []

                    def run_deferred(step=None):
                        while deferred and (step is None or deferred[0][0] <= step):
                            deferred.pop(0)[1]()

                    def attn_pair(kA_fn, kB_fn, q_t, ch, vA_fn, vB_fn, tiles, nq, scale):
                        pr = cnt["pair"] % 2
                        cnt["pair"] += 1
                        ob, db = (6, 7) if pr == 0 else (0, 1)
                        slots = []

                        def qk(i):
                            kt = tiles[i]
                            s_ = cnt["sp"] % 2
                            cnt["sp"] += 1
                            slots.append(s_)
                            fw.op("pe", lambda e: e.matmul(ps_all[:, 2 + 2 * s_, :nq], lhsT=kA_fn(kt), rhs=q_t[0:64, ch, :nq], start=True, stop=True),
                                  reads=[Bkd1, Bkd2, BkgA, BkgB] + Bqb, writes=[PB[2 + 2 * s_]], same=False, inc=False)
                            fw.op("pe", lambda e: e.matmul(ps_all[:, 3 + 2 * s_, :nq], lhsT=kB_fn(kt), rhs=q_t[64:128, ch, :nq], start=True, stop=True),
                                  reads=[Bkd1, Bkd2, BkgA, BkgB] + Bqb, writes=[PB[3 + 2 * s_]], same=False, inc=True)
                        qk(0)
                        nk = len(tiles)
                        for i, kt in enumerate(tiles):
                            if i + 1 < nk:
                                qk(i + 1)
                            s_ = slots[i]
                            pt_i = cnt["pt"] % 3
                            cnt["pt"] += 1
                            fw.op("act", lambda e: e.activation(out=pT[pt_i][:, :, :nq], in_=ps_all[:, 2 + 2 * s_:4 + 2 * s_, :nq], func=AF.Exp, scale=scale),
                                  reads=[PB[2 + 2 * s_], PB[3 + 2 * s_]], writes=[BpT[pt_i]])
                            first, last = (i == 0), (i == nk - 1)
                            for (bk_, lA, lB, Bl) in ((ob, vA_fn(kt), vB_fn(kt), [Bvd, Bvg]), (db, ones64[:, :], ones64[:, :], [Bones64])):
                                fw.op("pe", lambda e: e.matmul(ps_all[0:64, bk_, :nq], lhsT=lA, rhs=pT[pt_i][:, 0, :nq], start=first, stop=last,
                                                               skip_group_check=True, tile_position=(0, 0)),
                                      reads=Bl + [BpT[pt_i]], writes=[PB[bk_]], same=False, inc=False)
                                fw.op("pe", lambda e: e.matmul(ps_all[64:128, bk_, :nq], lhsT=lB, rhs=pT[pt_i][:, 1, :nq], start=first, stop=last,
                                                               skip_group_check=True, tile_position=(0, 64)),
                                      reads=Bl + [BpT[pt_i]], writes=[PB[bk_]], same=False, inc=(bk_ == db))
                            run_deferred(i)
                        run_deferred()
                        return ob, db

                    def recip_den(db, nq):
                        r_i = cnt["rec"] % 2
                        cnt["rec"] += 1
                        fw.op("dve", lambda e: e.reciprocal(out=rec[r_i][:, :nq], in_=ps_all[:, db, :nq]), reads=[PB[db]], writes=[Brec[r_i]])
                        return r_i

                    QBLK = [(0, NCX, [0, 1])] + [(NCX + 512 * i, 512, list(range(NT // 128))) for i in range(SEQ // 512)]
                    import os
                    QBLK = QBLK[:int(os.environ.get('DEV_NQB', '99'))]
                    if last:
                        QBLK = QBLK[1:]

                    def load_q(qi):
                        q0, nq, _ = QBLK[qi]
                        fw.dma("sp", qdb[qi % 2][:, :, :nq], S["qd"][:, :, q0:q0 + nq], reads=[B["qd"]], writes=[Bqb[qi % 2]])
                        fw.dma("sp", qgb[qi % 2][:, :, :nq], S["qg"][:, :, q0:q0 + nq], reads=[B["qg"]], writes=[Bqb[qi % 2]])
                    load_q(0)
                    for qi, (q0, nq, tiles) in enumerate(QBLK):
                        if qi + 1 < len(QBLK):
                            load_q(qi + 1)
                        qd_, qg_ = qdb[qi % 2], qgb[qi % 2]
                        for ch in range(2):
                            for m_ in range(2):
                                kk = kd1 if m_ == 0 else kd2
                                ob, db = attn_pair(lambda kt: kk[0:64, ch, kt * 128:(kt + 1) * 128], lambda kt: kk[64:128, ch, kt * 128:(kt + 1) * 128],
                                                   qd_, ch, lambda kt: vd[:, kt, 2 * ch, 0:64], lambda kt: vd[:, kt, 2 * ch + 1, 0:64], tiles, nq, 32 ** -0.5)
                                r_i = recip_den(db, nq)
                                if m_ == 1:
                                    fw.op("dve", lambda e: e.tensor_scalar(out=rec[r_i][:, :nq], in0=rec[r_i][:, :nq], scalar1=lamt[:, 0:1], scalar2=None, op0=ALU.mult),
                                          reads=[Brec[r_i], Blam], writes=[Brec[r_i]])
                                fw.op("dve", lambda e: e.tensor_tensor(out=tm[m_][:, :nq], in0=ps_all[:, ob, :nq], in1=rec[r_i][:, :nq], op=ALU.mult),
                                      reads=[PB[ob], Brec[r_i]], writes=[Btm[m_]])
                            fw.op("dve", lambda e: e.tensor_tensor(out=od[:, :nq], in0=tm[0][:, :nq], in1=tm[1][:, :nq], op=ALU.add), reads=[Btm[0], Btm[1]], writes=[Bod])

                            def stB(nq=nq):
                                fw.op("act", lambda e: e.activation(out=sqd[:, :nq], in_=od[:, :nq], func=AF.Square), reads=[Bod], writes=[Bsqd])

                            def stC(nq=nq, db=db):
                                fw.op("pe", lambda e: e.matmul(ps_all[:, db, :nq], lhsT=bd64[:], rhs=sqd[:, :nq], start=True, stop=True), reads=[Bsqd, Bbd64], writes=[PB[db]])

                            def stD(nq=nq, db=db, ch=ch, q0=q0):
                                rstd_from_ps(ps_all[:, db, :nq], rtd[:, :nq], Brtd, PB[db])
                                fw.op("dve", lambda e: e.tensor_tensor(out=od[:, :nq], in0=od[:, :nq], in1=rtd[:, :nq], op=ALU.mult), reads=[Bod, Brtd], writes=[Bod])
                                oi = cnt["of"] % 2
                                cnt["of"] += 1
                                fw.op("dve", lambda e: e.tensor_scalar(out=of[oi][:, :nq], in0=od[:, :nq], scalar1=lamt[:, 1:2], scalar2=None, op0=ALU.mult), reads=[Bod, Blam], writes=[Bof[oi]])
                                fw.dma("sp", S["mg"][:, 2 + ch, q0:q0 + nq], of[oi][:, :nq], reads=[Bof[oi]], writes=[B["mg"]])
                            deferred.extend([[3, stB], [5, stC], [8, stD]])
                        for ch in range(4):
                            g = ch // 2
                            kA = kgA if g == 0 else kgB
                            kB = kgA if g == 1 else kgB
                            ob, db = attn_pair(lambda kt: kA[0:64, kt * 128:(kt + 1) * 128], lambda kt: kB[64:128, kt * 128:(kt + 1) * 128],
                                               qg_, ch, lambda kt: vg[:, kt, g, 0:64], lambda kt: vg[:, kt, g, 0:64], tiles, nq, 64 ** -0.5)
                            r_i = recip_den(db, nq)
                            oi = cnt["of"] % 2
                            cnt["of"] += 1
                            fw.op("dve", lambda e: e.tensor_tensor(out=of[oi][:, :nq], in0=ps_all[:, ob, :nq], in1=rec[r_i][:, :nq], op=ALU.mult),
                                  reads=[PB[ob], Brec[r_i]], writes=[Bof[oi]])
                            fw.dma("sp", S["mg"][:, 4 + ch, q0:q0 + nq], of[oi][:, :nq], reads=[Bof[oi]], writes=[B["mg"]])
                    run_deferred()
                    fw.barrier()
            if stop == "p2":
                fw.finish()
                return nc, list(I.keys())

            with ExitStack() as ph:
                x0c = sb("x0c", [128, 2, NT], BF16, ph)
                v1 = sb("v1", [128, 2, NT], BF16, ph)
                Bx0c, Bv1 = Buf(), Buf()
                hsk = sb("hsk", [128, 2], F32, ph)
                Bhsk = Buf()
                fw.dma("sp", hsk[:], I["hy_skip_l"][li], reads=[Bin], writes=[Bhsk])
                def p3a_gen(lj, p3, bsin, bout):
                    w1 = sb("hw1", [33, 64], F32, p3)
                    w2 = sb("hw2", [64, 64], F32, p3)
                    w3 = sb("hw3", [64, 512], F32, p3)
                    hb = sb("hb", [64, 4], F32, p3)
                    Bhw = Buf()
                    fw.dma("sp", w1[:], I["hy_w1"][lj], reads=[Bin], writes=[Bhw])
                    fw.dma("sp", w2[:], I["hy_w2"][lj], reads=[Bin], writes=[Bhw])
                    fw.dma("sp", w3[:], I["hy_w3"][lj], reads=[Bin], writes=[Bhw])
                    fw.dma("sp", hb[:, 0:1], I["hy_b1_l"][lj], reads=[Bin], writes=[Bhw])
                    fw.dma("sp", hb[:, 1:2], I["hy_b2_l"][lj], reads=[Bin], writes=[Bhw])
                    fw.dma("sp", hb[:, 2:3], I["hy_fr_l"][lj], reads=[Bin], writes=[Bhw])
                    fw.op("dve", lambda e: e.tensor_scalar(out=hb[:, 0:2], in0=hb[:, 0:2], scalar1=hb[:, 2:3], scalar2=None, op0=ALU.mult), reads=[Bhw], writes=[Bhw])
                    ft = sb("ft", [33, 512], F32, p3)
                    dec = sb("dec", [128, 2, 512], F32, p3)
                    Bft, Bdec = Buf(), Buf()
                    pre = sb("pre", [64, 512], F32, p3)
                    mk = sb("mk", [64, 512], F32, p3)
                    act1 = sb("act1", [64, 512], F32, p3)
                    act2 = sb("act2", [64, 512], F32, p3)
                    Bpre, Bmk, Ba1, Ba2 = Buf(), Buf(), Buf(), Buf()
                    hrow = sb("hrow", [128, 2, 512], BF16, p3)
                    Bhrow = Buf()

                    def sin_layer(ps_ap, bcol, out_t, Bout_, PBk, n):
                        fw.op("dve", lambda e: e.tensor_scalar(out=pre[:, :n], in0=ps_ap, scalar1=hb[:, 2:3], scalar2=hb[:, bcol:bcol + 1], op0=ALU.mult, op1=ALU.add),
                              reads=[PBk, Bhw], writes=[Bpre])
                        fw.op("dve", lambda e: e.tensor_scalar(out=mk[:, :n], in0=pre[:, :n], scalar1=PI, scalar2=None, op0=ALU.is_gt), reads=[Bpre], writes=[Bmk])
                        fw.op("dve", lambda e: e.scalar_tensor_tensor(out=pre[:, :n], in0=mk[:, :n], scalar=-2 * PI, in1=pre[:, :n], op0=ALU.mult, op1=ALU.add), reads=[Bmk, Bpre], writes=[Bpre])
                        fw.op("dve", lambda e: e.tensor_scalar(out=mk[:, :n], in0=pre[:, :n], scalar1=-PI, scalar2=None, op0=ALU.is_lt), reads=[Bpre], writes=[Bmk])
                        fw.op("dve", lambda e: e.scalar_tensor_tensor(out=pre[:, :n], in0=mk[:, :n], scalar=2 * PI, in1=pre[:, :n], op0=ALU.mult, op1=ALU.add), reads=[Bmk, Bpre], writes=[Bpre])
                        fw.op("act", lambda e: e.activation(out=out_t[:, :n], in_=pre[:, :n], func=AF.Sin), reads=[Bpre], writes=[Bout_])

                    hs_ = lj % 2
                    for (L, kf, kfr, kd_, kdr, hkey) in ((SEQ, "featsT", "featsTr", "decT", "decTr", f"hrev{hs_}"), (NCX, "featsTc", "featsTcr", "decTc", "decTcr", f"hrevc{hs_}")):
                        hdst = S[hkey].rearrange("(c p) x -> p c x", p=128)
                        for direction in range(2):
                            fkey, dkey = (kfr, kdr) if direction == 0 else (kf, kd_)
                            colb = 0 if direction == 0 else 256
                            for p0 in range(0, L, 512):
                                n = min(512, L - p0)
                                fw.dma("sp", ft[:, :n], I[fkey][:, p0:p0 + n], reads=[Bin], writes=[Bft])
                                fw.dma("sp", dec[:, :, :n], I[dkey][:, :, p0:p0 + n], reads=[Bin], writes=[Bdec])
                                fw.op("pe", lambda e: e.matmul(ps_all[0:64, bsin[0], :n], lhsT=w1[:, :], rhs=ft[:, :n], start=True, stop=True), reads=[Bhw, Bft], writes=[PB[bsin[0]]])
                                sin_layer(ps_all[0:64, bsin[0], :n], 0, act1, Ba1, PB[bsin[0]], n)
                                yield
                                fw.op("pe", lambda e: e.matmul(ps_all[0:64, bsin[1], :n], lhsT=w2[:, :], rhs=act1[:, :n], start=True, stop=True), reads=[Bhw, Ba1], writes=[PB[bsin[1]]])
                                sin_layer(ps_all[0:64, bsin[1], :n], 1, act2, Ba2, PB[bsin[1]], n)
                                yield
                                for cc in range(2):
                                    fw.op("pe", lambda e: e.matmul(ps_all[:, bout[cc], :n], lhsT=w3[:, colb + cc * 128: colb + (cc + 1) * 128], rhs=act2[:, :n], start=True, stop=True),
                                          reads=[Bhw, Ba2], writes=[PB[bout[cc]]])
                                    fw.op("dve", lambda e: e.tensor_tensor(out=hrow[:, cc, :n], in0=ps_all[:, bout[cc], :n], in1=dec[:, cc, :n], op=ALU.mult), reads=[PB[bout[cc]], Bdec], writes=[Bhrow])
                                if direction == 0:
                                    fw.dma("sp", hdst[:, :, p0:p0 + n], hrow[:, :, :n], reads=[Bhrow], writes=[B[hkey]])
                                else:
                                    i0 = 1 if p0 == 0 else 0
                                    fw.dma("sp", hdst[:, :, L - 1 + p0 + i0: L - 1 + p0 + n], hrow[:, :, i0:n], reads=[Bhrow], writes=[B[hkey]])
                                yield

                if li == 0:
                    with ExitStack() as p3:
                        for _ in p3a_gen(0, p3, (0, 1), (2, 3)):
                            pass
                        fw.barrier()
                if stop == "p3a":
                    fw.finish()
                    return nc, list(I.keys())
                with ExitStack() as p3:
                    cw = sb("cw", [128, 6, 3], F32, p3)
                    cb = sb("cb", [128, 6], F32, p3)
                    Bcw = Buf()
                    fw.dma("sp", cw[:], I["hy_cw_l"][li], reads=[Bin], writes=[Bcw])
                    fw.dma("sp", cb[:], I["hy_cb_l"][li], reads=[Bin], writes=[Bcw])
                    zc = [sb(f"zc{k}", [128, NT], BF16, p3) for k in range(2)]
                    Bzc = [Buf() for _ in range(2)]
                    yA = sb("yA", [128, NT], F32, p3)
                    yB = sb("yB", [128, NT], F32, p3)
                    ByA, ByB = Buf(), Buf()
                    zi = 0
                    for cc in range(2):
                        for role, chunk in (("x1", 2 + cc), ("v", 4 + cc), ("x0", cc)):
                            z_, Bz_ = zc[zi % 2], Bzc[zi % 2]
                            zi += 1
                            fw.dma("sp", z_[:], S["zhy"][:, chunk, :], reads=[B["zhy"]], writes=[Bz_])
                            y_, By_ = (yA, ByA) if role == "x1" else (yB, ByB)
                            fw.op("dve", lambda e: e.tensor_scalar(out=y_[:], in0=z_[:], scalar1=cw[:, chunk, 1:2], scalar2=cb[:, chunk:chunk + 1], op0=ALU.mult, op1=ALU.add),
                                  reads=[Bz_, Bcw], writes=[By_])
                            for (s0, e0) in ((0, NCX), (NCX, NT)):
                                fw.op("dve", lambda e: e.scalar_tensor_tensor(out=y_[:, s0 + 1:e0], in0=z_[:, s0:e0 - 1], scalar=cw[:, chunk, 0:1], in1=y_[:, s0 + 1:e0], op0=ALU.mult, op1=ALU.add),
                                      reads=[Bz_, Bcw, By_], writes=[By_])
                                fw.op("dve", lambda e: e.scalar_tensor_tensor(out=y_[:, s0:e0 - 1], in0=z_[:, s0 + 1:e0], scalar=cw[:, chunk, 2:3], in1=y_[:, s0:e0 - 1], op0=ALU.mult, op1=ALU.add),
                                      reads=[Bz_, Bcw, By_], writes=[By_])
                            if role == "v":
                                fw.op("dve", lambda e: e.tensor_tensor(out=v1[:, cc, :], in0=yB[:], in1=yA[:], op=ALU.mult), reads=[ByA, ByB], writes=[Bv1])
                            elif role == "x0":
                                fw.op("act", lambda e: e.copy(out=x0c[:, cc, :], in_=yB[:]), reads=[ByB], writes=[Bx0c])
                    fw.barrier()
                with ExitStack() as p3:
                    NTL = NT // 128
                    Vt = sb("Vt", [128, 256, NTL], BF16, p3)
                    BVt = Buf()
                    Ysb = sb("Ysb", [128, NTL, 256], BF16, p3)
                    BYsb = Buf()
                    G = [sb(f"G{k}", [128, 8064], BF16, p3) for k in range(2)]
                    BG = [Buf() for _ in range(2)]
                    Gc = sb("Gc", [128, 16, 384], BF16, p3)
                    BGc = Buf()
                    hyo = sb("hyo", [128, 2, NT], BF16, p3)
                    Bhyo = Buf()
                    tmpf = sb("tmpf", [128, 512], F32, p3)
                    Btmpf = Buf()
                    gen3a = p3a_gen(li + 1, p3, (0, 1), (6, 7)) if li + 1 < depth else None
                    hkl, hkc = f"hrev{li % 2}", f"hrevc{li % 2}"
                    k4 = 0
                    for cc in range(2):
                        for j0 in range(0, NTL, 4):
                            nj = min(4, NTL - j0)
                            bk = k4 % 2
                            k4 += 1
                            for k in range(nj):
                                fw.op("pe", lambda e: e.matmul(ps_all[:, bk, k * 128:(k + 1) * 128], lhsT=v1[:, cc, (j0 + k) * 128:(j0 + k + 1) * 128], rhs=ident_bf[:], start=True, stop=True),
                                      reads=[Bv1, Bident], writes=[PB[bk]], same=False, inc=(k == nj - 1))
                            dstv = Vt[:, cc * 128:(cc + 1) * 128, j0:j0 + nj].rearrange("p c j -> p j c")
                            srcv = ps_all[:, bk, 0:nj * 128].rearrange("p (j c) -> p j c", j=nj)
                            if bk == 0:
                                fw.op("dve", lambda e: e.tensor_copy(out=dstv, in_=srcv), reads=[PB[bk]], writes=[BVt])
                            else:
                                fw.op("act", lambda e: e.copy(out=dstv, in_=srcv), reads=[PB[bk]], writes=[BVt])
                    import os
                    NCH = int(os.environ.get('DEV_NCH', '256'))
                    for c in range(NCH):
                        g_, Bg_ = G[c % 2], BG[c % 2]
                        src = bass.AP(S[hkl].tensor, c * 2 * SEQ, [[1, 128], [1, 8064]])
                        fw.dma("sp", g_[:], src, reads=[B[hkl]], writes=[Bg_])
                        bk = 2 + (c // 16) % 2
                        col0 = (c % 16) * 32
                        ds = [0] + [d for d in range(-31, 32) if d != 0]
                        for di, d in enumerate(ds):
                            o_d = SEQ - 128 - 128 * d
                            if d >= 0:
                                oc0, oc1, j0_ = d, 32, 0
                            else:
                                oc0, oc1, j0_ = 0, 32 + d, -d
                            nn = oc1 - oc0
                            last = (di == len(ds) - 1) and (c % 16 == 15 or c == NCH - 1)
                            fw.op("pe", lambda e: e.matmul(ps_all[:, bk, col0 + oc0:col0 + oc1], lhsT=g_[:, o_d:o_d + 128], rhs=Vt[:, c, 2 + j0_:2 + j0_ + nn],
                                                           start=(di == 0 and c % 16 == 0), stop=last, skip_group_check=True),
                                  reads=[Bg_, BVt], writes=[PB[bk]], same=False, inc=(di == len(ds) - 1))
                        if c % 16 == 15 or c == NCH - 1:
                            c0 = c - (c % 16)
                            ncg = c - c0 + 1
                            dsty = Ysb[:, 2:NTL, c0:c0 + ncg].rearrange("p i c -> p c i")
                            srcy = ps_all[:, bk, 0:ncg * 32].rearrange("p (c i) -> p c i", c=ncg)
                            if bk == 2:
                                fw.op("dve", lambda e: e.tensor_copy(out=dsty, in_=srcy), reads=[PB[bk]], writes=[BYsb])
                            else:
                                fw.op("act", lambda e: e.copy(out=dsty, in_=srcy), reads=[PB[bk]], writes=[BYsb])
                        if gen3a is not None and c % 4 == 3:
                            next(gen3a, None)
                    for c0 in range(0, min(NCH, 256), 16):
                        src = bass.AP(S[hkc].tensor, c0 * 2 * NCX, [[1, 128], [2 * NCX, 16], [1, 384]])
                        fw.dma("sp", Gc[:], src, reads=[B[hkc]], writes=[BGc])
                        bk = 4 + (c0 // 16) % 2
                        for cl in range(16):
                            c = c0 + cl
                            for di, d in enumerate((0, 1, -1)):
                                o_d = NCX - 128 - 128 * d
                                if d == 0:
                                    oc0, oc1, j0_ = 0, 2, 0
                                elif d == 1:
                                    oc0, oc1, j0_ = 1, 2, 0
                                else:
                                    oc0, oc1, j0_ = 0, 1, 1
                                nn = oc1 - oc0
                                last = (di == 2 and cl == 15)
                                fw.op("pe", lambda e: e.matmul(ps_all[:, bk, cl * 2 + oc0:cl * 2 + oc1], lhsT=Gc[:, cl, o_d:o_d + 128], rhs=Vt[:, c, j0_:j0_ + nn],
                                                               start=(di == 0 and cl == 0), stop=last, skip_group_check=True),
                                      reads=[BGc, BVt], writes=[PB[bk]], same=False, inc=last)
                        dsty = Ysb[:, 0:2, c0:c0 + 16].rearrange("p i c -> p c i")
                        srcy = ps_all[:, bk, 0:32].rearrange("p (c i) -> p c i", c=16)
                        fw.op("dve", lambda e: e.tensor_copy(out=dsty, in_=srcy), reads=[PB[bk]], writes=[BYsb])
                    if gen3a is not None:
                        for _ in gen3a:
                            pass
                    k4 = 0
                    for cc in range(2):
                        for i0 in range(0, NTL, 4):
                            ni = min(4, NTL - i0)
                            bk = 6 + k4 % 2
                            k4 += 1
                            for k in range(ni):
                                fw.op("pe", lambda e: e.matmul(ps_all[:, bk, k * 128:(k + 1) * 128], lhsT=Ysb[:, i0 + k, cc * 128:(cc + 1) * 128], rhs=anti_bf[:], start=True, stop=True),
                                      reads=[BYsb, Banti], writes=[PB[bk]], same=False, inc=(k == ni - 1))
                            tsl = slice(i0 * 128, (i0 + ni) * 128)
                            nn = ni * 128
                            fw.op("dve", lambda e: e.scalar_tensor_tensor(out=tmpf[:, :nn], in0=v1[:, cc, tsl], scalar=hsk[:, cc:cc + 1], in1=ps_all[:, bk, :nn], op0=ALU.mult, op1=ALU.add),
                                  reads=[Bv1, Bhsk, PB[bk]], writes=[Btmpf])
                            fw.op("dve", lambda e: e.tensor_tensor(out=hyo[:, cc, tsl], in0=tmpf[:, :nn], in1=x0c[:, cc, tsl], op=ALU.mult), reads=[Btmpf, Bx0c], writes=[Bhyo])
                    fw.dma("sp", S["mg"][:, 0:2, :], hyo[:], reads=[Bhyo], writes=[B["mg"]])
                    fw.barrier()
            if stop == "p3":
                fw.finish()
                return nc, list(I.keys())

            with ExitStack() as lf:
                gT = sb("gT", [8, NT], BF16, lf)
                BgT = Buf()
                with ExitStack() as ph:
                    wout = sb("wout", [128, 8, D], BF16, ph)
                    Bwout = Buf()
                    fw.dma("pool", wout[:], I["w_out"][li].rearrange("(j p) n -> p j n", p=128), reads=[Bin], writes=[Bwout])
                    mgb2 = [sb(f"mgb{k}", [128, 8, 512], BF16, ph) for k in range(2)]
                    Bmgb2 = [Buf() for _ in range(2)]
                    hblk2 = [sb(f"hblk4{k}", [128, 8, 512], F32, ph) for k in range(2)]
                    Bhblk2 = [Buf() for _ in range(2)]
                    sq = sb("sq4", [128, 8, 512], BF16, ph)
                    Bsq = Buf()
                    rt = sb("rt4", [128, 512], F32, ph)
                    Brt = Buf()
                    tmp = sb("tmp4", [128, 512], F32, ph)
                    Btmp = Buf()
                    u2b = sb("u2b", [128, 8, 512], BF16, ph)
                    Bu2b = Buf()
                    if moe:
                        u2f = sb("u2f", [128, 8, 512], F32, ph)
                        Bu2f = Buf()
                        rtr = sb("rtr", [128, 8, NEXP], F32, ph)
                        Brtr = Buf()
                        fw.dma("sp", rtr[:], I["moe_router"][li // 2].rearrange("(j p) n -> p j n", p=128), reads=[Bin], writes=[Brtr])
                        lg = sb("lg", [128, 8], F32, ph)
                        l2 = sb("l2", [128, 8], F32, ph)
                        eq1 = sb("eq1", [128, 8], F32, ph)
                        eq2 = sb("eq2", [128, 8], F32, ph)
                        gts = sb("gts", [128, 8], F32, ph)
                        mm_ = sb("mm_", [128, 8], F32, ph)
                        Brt_ = Buf()
                    for bi4, (t0, n, seg) in enumerate(TBLK[1:] if last else TBLK):
                        mgb, Bmgb, hblk, Bhblk = mgb2[bi4 % 2], Bmgb2[bi4 % 2], hblk2[bi4 % 2], Bhblk2[bi4 % 2]
                        fw.dma("sp", mgb[:, :, :n], S["mg"][:, :, t0:t0 + n], reads=[B["mg"]], writes=[Bmgb])
                        fw.dma("sp", hblk[:, :, :n], S["hT"][:, :, t0:t0 + n], reads=[B["hT"]], writes=[Bhblk])
                        for dc in range(8):
                            bk = dc % 2
                            mm_group(ps_all[:, bk, :n], [(wout[:, j, dc * 128:(dc + 1) * 128], mgb[:, j, :n]) for j in range(8)], [Bwout, Bmgb], PB[bk])
                            fw.op("dve", lambda e: e.scalar_tensor_tensor(out=hblk[:, dc, :n], in0=ps_all[:, bk, :n], scalar=prm[:, 2, dc, seg:seg + 1], in1=hblk[:, dc, :n], op0=ALU.mult, op1=ALU.add),
                                  reads=[PB[bk], Bprm, Bhblk], writes=[Bhblk])
                        fw.dma("sp", S["hT"][:, :, t0:t0 + n], hblk[:, :, :n], reads=[Bhblk], writes=[B["hT"]])
                        outs = [(u2b, Bu2b)] + ([(u2f, Bu2f)] if moe else [])
                        norm_block(hblk, Bhblk, n, seg, 3, sq, Bsq, rt, Brt, tmp, Btmp, outs)
                        fw.dma("sp", S["u2"][:, :, t0:t0 + n], u2b[:, :, :n], reads=[Bu2b], writes=[B["u2"]])
                        if moe:
                            for tt in range(n // 128):
                                tok0 = t0 + tt * 128
                                mm_group(ps_all[:, 2, 0:8], [(u2f[:, j, tt * 128:(tt + 1) * 128], rtr[:, j, :]) for j in range(8)], [Bu2f, Brtr], PB[2])
                                fw.op("dve", lambda e: e.tensor_copy(out=lg[:], in_=ps_all[:, 2, 0:8]), reads=[PB[2]], writes=[Brt_])
                                fw.op("dve", lambda e: e.reduce_max(out=mm_[:, 0:1], in_=lg[:], axis=AX.X), reads=[Brt_], writes=[Brt_])
                                fw.op("dve", lambda e: e.tensor_scalar(out=eq1[:], in0=lg[:], scalar1=mm_[:, 0:1], scalar2=None, op0=ALU.is_equal), reads=[Brt_], writes=[Brt_])
                                fw.op("dve", lambda e: e.scalar_tensor_tensor(out=l2[:], in0=eq1[:], scalar=-1e30, in1=lg[:], op0=ALU.mult, op1=ALU.add), reads=[Brt_], writes=[Brt_])
                                fw.op("dve", lambda e: e.reduce_max(out=mm_[:, 1:2], in_=l2[:], axis=AX.X), reads=[Brt_], writes=[Brt_])
                                fw.op("dve", lambda e: e.tensor_scalar(out=eq2[:], in0=l2[:], scalar1=mm_[:, 1:2], scalar2=None, op0=ALU.is_equal), reads=[Brt_], writes=[Brt_])
                                fw.op("dve", lambda e: e.tensor_tensor(out=mm_[:, 2:3], in0=mm_[:, 1:2], in1=mm_[:, 0:1], op=ALU.subtract), reads=[Brt_], writes=[Brt_])
                                fw.op("act", lambda e: e.activation(out=mm_[:, 3:4], in_=mm_[:, 2:3], func=AF.Exp), reads=[Brt_], writes=[Brt_])
                                fw.op("dve", lambda e: e.tensor_scalar(out=mm_[:, 4:5], in0=mm_[:, 3:4], scalar1=1.0, scalar2=None, op0=ALU.add), reads=[Brt_], writes=[Brt_])
                                fw.op("dve", lambda e: e.reciprocal(out=mm_[:, 5:6], in_=mm_[:, 4:5]), reads=[Brt_], writes=[Brt_])
                                fw.op("dve", lambda e: e.tensor_tensor(out=mm_[:, 6:7], in0=mm_[:, 3:4], in1=mm_[:, 5:6], op=ALU.mult), reads=[Brt_], writes=[Brt_])
                                fw.op("dve", lambda e: e.tensor_scalar(out=gts[:], in0=eq1[:], scalar1=mm_[:, 5:6], scalar2=None, op0=ALU.mult), reads=[Brt_], writes=[Brt_])
                                fw.op("dve", lambda e: e.scalar_tensor_tensor(out=gts[:], in0=eq2[:], scalar=mm_[:, 6:7], in1=gts[:], op0=ALU.mult, op1=ALU.add), reads=[Brt_], writes=[Brt_])
                                fw.op("pe", lambda e: e.matmul(ps_all[0:8, 3, 0:128], lhsT=gts[:], rhs=ident_f[:], start=True, stop=True), reads=[Brt_, Bidentf], writes=[PB[3]])
                                fw.op("dve", lambda e: e.tensor_copy(out=gT[:, tok0:tok0 + 128], in_=ps_all[0:8, 3, 0:128]), reads=[PB[3]], writes=[BgT])
                    fw.barrier()
                if stop == "p4":
                    if debug and moe:
                        with ExitStack() as ph:
                            dbg = sb("dbg4", [8, NT], F32, ph)
                            Bdbg = Buf()
                            fw.op("dve", lambda e: e.tensor_copy(out=dbg[:], in_=gT[:]), reads=[BgT], writes=[Bdbg])
                            fw.dma("sp", S["dbg_kv"][0:8, 0, :], dbg[:], reads=[Bdbg], writes=[Bout])
                    fw.finish()
                    return nc, list(I.keys())

                with ExitStack() as ph:
                    SBL = [(0, 1536), (1536, 1536), (3072, NT - 3072)]
                    if last:
                        SBL = [(NCX, 1536), (NCX + 1536, 1536), (NCX + 3072, 1024)]
                    SBW = 1536
                    if moe:
                        groups = [(e_, f0, 512) for e_ in range(NEXP) for f0 in range(0, DFE, 512)]
                    else:
                        groups = [(0, f0, min(512, DFF - f0)) for f0 in range(0, DFF, 512)]

                    def wviews(e_):
                        if moe:
                            return (I["moe_wg"][li // 2][e_].rearrange("(j p) f -> p j f", p=128),
                                    I["moe_wu"][li // 2][e_].rearrange("(j p) f -> p j f", p=128),
                                    I["moe_wd"][li // 2][e_].rearrange("(c p) d -> p c d", p=128))
                        return (I["ffn_wg"][li // 2].rearrange("(j p) f -> p j f", p=128),
                                I["ffn_wu"][li // 2].rearrange("(j p) f -> p j f", p=128),
                                I["ffn_wd"][li // 2].rearrange("(c p) d -> p c d", p=128))
                    wgb = [sb(f"wgb{k}", [128, 8, 512], BF16, ph) for k in range(2)]
                    wub = [sb(f"wub{k}", [128, 8, 512], BF16, ph) for k in range(2)]
                    wdb = [sb(f"wdb{k}", [128, 4, D], BF16, ph) for k in range(2)]
                    Bw = [Buf() for _ in range(2)]
                    u2k = sb("u2k", [128, 8, SBW], BF16, ph)
                    Bu2k = Buf()
                    acc = sb("acc", [128, 8, SBW], F32, ph)
                    Bacc = Buf()
                    Hb = [sb(f"Hb{k}", [128, 4, SBW], BF16, ph) for k in range(2)]
                    BH = [Buf() for _ in range(2)]
                    sgt = [sb(f"sgt{k}", [128, 512], BF16, ph) for k in range(2)]
                    Bsgt = [Buf() for _ in range(2)]
                    gbc = sb("gbc", [128, SBW], BF16, ph)
                    Bgbc = Buf()
                    hres = sb("hres", [128, SBW], F32, ph)
                    Bhres = Buf()
                    import os
                    NSB = int(os.environ.get('DEV_NSB', '3'))
                    NGR = int(os.environ.get('DEV_NGR', '999'))
                    groups = groups[:NGR]

                    Bwd = [Buf() for _ in range(2)]

                    def load_w(gi):
                        e_, f0, nf = groups[gi]
                        wg_, wu_, wd_ = wviews(e_)
                        k = gi % 2
                        fw.dma("pool", wgb[k][:, :, :nf], wg_[:, :, f0:f0 + nf], reads=[Bin], writes=[Bw[k]])
                        fw.dma("pool", wub[k][:, :, :nf], wu_[:, :, f0:f0 + nf], reads=[Bin], writes=[Bw[k]])

                    def load_wd(gi):
                        e_, f0, nf = groups[gi]
                        wg_, wu_, wd_ = wviews(e_)
                        k = gi % 2
                        fw.dma("pool", wdb[k][:, :nf // 128, :], wd_[:, f0 // 128:(f0 + nf) // 128, :], reads=[Bin], writes=[Bwd[k]])

                    for sbi in range(NSB):
                        t0, sw = SBL[sbi]
                        subs = [(s0, min(512, sw - s0)) for s0 in range(0, sw, 512)]
                        fw.dma("sp", u2k[:, :, :sw], S["u2"][:, :, t0:t0 + sw], reads=[B["u2"]], writes=[Bu2k])
                        fw.op("pool", lambda e: e.memset(acc[:], 0.0), writes=[Bacc])
                        load_w(0)
                        load_wd(0)
                        cnt5 = {"sg": 0, "ob": 0}

                        def phaseB_units(gi):
                            e_, f0, nf = groups[gi]
                            k = gi % 2
                            nfc = nf // 128
                            units = []
                            for dc in range(8):
                                for (s0, ns) in subs:
                                    units.append((k, nfc, dc, s0, ns))
                            return units

                        def emitB(unit):
                            k, nfc, dc, s0, ns = unit
                            ob = 6 + cnt5["ob"] % 2
                            cnt5["ob"] += 1
                            mm_group(ps_all[:, ob, :ns], [(wdb[k][:, fc2, dc * 128:(dc + 1) * 128], Hb[k][:, fc2, s0:s0 + ns]) for fc2 in range(nfc)], [Bwd[k], BH[k]], PB[ob])
                            fw.op("dve", lambda e: e.tensor_tensor(out=acc[:, dc, s0:s0 + ns], in0=acc[:, dc, s0:s0 + ns], in1=ps_all[:, ob, :ns], op=ALU.add),
                                  reads=[Bacc, PB[ob]], writes=[Bacc])

                        pendingB = []
                        cur_e = -1
                        for gi, (e_, f0, nf) in enumerate(groups):
                            if gi + 1 < len(groups):
                                load_w(gi + 1)
                            k = gi % 2
                            nfc = nf // 128
                            if moe and e_ != cur_e:
                                cur_e = e_
                                for (s0, ns) in subs:
                                    fw.op("pe", lambda e: e.matmul(ps_all[:, 7, :ns], lhsT=sel_bf[:, e_, :], rhs=gT[:, t0 + s0:t0 + s0 + ns], start=True, stop=True),
                                          reads=[Bsel, BgT], writes=[PB[7]])
                                    fw.op("act", lambda e: e.copy(out=gbc[:, s0:s0 + ns], in_=ps_all[:, 7, :ns]), reads=[PB[7]], writes=[Bgbc])
                            per_fc = (len(pendingB) + nfc - 1) // nfc if pendingB else 0
                            for fc in range(nfc):
                                for si, (s0, ns) in enumerate(subs):
                                    gbk, ubk = (2 + si, 4 + si) if si < 2 else (0, 1)
                                    gcol = 0
                                    ucol = 0
                                    mm_group(ps_all[:, gbk, gcol:gcol + ns], [(wgb[k][:, j, fc * 128:(fc + 1) * 128], u2k[:, j, s0:s0 + ns]) for j in range(8)], [Bw[k], Bu2k], PB[gbk])
                                    mm_group(ps_all[:, ubk, ucol:ucol + ns], [(wub[k][:, j, fc * 128:(fc + 1) * 128], u2k[:, j, s0:s0 + ns]) for j in range(8)], [Bw[k], Bu2k], PB[ubk])
                                    sgi = cnt5["sg"] % 2
                                    cnt5["sg"] += 1
                                    fw.op("act", lambda e: e.activation(out=sgt[sgi][:, :ns], in_=ps_all[:, gbk, gcol:gcol + ns], func=AF.Silu), reads=[PB[gbk]], writes=[Bsgt[sgi]])
                                    if moe:
                                        fw.op("dve", lambda e: e.tensor_tensor(out=sgt[sgi][:, :ns], in0=sgt[sgi][:, :ns], in1=gbc[:, s0:s0 + ns], op=ALU.mult), reads=[Bsgt[sgi], Bgbc], writes=[Bsgt[sgi]])
                                    fw.op("dve", lambda e: e.tensor_tensor(out=Hb[k][:, fc, s0:s0 + ns], in0=sgt[sgi][:, :ns], in1=ps_all[:, ubk, ucol:ucol + ns], op=ALU.mult),
                                          reads=[Bsgt[sgi], PB[ubk]], writes=[BH[k]])
                                for _ in range(per_fc):
                                    if pendingB:
                                        emitB(pendingB.pop(0))
                            while pendingB:
                                emitB(pendingB.pop(0))
                            if gi + 1 < len(groups):
                                load_wd(gi + 1)
                            pendingB = phaseB_units(gi)
                        while pendingB:
                            emitB(pendingB.pop(0))
                        for dc in range(8):
                            fw.dma("sp", hres[:, :sw], S["hT"][:, dc, t0:t0 + sw], reads=[B["hT"]], writes=[Bhres])
                            rngs = []
                            if t0 < NCX:
                                rngs.append((0, NCX - t0, 1))
                                rngs.append((NCX - t0, sw, 0))
                            else:
                                rngs.append((0, sw, 0))
                            for (c0, c1, seg) in rngs:
                                fw.op("dve", lambda e: e.scalar_tensor_tensor(out=hres[:, c0:c1], in0=acc[:, dc, c0:c1], scalar=prm[:, 5, dc, seg:seg + 1], in1=hres[:, c0:c1], op0=ALU.mult, op1=ALU.add),
                                      reads=[Bacc, Bprm, Bhres], writes=[Bhres])
                            fw.dma("sp", S["hT"][:, dc, t0:t0 + sw], hres[:, :sw], reads=[Bhres], writes=[B["hT"]])
                    fw.barrier()
            if stop == f"l{li}":
                fw.finish()
                return nc, list(I.keys())

        with ExitStack() as ph:
            hblk = sb("hblkF", [128, 8, 512], F32, ph)
            Bhblk = Buf()
            sq = sb("sqF", [128, 8, 512], BF16, ph)
            Bsq = Buf()
            rt = sb("rtF", [128, 512], F32, ph)
            Brt = Buf()
            hn = sb("hnF", [128, 8, 512], F32, ph)
            Bhn = Buf()
            ot = [sb(f"otF{k}", [128, D], F32, ph) for k in range(2)]
            Bot = [Buf() for _ in range(2)]
            kk = 0
            for (t0, n, seg) in TBLK[1:]:
                fw.dma("sp", hblk[:, :, :n], S["hT"][:, :, t0:t0 + n], reads=[B["hT"]], writes=[Bhblk])
                fw.op("act", lambda e: e.activation(out=sq[:, :, :n], in_=hblk[:, :, :n], func=AF.Square), reads=[Bhblk], writes=[Bsq])
                mm_group(ps_all[:, 0, :n], [(onesD[:], sq[:, j, :n]) for j in range(8)], [BonesD, Bsq], PB[0])
                rstd_from_ps(ps_all[:, 0, :n], rt[:, :n], Brt, PB[0])
                for j in range(8):
                    fw.op("dve", lambda e: e.scalar_tensor_tensor(out=hn[:, j, :n], in0=hblk[:, j, :n], scalar=gfin[:, j:j + 1], in1=rt[:, :n], op0=ALU.mult, op1=ALU.mult),
                          reads=[Bhblk, Bgfin, Brt], writes=[Bhn])
                for tt in range(n // 128):
                    o_, Bo_ = ot[kk % 2], Bot[kk % 2]
                    for half in range(2):
                        bk = 2 + 2 * (kk % 2) + half
                        for jj in range(4):
                            j = half * 4 + jj
                            fw.op("pe", lambda e: e.matmul(ps_all[:, bk, jj * 128:(jj + 1) * 128], lhsT=hn[:, j, tt * 128:(tt + 1) * 128], rhs=ident_f[:], start=True, stop=True),
                                  reads=[Bhn, Bidentf], writes=[PB[bk]], same=False, inc=(jj == 3))
                        if half == 0:
                            fw.op("dve", lambda e: e.tensor_copy(out=o_[:, 0:512], in_=ps_all[:, bk, :]), reads=[PB[bk]], writes=[Bo_])
                        else:
                            fw.op("act", lambda e: e.copy(out=o_[:, 512:1024], in_=ps_all[:, bk, :]), reads=[PB[bk]], writes=[Bo_])
                    r0 = t0 - NCX + tt * 128
                    fw.dma("sp", out[r0:r0 + 128, :], o_[:], reads=[Bo_], writes=[Bout])
                    kk += 1
        fw.finish()
        return nc, list(I.keys())

    return nc, list(I.keys())


def _in_map(inputs, b, consts, names, small=None):
    small = small if small is not None else _layer_small(inputs, b)
    m = {}
    for k in names:
        if k == "x":
            m[k] = np.ascontiguousarray(inputs["x"][b], dtype=np.float32)
        elif k == "ctx":
            m[k] = np.ascontiguousarray(inputs["ctx"][b], dtype=np.float32)
        elif k in small:
            m[k] = small[k]
        elif k in consts:
            m["k_" + k] = consts[k]
        else:
            m[k] = np.ascontiguousarray(inputs[k], dtype=np.float32)
    return m


def kernel(**inputs):
    consts = _constants()
    nc, names = build_program()
    big = {k: np.ascontiguousarray(inputs[k], dtype=np.float32) for k in names if k in BIG_KEYS}
    in_maps = []
    for b in range(8):
        m = _in_map(inputs, b, consts, [k for k in names if k not in BIG_KEYS])
        m.update(big)
        in_maps.append(m)
    res = run_bass_kernel_spmd(nc, in_maps, core_ids=list(range(8)))
    return np.stack([np.asarray(r["out"], np.float32) for r in res.results], axis=0)
```

```python
import math
from contextlib import ExitStack

import numpy as np
import ml_dtypes
import concourse.bass as bass
import concourse.mybir as mybir
from concourse.bass_utils import run_bass_kernel_spmd

F32 = mybir.dt.float32
BF16 = mybir.dt.bfloat16
AF = mybir.ActivationFunctionType
ALU = mybir.AluOpType
AX = mybir.AxisListType

D = 1024
NJ = 8
SEQ = 4096
NCX = 256
NT = SEQ + NCX
DEPTH = 4
GRID_W = 64
EPS = 1e-6
DFF = 2816
DFE = 3584
NEXP = 8
OFF_DQ, OFF_DK, OFF_DV, OFF_GQ, OFF_GK, OFF_GV, IN_COLS = 768, 1024, 1280, 1536, 2048, 2176, 2304
PI = float(np.pi)


class Buf:
    __slots__ = ("name", "w", "r", "excl")

    def __init__(self, name="", excl=False):
        self.name = name
        self.w = None
        self.r = {}
        self.excl = excl


class _Eng:
    def __init__(self, name, eng, sem):
        self.name = name
        self.eng = eng
        self.sem = sem
        self.tick = 0
        self.pending = False
        self.seen = {}


class FW:
    NDMA = 32

    def __init__(self, nc, stack):
        self.nc = nc
        self.engs = {}
        for nm, e in (("pe", nc.tensor), ("dve", nc.vector), ("act", nc.scalar),
                      ("pool", nc.gpsimd), ("sp", nc.sync)):
            sem = stack.enter_context(nc.semaphore("s_" + nm))
            self.engs[nm] = _Eng(nm, e, sem)
        self.dma_sems = [stack.enter_context(nc.semaphore(f"s_dma{i}")) for i in range(self.NDMA)]
        self.dma_cnt = [0] * self.NDMA
        self.dma_next = 0
        self.n_inst = 0

    def _wait(self, E, prod, tick):
        if E.seen.get(prod, 0) >= tick:
            return
        if prod == E.name and tick > E.tick:
            return
        E.seen[prod] = tick
        if prod.startswith("dma"):
            sem = self.dma_sems[int(prod[3:])]
        else:
            sem = self.engs[prod].sem
        E.eng.wait_ge(sem, tick)

    def _deps(self, E, reads, writes, same):
        for b in reads:
            if b.w is not None and (same or b.w[0] != E.name):
                self._wait(E, *b.w)
            if b.excl:
                for p, t in b.r.items():
                    if p != E.name:
                        self._wait(E, p, t)
        for b in writes:
            if b.w is not None and (same or b.w[0] != E.name):
                self._wait(E, *b.w)
            for p, t in b.r.items():
                if p != E.name:
                    self._wait(E, p, t)

    @staticmethod
    def _mark(prod, tick, reads, writes):
        for b in reads:
            if b.r.get(prod, 0) < tick:
                b.r[prod] = tick
        for b in writes:
            b.w = (prod, tick)
            b.r = {}

    def op(self, engname, fn, reads=(), writes=(), same=True, inc=True):
        E = self.engs[engname]
        self._deps(E, reads, writes, same)
        ins = fn(E.eng)
        if inc:
            E.tick += 1
            E.pending = False
            ins.then_inc(E.sem, 1)
            self._mark(E.name, E.tick, reads, writes)
        else:
            E.pending = True
            self._mark(E.name, E.tick + 1, reads, writes)
        self.n_inst += 1
        return ins

    def dma(self, qname, out, in_, reads=(), writes=(), **kw):
        E = self.engs[qname]
        slot = self.dma_next
        self.dma_next = (self.dma_next + 1) % self.NDMA
        pname = f"dma{slot}"
        if self.dma_cnt[slot] > 0:
            self._wait(E, pname, 16 * self.dma_cnt[slot])
        self._deps(E, reads, writes, True)
        ins = E.eng.dma_start(out=out, in_=in_, **kw)
        self.dma_cnt[slot] += 1
        ins.then_inc(self.dma_sems[slot], 16)
        self._mark(pname, 16 * self.dma_cnt[slot], reads, writes)
        self.n_inst += 1
        return ins

    def _flush_pending(self):
        for nm, E in self.engs.items():
            if E.pending:
                E.tick += 1
                E.pending = False
                E.eng.nop().then_inc(E.sem, 1)

    def barrier(self):
        self._flush_pending()
        for nm, E in self.engs.items():
            for nm2, E2 in self.engs.items():
                if nm2 != nm and E2.tick:
                    self._wait(E, nm2, E2.tick)
            for s in range(self.NDMA):
                if self.dma_cnt[s]:
                    self._wait(E, f"dma{s}", 16 * self.dma_cnt[s])

    def finish(self):
        self._flush_pending()
        E = self.engs["sp"]
        for s in range(self.NDMA):
            if self.dma_cnt[s]:
                self._wait(E, f"dma{s}", 16 * self.dma_cnt[s])
        for nm, e in self.engs.items():
            if nm != "sp" and e.tick:
                self._wait(E, nm, e.tick)


def _rope_tables(head_dim):
    t = np.arange(SEQ)
    row = (t // GRID_W).astype(np.float32)
    col = (t % GRID_W).astype(np.float32)
    n = head_dim // 4
    inv = (10000.0 ** (-np.arange(n, dtype=np.float32) / n)).astype(np.float32)
    ang = np.concatenate([row[:, None] * inv, col[:, None] * inv], axis=-1).astype(np.float32)
    cos = np.cos(ang).astype(np.float32)
    sin = np.sin(ang).astype(np.float32)
    half = head_dim // 2
    C = np.ones((128, NT), np.float32)
    S = np.zeros((128, NT), np.float32)
    for p in range(128):
        dd = p % head_dim
        a = dd % half
        C[p, NCX:] = cos[:, a]
        S[p, NCX:] = -sin[:, a] if dd < half else sin[:, a]
    return C, S


def _hyena_tables(L):
    t = np.linspace(0.0, 1.0, L, dtype=np.float32)[:, None]
    bands = 16
    ang = (np.float32(2.0 * math.pi / L) * np.arange(L, dtype=np.float32)[:, None]
           * np.linspace(1e-4, bands - 1, bands, dtype=np.float32)).astype(np.float32)
    feats = np.concatenate([t, np.cos(ang), -np.sin(ang)], axis=-1).astype(np.float32)
    max_decay = math.log(1e-2) / 0.3
    min_decay = math.log(1e-2) / 1.5
    deltas = np.abs(np.linspace(min_decay, max_decay, 256, dtype=np.float32))
    decay = np.exp(-t * deltas).astype(np.float32)
    featsT = np.ascontiguousarray(feats.T)
    decT = np.ascontiguousarray(decay.T.reshape(2, 128, L).transpose(1, 0, 2))
    return featsT, np.ascontiguousarray(featsT[:, ::-1]), decT, np.ascontiguousarray(decT[:, :, ::-1])


def _pj(v, n=NJ):
    return np.ascontiguousarray(np.asarray(v, np.float32).reshape(n, 128).T)


_CONST_CACHE = {}


def _constants():
    if _CONST_CACHE:
        return _CONST_CACHE
    c = _CONST_CACHE
    c["cos_g"], c["sin_g"] = _rope_tables(64)
    c["cos_d"], c["sin_d"] = _rope_tables(32)
    c["featsT"], c["featsTr"], c["decT"], c["decTr"] = _hyena_tables(SEQ)
    c["featsTc"], c["featsTcr"], c["decTc"], c["decTcr"] = _hyena_tables(NCX)
    bf = ml_dtypes.bfloat16
    c["ident_bf"] = np.eye(128, dtype=np.float32).astype(bf)
    c["anti_bf"] = np.ascontiguousarray(np.eye(128, dtype=np.float32)[::-1]).astype(bf)
    c["ident_f"] = np.eye(128, dtype=np.float32)
    c["onesD_bf"] = np.full((128, 128), 1.0 / D, np.float32).astype(bf)
    bd = np.zeros((128, 128), np.float32)
    bd[:64, :64] = 1.0 / 64
    bd[64:, 64:] = 1.0 / 64
    c["bd64_bf"] = bd.astype(bf)
    c["ones_f"] = np.ones((128, 128), np.float32)
    sel = np.zeros((8, 8, 128), np.float32)
    for e in range(8):
        sel[e, e, :] = 1.0
    c["sel_bf"] = sel.astype(bf)
    m = np.zeros((128, 2), np.float32)
    for p in range(128):
        m[p, (p // 32) % 2] = 1.0
    c["dmask"] = m
    c["ones64_bf"] = np.ones((128, 64), np.float32).astype(bf)
    return c


def _layer_small(inputs, b):
    s = {}
    s["cvec"] = np.ascontiguousarray(np.stack([_pj(inputs["c"][b]), _pj(inputs["c_ctx"])], axis=-1))
    s["b_ada_l"] = np.ascontiguousarray(np.stack([_pj(inputs["b_ada"][i], 48) for i in range(DEPTH)]))
    s["g_mix_l"] = np.ascontiguousarray(np.stack([_pj(inputs["g_mix"][i]) for i in range(DEPTH)]))
    s["g_ffn_l"] = np.ascontiguousarray(np.stack([_pj(inputs["g_ffn"][i]) for i in range(DEPTH)]))
    s["g_final_l"] = _pj(inputs["g_final"])
    cw = np.asarray(inputs["hy_conv_w"], np.float32)
    s["hy_cw_l"] = np.ascontiguousarray(cw.reshape(DEPTH, 3, 6, 128).transpose(0, 3, 2, 1))
    s["hy_cb_l"] = np.ascontiguousarray(np.asarray(inputs["hy_conv_b"], np.float32).reshape(DEPTH, 6, 128).transpose(0, 2, 1))
    s["hy_b1_l"] = np.ascontiguousarray(np.asarray(inputs["hy_b1"], np.float32)[:, :, None])
    s["hy_b2_l"] = np.ascontiguousarray(np.asarray(inputs["hy_b2"], np.float32)[:, :, None])
    s["hy_fr_l"] = np.ascontiguousarray(np.asarray(inputs["hy_freq"], np.float32)[:, :, None])
    s["hy_skip_l"] = np.ascontiguousarray(np.asarray(inputs["hy_skip"], np.float32).reshape(DEPTH, 2, 128).transpose(0, 2, 1))
    s["diff_l"] = np.ascontiguousarray(np.stack([inputs["diff_lq1"], inputs["diff_lk1"], inputs["diff_lq2"],
                                                 inputs["diff_lk2"]], axis=1).astype(np.float32)[:, None])
    sub = np.asarray(inputs["diff_subln"], np.float32)
    s["subln_l"] = np.ascontiguousarray(np.stack([np.tile(sub[i], 2) for i in range(DEPTH)])[:, :, None])
    idx = np.arange(128) % 64
    idx_sw = (idx + 32) % 64
    for nm, key in (("gq_l", "gqa_qnorm"), ("gk_l", "gqa_knorm")):
        g = np.asarray(inputs[key], np.float32)
        s[nm] = np.ascontiguousarray(np.stack([np.stack([g[i][idx], g[i][idx_sw]], axis=-1) for i in range(DEPTH)]))
    return s


BIG_KEYS = ["w_ada", "w_in", "w_out", "hy_w1", "hy_w2", "hy_w3", "ffn_wg", "ffn_wu", "ffn_wd",
            "moe_router", "moe_wg", "moe_wu", "moe_wd"]


def build_program(depth=DEPTH, stop=None, debug=False):
    nc = bass.Bass("TRN2", target_bir_lowering=False)
    consts = _constants()
    np2dt = {np.dtype(np.float32): F32, np.dtype(ml_dtypes.bfloat16): BF16}

    def din(name, shape, dt=F32):
        return nc.dram_tensor(name, list(shape), dt, kind="ExternalInput").ap()

    shapes = {
        "x": [SEQ, D], "ctx": [NCX, D],
        "cvec": [128, 8, 2], "b_ada_l": [DEPTH, 128, 48], "g_mix_l": [DEPTH, 128, 8], "g_ffn_l": [DEPTH, 128, 8],
        "g_final_l": [128, 8], "hy_cw_l": [DEPTH, 128, 6, 3], "hy_cb_l": [DEPTH, 128, 6], "hy_b1_l": [DEPTH, 64, 1],
        "hy_b2_l": [DEPTH, 64, 1], "hy_fr_l": [DEPTH, 64, 1], "hy_skip_l": [DEPTH, 128, 2], "diff_l": [DEPTH, 1, 4, 32],
        "subln_l": [DEPTH, 128, 1], "gq_l": [DEPTH, 128, 2], "gk_l": [DEPTH, 128, 2],
        "w_ada": [DEPTH, D, 6 * D], "w_in": [DEPTH, D, IN_COLS], "w_out": [DEPTH, D, D],
        "hy_w1": [DEPTH, 33, 64], "hy_w2": [DEPTH, 64, 64], "hy_w3": [DEPTH, 64, 512],
        "ffn_wg": [2, D, DFF], "ffn_wu": [2, D, DFF], "ffn_wd": [2, DFF, D],
        "moe_router": [2, D, NEXP], "moe_wg": [2, NEXP, D, DFE], "moe_wu": [2, NEXP, D, DFE], "moe_wd": [2, NEXP, DFE, D],
    }

    class _Inputs(dict):
        def __missing__(self, key):
            if key in shapes:
                ap = din(key, shapes[key])
            else:
                v = consts[key]
                ap = din("k_" + key, v.shape, np2dt[v.dtype])
            self[key] = ap
            return ap

    I = _Inputs()
    out = nc.dram_tensor("out", [SEQ, D], F32, kind="ExternalOutput").ap()

    skind = "ExternalOutput" if debug else "Internal"

    def dscr(name, shape, dt):
        return nc.dram_tensor(name, list(shape), dt, kind=skind).ap()

    S = {}
    S["hT"] = dscr("s_hT", [128, NJ, NT], F32)
    S["zhy"] = dscr("s_zhy", [128, 6, NT], BF16)
    S["qd"] = dscr("s_qd", [128, 2, NT], BF16)
    S["qg"] = dscr("s_qg", [128, 4, NT], BF16)
    S["mg"] = dscr("s_mg", [128, NJ, NT], BF16)
    S["u2"] = dscr("s_u2", [128, NJ, NT], BF16)
    for k_ in range(2):
        S[f"hrev{k_}"] = dscr(f"s_hrev{k_}", [256, 2 * SEQ], BF16)
        S[f"hrevc{k_}"] = dscr(f"s_hrevc{k_}", [256, 2 * NCX], BF16)
    if debug:
        S["dbg_kv"] = dscr("s_dbgkv", [128, 12, NT], F32)

    B = {k: Buf(k) for k in list(S.keys())}
    Bin = Buf("inputs")
    Bout = Buf("out")

    with ExitStack() as st:
        fw = FW(nc, st)

        _uid = [0]

        def sb(name, shape, dt, stack=st):
            _uid[0] += 1
            return stack.enter_context(nc.sbuf_tensor(f"sb{_uid[0]}_{name}", list(shape), dt))

        ps_all = st.enter_context(nc.psum_tensor("ps_all", [128, 8, 512], F32))
        PB = [Buf(f"bank{k}", excl=True) for k in range(8)]

        def bank(k):
            return ps_all[:, k, :]

        def load_const(name, key, dt, q="sp"):
            a = consts[key]
            t = sb(name, a.shape, dt)
            b = Buf(name)
            fw.dma(q, t[:], I[key], reads=[Bin], writes=[b])
            return t, b

        ident_bf, Bident = load_const("ident_bf", "ident_bf", BF16)
        anti_bf, Banti = load_const("anti_bf", "anti_bf", BF16)
        ident_f, Bidentf = load_const("ident_f", "ident_f", F32)
        onesD, BonesD = load_const("onesD", "onesD_bf", BF16)
        bd64, Bbd64 = load_const("bd64", "bd64_bf", BF16)
        ones_f, Bonesf = load_const("ones_f", "ones_f", F32)
        sel_bf, Bsel = load_const("sel_bf", "sel_bf", BF16)
        dmask, Bdmask = load_const("dmask", "dmask", F32)
        ones64, Bones64 = load_const("ones64", "ones64_bf", BF16)
        cvec = sb("cvec", [128, 8, 2], F32)
        Bcvec = Buf("cvec")
        fw.dma("sp", cvec[:], I["cvec"], reads=[Bin], writes=[Bcvec])
        scb = sb("scb", [128, 8, 2], BF16)
        Bscb = Buf("scb")
        fw.op("act", lambda e: e.activation(out=scb[:], in_=cvec[:], func=AF.Silu), reads=[Bcvec], writes=[Bscb])
        gfin = sb("gfin", [128, 8], F32)
        Bgfin = Buf("gfin")
        fw.dma("sp", gfin[:], I["g_final_l"], reads=[Bin], writes=[Bgfin])

        if stop == "consts":
            fw.finish()
            return nc, list(I.keys())
        TBLK = [(0, NCX, 1)] + [(NCX + 512 * i, 512, 0) for i in range(SEQ // 512)]

        with ExitStack() as ph:
            xt = [sb(f"xt{i}", [128, D], F32, ph) for i in range(2)]
            Bxt = [Buf() for _ in range(2)]
            hst = [sb(f"hst{i}", [128, NJ, 512], F32, ph) for i in range(2)]
            Bhst = [Buf() for _ in range(2)]
            k = 0
            import os
            for bi, (t0, n, seg) in enumerate(TBLK[:int(os.environ.get('DEV_NBLK', '99'))]):
                hs, Bh = hst[bi % 2], Bhst[bi % 2]
                for tt in range(n // 128):
                    src = I["ctx"][tt * 128:(tt + 1) * 128, :] if seg else I["x"][t0 - NCX + tt * 128: t0 - NCX + (tt + 1) * 128, :]
                    x_, Bx_ = xt[k % 2], Bxt[k % 2]
                    fw.dma("sp", x_[:], src, reads=[Bin], writes=[Bx_])
                    for half in range(2):
                        pbk = 2 * (k % 2) + half
                        for jj in range(4):
                            j = half * 4 + jj
                            fw.op("pe", lambda e: e.matmul(ps_all[:, pbk, jj * 128:(jj + 1) * 128], lhsT=x_[:, j * 128:(j + 1) * 128], rhs=ident_f[:], start=True, stop=True),
                                  reads=[Bx_, Bidentf], writes=[PB[pbk]], inc=(jj == 3))
                        eng = "dve" if half == 0 else "act"
                        src_ps = ps_all[:, pbk, :].rearrange("p (j t) -> p j t", j=4)
                        dst = hs[:, half * 4:(half + 1) * 4, tt * 128:(tt + 1) * 128]
                        if eng == "dve":
                            fw.op("dve", lambda e: e.tensor_copy(out=dst, in_=src_ps), reads=[PB[pbk]], writes=[Bh])
                        else:
                            fw.op("act", lambda e: e.copy(out=dst, in_=src_ps), reads=[PB[pbk]], writes=[Bh])
                    k += 1
                fw.dma("sp", S["hT"][:, :, t0:t0 + n], hs[:, :, :n], reads=[Bh], writes=[B["hT"]])
            fw.barrier()

        if stop == "init":
            fw.finish()
            return nc, list(I.keys())

        def mm_group(out_ap, pairs, reads, wbuf, skip=False):
            n = len(pairs)
            for k, (l, r) in enumerate(pairs):
                fw.op("pe", lambda e: e.matmul(out_ap, lhsT=l, rhs=r, start=(k == 0), stop=(k == n - 1)),
                      reads=reads, writes=[wbuf], same=False, inc=(k == n - 1))

        def rstd_from_ps(ps_ap, rt_ap, Brt, PBk):
            fw.op("act", lambda e: e.activation(out=rt_ap, in_=ps_ap, func=AF.Sqrt, bias=EPS, scale=1.0), reads=[PBk], writes=[Brt])
            fw.op("dve", lambda e: e.reciprocal(out=rt_ap, in_=rt_ap), reads=[Brt], writes=[Brt])

        modsb = sb("modsb", [128, 48, 2], F32)
        Bmod = Buf("mod")
        prm = sb("prm", [128, 6, 8, 2], F32)
        Bprm = Buf("prm")
        gmix = sb("gmix", [128, 8], F32)
        gffn = sb("gffn", [128, 8], F32)
        bada = sb("bada", [128, 48], F32)
        Bsmall = Buf("small")
        gqk = sb("gqk", [128, 4], F32)
        lamt = sb("lamt", [128, 4], F32)
        Blam = Buf("lam")
        dl = sb("dl", [1, 4, 32], F32)
        dl2 = sb("dl2", [1, 8], F32)

        def layer_setup(i, ph):
            fw.dma("sp", gmix[:], I["g_mix_l"][i], reads=[Bin], writes=[Bsmall])
            fw.dma("sp", gffn[:], I["g_ffn_l"][i], reads=[Bin], writes=[Bsmall])
            fw.dma("sp", bada[:], I["b_ada_l"][i], reads=[Bin], writes=[Bsmall])
            fw.dma("sp", gqk[:, 0:2], I["gq_l"][i], reads=[Bin], writes=[Bsmall])
            fw.dma("sp", gqk[:, 2:4], I["gk_l"][i], reads=[Bin], writes=[Bsmall])
            fw.dma("sp", dl[:], I["diff_l"][i], reads=[Bin], writes=[Bsmall])
            fw.dma("sp", lamt[:, 1:2], I["subln_l"][i], reads=[Bin], writes=[Blam])
            wa = [sb(f"wada{k}", [128, 8, 1024], BF16, ph) for k in range(2)]
            Bwa = [Buf() for _ in range(2)]
            wsrc = I["w_ada"][i].rearrange("(j p) n -> p j n", p=128)
            for sidx in range(6):
                w_, Bw_ = wa[sidx % 2], Bwa[sidx % 2]
                fw.dma("pool", w_[:], wsrc[:, :, sidx * 1024:(sidx + 1) * 1024], reads=[Bin], writes=[Bw_])
                for jj in range(8):
                    col = (sidx * 8 + jj) * 2
                    mm_group(ps_all[:, 0, col:col + 2], [(w_[:, j, jj * 128:(jj + 1) * 128], scb[:, j, :]) for j in range(8)],
                             [Bw_, Bscb], PB[0])
            for n_ in range(2):
                fw.op("dve", lambda e: e.tensor_tensor(out=modsb[:, :, n_], in0=ps_all[:, 0, 0:96].rearrange("p (a b) -> p a b", b=2)[:, :, n_],
                                                       in1=bada[:], op=ALU.add), reads=[PB[0], Bsmall], writes=[Bmod])
            for (dst, g_, sc0, sh0, gt0) in ((0, gmix, 8, 0, 16), (3, gffn, 32, 24, 40)):
                for n_ in range(2):
                    fw.op("dve", lambda e: e.scalar_tensor_tensor(out=prm[:, dst, :, n_], in0=modsb[:, sc0:sc0 + 8, n_], scalar=1.0, in1=g_[:],
                                                                  op0=ALU.add, op1=ALU.mult), reads=[Bmod, Bsmall], writes=[Bprm])
                    fw.op("dve", lambda e: e.tensor_copy(out=prm[:, dst + 1, :, n_], in_=modsb[:, sh0:sh0 + 8, n_]), reads=[Bmod], writes=[Bprm])
                    fw.op("dve", lambda e: e.tensor_copy(out=prm[:, dst + 2, :, n_], in_=modsb[:, gt0:gt0 + 8, n_]), reads=[Bmod], writes=[Bprm])
            lam_init = 0.8 - 0.6 * math.exp(-0.3 * i)
            fw.op("dve", lambda e: e.tensor_tensor(out=dl[:, 0, :], in0=dl[:, 0, :], in1=dl[:, 1, :], op=ALU.mult), reads=[Bsmall], writes=[Bsmall])
            fw.op("dve", lambda e: e.tensor_tensor(out=dl[:, 2, :], in0=dl[:, 2, :], in1=dl[:, 3, :], op=ALU.mult), reads=[Bsmall], writes=[Bsmall])
            fw.op("dve", lambda e: e.reduce_sum(out=dl2[:, 0:1], in_=dl[:, 0, :], axis=AX.X), reads=[Bsmall], writes=[Bsmall])
            fw.op("dve", lambda e: e.reduce_sum(out=dl2[:, 1:2], in_=dl[:, 2, :], axis=AX.X), reads=[Bsmall], writes=[Bsmall])
            fw.op("act", lambda e: e.activation(out=dl2[:, 2:4], in_=dl2[:, 0:2], func=AF.Exp), reads=[Bsmall], writes=[Bsmall])
            fw.op("dve", lambda e: e.tensor_tensor(out=dl2[:, 4:5], in0=dl2[:, 3:4], in1=dl2[:, 2:3], op=ALU.subtract), reads=[Bsmall], writes=[Bsmall])
            fw.op("dve", lambda e: e.tensor_scalar(out=dl2[:, 4:5], in0=dl2[:, 4:5], scalar1=-lam_init, scalar2=None, op0=ALU.add), reads=[Bsmall], writes=[Bsmall])
            fw.op("pe", lambda e: e.matmul(ps_all[:, 1, 0:1], lhsT=ones_f[0:1, :], rhs=dl2[:, 4:5], start=True, stop=True),
                  reads=[Bsmall, Bonesf], writes=[PB[1]])
            fw.op("dve", lambda e: e.tensor_copy(out=lamt[:, 0:1], in_=ps_all[:, 1, 0:1]), reads=[PB[1]], writes=[Blam])
            fw.op("dve", lambda e: e.tensor_scalar(out=lamt[:, 1:2], in0=lamt[:, 1:2], scalar1=(1.0 - lam_init), scalar2=None, op0=ALU.mult), reads=[Blam], writes=[Blam])

        def norm_block(hblk, Bh, n, seg, a_idx, sq, Bsq, rt, Brt, tmp, Btmp, outs):
            fw.op("act", lambda e: e.activation(out=sq[:, :, :n], in_=hblk[:, :, :n], func=AF.Square), reads=[Bh], writes=[Bsq])
            mm_group(ps_all[:, 0, :n], [(onesD[:], sq[:, j, :n]) for j in range(8)], [BonesD, Bsq], PB[0])
            rstd_from_ps(ps_all[:, 0, :n], rt[:, :n], Brt, PB[0])
            for j in range(8):
                fw.op("dve", lambda e: e.tensor_tensor(out=tmp[:, :n], in0=hblk[:, j, :n], in1=rt[:, :n], op=ALU.mult), reads=[Bh, Brt], writes=[Btmp])
                for (ot, Bo_) in outs:
                    fw.op("dve", lambda e: e.tensor_scalar(out=ot[:, j, :n], in0=tmp[:, :n], scalar1=prm[:, a_idx, j, seg:seg + 1],
                                                           scalar2=prm[:, a_idx + 1, j, seg:seg + 1], op0=ALU.mult, op1=ALU.add),
                          reads=[Btmp, Bprm], writes=[Bo_])

        for li in range(depth):
            moe = (li % 2 == 1)
            last = (li == DEPTH - 1)
            with ExitStack() as kv:
                kd1 = sb("kd1", [128, 2, NT], BF16, kv)
                kd2 = sb("kd2", [128, 2, NT], BF16, kv)
                kgA = sb("kgA", [128, NT], BF16, kv)
                kgB = sb("kgB", [128, NT], BF16, kv)
                vd = sb("vd", [128, NT // 128, 4, 65], BF16, kv)
                vg = sb("vg", [128, NT // 128, 2, 65], BF16, kv)
                Bkd1, Bkd2, BkgA, BkgB, Bvd, Bvg = (Buf() for _ in range(6))
                fw.op("pool", lambda e: e.memset(vd[:, :, :, 64:65], 1.0), writes=[Bvd])
                fw.op("pool", lambda e: e.memset(vg[:, :, :, 64:65], 1.0), writes=[Bvg])
                with ExitStack() as ph:
                    layer_setup(li, ph)
                    fw.barrier()
                if stop == "setup":
                    if debug:
                        fw.dma("sp", S["dbg_kv"][:, 0, 0:96], modsb[:].rearrange("p a b -> p (a b)"), reads=[Bmod], writes=[Bout])
                        fw.dma("sp", S["dbg_kv"][:, 1, 0:96], prm[:].rearrange("p a b c -> p (a b c)"), reads=[Bprm], writes=[Bout])
                        fw.dma("sp", S["dbg_kv"][:, 2, 0:4], lamt[:], reads=[Blam], writes=[Bout])
                    fw.finish()
                    return nc, list(I.keys())

                with ExitStack() as ph:
                    win = sb("win", [128, 8, IN_COLS], BF16, ph)
                    Bwin = Buf("win")
                    wsrc = I["w_in"][li].rearrange("(j p) n -> p j n", p=128)
                    for c0 in range(0, IN_COLS, 768):
                        fw.dma("pool", win[:, :, c0:c0 + 768], wsrc[:, :, c0:c0 + 768], reads=[Bin], writes=[Bwin])
                    wx = sb("wx", [128, 8, 11 * 128], BF16, ph)
                    Bwx = Buf("wx")

                    def perm_copy(dst_c, src_col, hd, swap_heads=False, swap_halves=True):
                        half = hd // 2
                        ng = 128 // hd
                        for g in range(ng):
                            gs = (ng - 1 - g) if swap_heads else g
                            for h in range(2):
                                hs_ = (1 - h) if swap_halves else h
                                d0 = dst_c * 128 + g * hd + h * half
                                s0 = src_col + gs * hd + hs_ * half
                                fw.op("pool", lambda e: e.tensor_copy(out=wx[:, :, d0:d0 + half], in_=win[:, :, s0:s0 + half]),
                                      reads=[Bwin], writes=[Bwx])
                    for c in range(2):
                        perm_copy(0 + c, OFF_DQ + 128 * c, 32)
                        perm_copy(2 + c, OFF_DK + 128 * c, 32)
                    for c in range(4):
                        perm_copy(4 + c, OFF_GQ + 128 * c, 64)
                    perm_copy(8, OFF_GK, 64)
                    perm_copy(9, OFF_GK, 64, swap_heads=True, swap_halves=False)
                    perm_copy(10, OFF_GK, 64, swap_heads=True, swap_halves=True)

                    hblk = sb("hblk", [128, 8, 512], F32, ph)
                    Bhblk = Buf()
                    sq = sb("sq", [128, 8, 512], BF16, ph)
                    Bsq = Buf()
                    ub = sb("ub", [128, 8, 512], BF16, ph)
                    Bub = Buf()
                    rt = sb("rt", [128, 512], F32, ph)
                    Brt = Buf()
                    tmp = sb("tmp", [128, 512], F32, ph)
                    Btmp = Buf()
                    tabs = sb("tabs", [128, 4, 512], F32, ph)
                    Btabs = Buf()
                    t1 = sb("t1", [128, 512], F32, ph)
                    t2 = sb("t2", [128, 512], F32, ph)
                    t3 = sb("t3", [128, 512], F32, ph)
                    Bt1, Bt2, Bt3 = Buf(), Buf(), Buf()
                    rt2 = sb("rt2", [128, 512], F32, ph)
                    Brt2 = Buf()
                    sq2 = sb("sq2", [128, 512], BF16, ph)
                    Bsq2 = Buf()
                    zst = sb("zst", [128, 6, 512], BF16, ph)
                    Bzst = Buf()
                    qst = sb("qst", [128, 6, 512], BF16, ph)
                    Bqst = Buf()

                    def proj(pbk, wtile_fn, n):
                        mm_group(ps_all[:, pbk, :n], [(wtile_fn(j), ub[:, j, :n]) for j in range(8)], [Bwin, Bwx, Bub], PB[pbk])

                    import os
                    PARTS = os.environ.get('DEV_PARTS', 'nhdgv')
                    for (t0, n, seg) in TBLK[:int(os.environ.get('DEV_NBLK', '99'))]:
                        fw.dma("sp", hblk[:, :, :n], S["hT"][:, :, t0:t0 + n], reads=[B["hT"]], writes=[Bhblk])
                        for ti, key in enumerate(("cos_g", "sin_g", "cos_d", "sin_d")):
                            fw.dma("sp", tabs[:, ti, :n], I[key][:, t0:t0 + n], reads=[Bin], writes=[Btabs])
                        norm_block(hblk, Bhblk, n, seg, 0, sq, Bsq, rt, Brt, tmp, Btmp, [(ub, Bub)])
                        for c in range(6 if 'h' in PARTS else 0):
                            pbk = 2 + (c % 2)
                            proj(pbk, lambda j: win[:, j, c * 128:(c + 1) * 128], n)
                            if c % 2 == 0:
                                fw.op("act", lambda e: e.copy(out=zst[:, c, :n], in_=ps_all[:, pbk, :n]), reads=[PB[pbk]], writes=[Bzst])
                            else:
                                fw.op("dve", lambda e: e.tensor_copy(out=zst[:, c, :n], in_=ps_all[:, pbk, :n]), reads=[PB[pbk]], writes=[Bzst])
                        fw.dma("sp", S["zhy"][:, :, t0:t0 + n], zst[:, :, :n], reads=[Bzst], writes=[B["zhy"]])
                        for c in range(4 if 'd' in PARTS else 0):
                            isk = c >= 2
                            col = (OFF_DK if isk else OFF_DQ) + 128 * (c % 2)
                            xc = (2 if isk else 0) + (c % 2)
                            proj(4, lambda j: win[:, j, col:col + 128], n)
                            proj(5, lambda j: wx[:, j, xc * 128:(xc + 1) * 128], n)
                            fw.op("dve", lambda e: e.tensor_tensor(out=t1[:, :n], in0=ps_all[:, 4, :n], in1=tabs[:, 2, :n], op=ALU.mult), reads=[PB[4], Btabs], writes=[Bt1])
                            fw.op("dve", lambda e: e.tensor_tensor(out=t2[:, :n], in0=ps_all[:, 5, :n], in1=tabs[:, 3, :n], op=ALU.mult), reads=[PB[5], Btabs], writes=[Bt2])
                            if not isk:
                                fw.op("dve", lambda e: e.tensor_tensor(out=qst[:, c, :n], in0=t1[:, :n], in1=t2[:, :n], op=ALU.add), reads=[Bt1, Bt2], writes=[Bqst])
                            else:
                                fw.op("dve", lambda e: e.tensor_tensor(out=t3[:, :n], in0=t1[:, :n], in1=t2[:, :n], op=ALU.add), reads=[Bt1, Bt2], writes=[Bt3])
                                fw.op("dve", lambda e: e.tensor_scalar(out=kd1[:, c - 2, t0:t0 + n], in0=t3[:, :n], scalar1=dmask[:, 0:1], scalar2=None, op0=ALU.mult),
                                      reads=[Bt3, Bdmask], writes=[Bkd1])
                                fw.op("dve", lambda e: e.tensor_scalar(out=kd2[:, c - 2, t0:t0 + n], in0=t3[:, :n], scalar1=dmask[:, 1:2], scalar2=None, op0=ALU.mult),
                                      reads=[Bt3, Bdmask], writes=[Bkd2])
                        for c in range(6 if 'g' in PARTS else 0):
                            if c < 4:
                                wa_ = lambda j: win[:, j, OFF_GQ + c * 128: OFF_GQ + (c + 1) * 128]
                                wb_ = lambda j: wx[:, j, (4 + c) * 128:(5 + c) * 128]
                                g0 = 0
                            elif c == 4:
                                wa_ = lambda j: win[:, j, OFF_GK:OFF_GK + 128]
                                wb_ = lambda j: wx[:, j, 8 * 128:9 * 128]
                                g0 = 2
                            else:
                                wa_ = lambda j: wx[:, j, 9 * 128:10 * 128]
                                wb_ = lambda j: wx[:, j, 10 * 128:11 * 128]
                                g0 = 2
                            proj(4, wa_, n)
                            proj(5, wb_, n)
                            fw.op("act", lambda e: e.activation(out=sq2[:, :n], in_=ps_all[:, 4, :n], func=AF.Square), reads=[PB[4]], writes=[Bsq2])
                            fw.op("pe", lambda e: e.matmul(ps_all[:, 6, :n], lhsT=bd64[:], rhs=sq2[:, :n], start=True, stop=True), reads=[Bbd64, Bsq2], writes=[PB[6]])
                            rstd_from_ps(ps_all[:, 6, :n], rt2[:, :n], Brt2, PB[6])
                            fw.op("act", lambda e: e.activation(out=t1[:, :n], in_=ps_all[:, 4, :n], func=AF.Copy, scale=gqk[:, g0:g0 + 1]), reads=[PB[4], Bsmall], writes=[Bt1])
                            fw.op("act", lambda e: e.activation(out=t2[:, :n], in_=ps_all[:, 5, :n], func=AF.Copy, scale=gqk[:, g0 + 1:g0 + 2]), reads=[PB[5], Bsmall], writes=[Bt2])
                            fw.op("dve", lambda e: e.tensor_tensor(out=t1[:, :n], in0=t1[:, :n], in1=tabs[:, 0, :n], op=ALU.mult), reads=[Bt1, Btabs], writes=[Bt1])
                            fw.op("dve", lambda e: e.tensor_tensor(out=t2[:, :n], in0=t2[:, :n], in1=tabs[:, 1, :n], op=ALU.mult), reads=[Bt2, Btabs], writes=[Bt2])
                            fw.op("dve", lambda e: e.tensor_tensor(out=t3[:, :n], in0=t1[:, :n], in1=t2[:, :n], op=ALU.add), reads=[Bt1, Bt2], writes=[Bt3])
                            if c < 4:
                                dst, Bd = qst[:, 2 + c, :n], Bqst
                            elif c == 4:
                                dst, Bd = kgA[:, t0:t0 + n], BkgA
                            else:
                                dst, Bd = kgB[:, t0:t0 + n], BkgB
                            fw.op("dve", lambda e: e.tensor_tensor(out=dst, in0=t3[:, :n], in1=rt2[:, :n], op=ALU.mult), reads=[Bt3, Brt2], writes=[Bd])
                        fw.dma("sp", S["qd"][:, :, t0:t0 + n], qst[:, 0:2, :n], reads=[Bqst], writes=[B["qd"]])
                        fw.dma("sp", S["qg"][:, :, t0:t0 + n], qst[:, 2:6, :n], reads=[Bqst], writes=[B["qg"]])
                        for tt in range(n // 128 if 'v' in PARTS else 0):
                            kt = t0 // 128 + tt
                            pbk = 2 + (tt % 2)
                            if 'x' not in PARTS:
                                mm_group(ps_all[:, pbk, 0:256], [(ub[:, j, tt * 128:(tt + 1) * 128], win[:, j, OFF_DV:OFF_DV + 256]) for j in range(8)], [Bub, Bwin], PB[pbk])
                            if 'y' not in PARTS:
                                mm_group(ps_all[:, pbk, 256:384], [(ub[:, j, tt * 128:(tt + 1) * 128], win[:, j, OFF_GV:OFF_GV + 128]) for j in range(8)], [Bub, Bwin], PB[pbk])
                            if 'x' not in PARTS and 'z' not in PARTS:
                                fw.op("act", lambda e: e.copy(out=vd[:, kt, :, 0:64], in_=ps_all[:, pbk, 0:256].rearrange("p (h d) -> p h d", h=4)), reads=[PB[pbk]], writes=[Bvd])
                            if 'y' not in PARTS and 'z' not in PARTS:
                                fw.op("dve", lambda e: e.tensor_copy(out=vg[:, kt, :, 0:64], in_=ps_all[:, pbk, 256:384].rearrange("p (h d) -> p h d", h=2)), reads=[PB[pbk]], writes=[Bvg])
                    fw.barrier()
                if stop == "p1":
                    if debug:
                        with ExitStack() as ph:
                            dbg = sb("dbg", [128, 8840], F32, ph)
                            Bdbg = Buf()
                            for k_, (src_, Bs_) in enumerate(((kd1[:, 0, :], Bkd1), (kd1[:, 1, :], Bkd1), (kd2[:, 0, :], Bkd2), (kd2[:, 1, :], Bkd2), (kgA[:], BkgA), (kgB[:], BkgB))):
                                fw.op("dve", lambda e: e.tensor_copy(out=dbg[:, 0:NT], in_=src_), reads=[Bs_], writes=[Bdbg])
                                fw.dma("sp", S["dbg_kv"][:, k_, :], dbg[:, 0:NT], reads=[Bdbg], writes=[Bout])
                            fw.op("dve", lambda e: e.tensor_copy(out=dbg[:, 0:34 * 4 * 65], in_=vd[:].rearrange("p a b c -> p (a b c)")), reads=[Bvd], writes=[Bdbg])
                            fw.dma("sp", S["dbg_kv"][:, 6:9, :].rearrange("p a b -> p (a b)")[:, 0:8840], dbg[:, 0:8840], reads=[Bdbg], writes=[Bout])
                            fw.op("dve", lambda e: e.tensor_copy(out=dbg[:, 0:34 * 2 * 65], in_=vg[:].rearrange("p a b c -> p (a b c)")), reads=[Bvg], writes=[Bdbg])
                            fw.dma("sp", S["dbg_kv"][:, 9:11, :].rearrange("p a b -> p (a b)")[:, 0:4420], dbg[:, 0:4420], reads=[Bdbg], writes=[Bout])
                    fw.finish()
                    return nc, list(I.keys())

                with ExitStack() as ph:
                    qdb = [sb(f"qdb{k}", [128, 2, 512], BF16, ph) for k in range(2)]
                    qgb = [sb(f"qgb{k}", [128, 4, 512], BF16, ph) for k in range(2)]
                    Bqb = [Buf() for _ in range(2)]
                    pT = [sb(f"pT{k}", [128, 2, 512], BF16, ph) for k in range(3)]
                    BpT = [Buf() for _ in range(3)]
                    rec = [sb(f"rec{k}", [128, 512], F32, ph) for k in range(2)]
                    Brec = [Buf() for _ in range(2)]
                    tm = [sb(f"tm{k}", [128, 512], F32, ph) for k in range(2)]
                    Btm = [Buf() for _ in range(2)]
                    od = sb("od", [128, 512], F32, ph)
                    Bod = Buf()
                    sqd = sb("sqd", [128, 512], BF16, ph)
                    Bsqd = Buf()
                    rtd = sb("rtd", [128, 512], F32, ph)
                    Brtd = Buf()
                    of = [sb(f"of{k}", [128, 512], BF16, ph) for k in range(2)]
                    Bof = [Buf() for _ in range(2)]
                    cnt = {"pt": 0, "of": 0, "sp": 0, "pair": 0, "rec": 0}
                    deferred = []

                    def run_deferred(step=None):
                        while deferred and (step is None or deferred[0][0] <= step):
                            deferred.pop(0)[1]()

                    def attn_pair(kA_fn, kB_fn, q_t, ch, vA_fn, vB_fn, tiles, nq, scale):
                        pr = cnt["pair"] % 2
                        cnt["pair"] += 1
                        ob, db = (6, 7) if pr == 0 else (0, 1)
                        slots = []

                        def qk(i):
                            kt = tiles[i]
                            s_ = cnt["sp"] % 2
                            cnt["sp"] += 1
                            slots.append(s_)
                            fw.op("pe", lambda e: e.matmul(ps_all[:, 2 + 2 * s_, :nq], lhsT=kA_fn(kt), rhs=q_t[0:64, ch, :nq], start=True, stop=True),
                                  reads=[Bkd1, Bkd2, BkgA, BkgB] + Bqb, writes=[PB[2 + 2 * s_]], same=False, inc=False)
                            fw.op("pe", lambda e: e.matmul(ps_all[:, 3 + 2 * s_, :nq], lhsT=kB_fn(kt), rhs=q_t[64:128, ch, :nq], start=True, stop=True),
                                  reads=[Bkd1, Bkd2, BkgA, BkgB] + Bqb, writes=[PB[3 + 2 * s_]], same=False, inc=True)
                        qk(0)
                        nk = len(tiles)
                        for i, kt in enumerate(tiles):
                            if i + 1 < nk:
                                qk(i + 1)
                            s_ = slots[i]
                            pt_i = cnt["pt"] % 3
                            cnt["pt"] += 1
                            fw.op("act", lambda e: e.activation(out=pT[pt_i][:, :, :nq], in_=ps_all[:, 2 + 2 * s_:4 + 2 * s_, :nq], func=AF.Exp, scale=scale),
                                  reads=[PB[2 + 2 * s_], PB[3 + 2 * s_]], writes=[BpT[pt_i]])
                            first, last = (i == 0), (i == nk - 1)
                            for (bk_, lA, lB, Bl) in ((ob, vA_fn(kt), vB_fn(kt), [Bvd, Bvg]), (db, ones64[:, :], ones64[:, :], [Bones64])):
                                fw.op("pe", lambda e: e.matmul(ps_all[0:64, bk_, :nq], lhsT=lA, rhs=pT[pt_i][:, 0, :nq], start=first, stop=last,
                                                               skip_group_check=True, tile_position=(0, 0)),
                                      reads=Bl + [BpT[pt_i]], writes=[PB[bk_]], same=False, inc=False)
                                fw.op("pe", lambda e: e.matmul(ps_all[64:128, bk_, :nq], lhsT=lB, rhs=pT[pt_i][:, 1, :nq], start=first, stop=last,
                                                               skip_group_check=True, tile_position=(0, 64)),
                                      reads=Bl + [BpT[pt_i]], writes=[PB[bk_]], same=False, inc=(bk_ == db))
                            run_deferred(i)
                        run_deferred()
                        return ob, db

                    def recip_den(db, nq):
                        r_i = cnt["rec"] % 2
                        cnt["rec"] += 1
                        fw.op("dve", lambda e: e.reciprocal(out=rec[r_i][:, :nq], in_=ps_all[:, db, :nq]), reads=[PB[db]], writes=[Brec[r_i]])
                        return r_i

                    QBLK = [(0, NCX, [0, 1])] + [(NCX + 512 * i, 512, list(range(NT // 128))) for i in range(SEQ // 512)]
                    import os
                    QBLK = QBLK[:int(os.environ.get('DEV_NQB', '99'))]

                    def load_q(qi):
                        q0, nq, _ = QBLK[qi]
                        fw.dma("sp", qdb[qi % 2][:, :, :nq], S["qd"][:, :, q0:q0 + nq], reads=[B["qd"]], writes=[Bqb[qi % 2]])
                        fw.dma("sp", qgb[qi % 2][:, :, :nq], S["qg"][:, :, q0:q0 + nq], reads=[B["qg"]], writes=[Bqb[qi % 2]])
                    load_q(0)
                    for qi, (q0, nq, tiles) in enumerate(QBLK):
                        if qi + 1 < len(QBLK):
                            load_q(qi + 1)
                        qd_, qg_ = qdb[qi % 2], qgb[qi % 2]
                        for ch in range(2):
                            for m_ in range(2):
                                kk = kd1 if m_ == 0 else kd2
                                ob, db = attn_pair(lambda kt: kk[0:64, ch, kt * 128:(kt + 1) * 128], lambda kt: kk[64:128, ch, kt * 128:(kt + 1) * 128],
                                                   qd_, ch, lambda kt: vd[:, kt, 2 * ch, 0:64], lambda kt: vd[:, kt, 2 * ch + 1, 0:64], tiles, nq, 32 ** -0.5)
                                r_i = recip_den(db, nq)
                                if m_ == 1:
                                    fw.op("dve", lambda e: e.tensor_scalar(out=rec[r_i][:, :nq], in0=rec[r_i][:, :nq], scalar1=lamt[:, 0:1], scalar2=None, op0=ALU.mult),
                                          reads=[Brec[r_i], Blam], writes=[Brec[r_i]])
                                fw.op("dve", lambda e: e.tensor_tensor(out=tm[m_][:, :nq], in0=ps_all[:, ob, :nq], in1=rec[r_i][:, :nq], op=ALU.mult),
                                      reads=[PB[ob], Brec[r_i]], writes=[Btm[m_]])
                            fw.op("dve", lambda e: e.tensor_tensor(out=od[:, :nq], in0=tm[0][:, :nq], in1=tm[1][:, :nq], op=ALU.add), reads=[Btm[0], Btm[1]], writes=[Bod])

                            def stB(nq=nq):
                                fw.op("act", lambda e: e.activation(out=sqd[:, :nq], in_=od[:, :nq], func=AF.Square), reads=[Bod], writes=[Bsqd])

                            def stC(nq=nq, db=db):
                                fw.op("pe", lambda e: e.matmul(ps_all[:, db, :nq], lhsT=bd64[:], rhs=sqd[:, :nq], start=True, stop=True), reads=[Bsqd, Bbd64], writes=[PB[db]])

                            def stD(nq=nq, db=db, ch=ch, q0=q0):
                                rstd_from_ps(ps_all[:, db, :nq], rtd[:, :nq], Brtd, PB[db])
                                fw.op("dve", lambda e: e.tensor_tensor(out=od[:, :nq], in0=od[:, :nq], in1=rtd[:, :nq], op=ALU.mult), reads=[Bod, Brtd], writes=[Bod])
                                oi = cnt["of"] % 2
                                cnt["of"] += 1
                                fw.op("dve", lambda e: e.tensor_scalar(out=of[oi][:, :nq], in0=od[:, :nq], scalar1=lamt[:, 1:2], scalar2=None, op0=ALU.mult), reads=[Bod, Blam], writes=[Bof[oi]])
                                fw.dma("sp", S["mg"][:, 2 + ch, q0:q0 + nq], of[oi][:, :nq], reads=[Bof[oi]], writes=[B["mg"]])
                            deferred.extend([[3, stB], [5, stC], [8, stD]])
                        for ch in range(4):
                            g = ch // 2
                            kA = kgA if g == 0 else kgB
                            kB = kgA if g == 1 else kgB
                            ob, db = attn_pair(lambda kt: kA[0:64, kt * 128:(kt + 1) * 128], lambda kt: kB[64:128, kt * 128:(kt + 1) * 128],
                                               qg_, ch, lambda kt: vg[:, kt, g, 0:64], lambda kt: vg[:, kt, g, 0:64], tiles, nq, 64 ** -0.5)
                            r_i = recip_den(db, nq)
                            oi = cnt["of"] % 2
                            cnt["of"] += 1
                            fw.op("dve", lambda e: e.tensor_tensor(out=of[oi][:, :nq], in0=ps_all[:, ob, :nq], in1=rec[r_i][:, :nq], op=ALU.mult),
                                  reads=[PB[ob], Brec[r_i]], writes=[Bof[oi]])
                            fw.dma("sp", S["mg"][:, 4 + ch, q0:q0 + nq], of[oi][:, :nq], reads=[Bof[oi]], writes=[B["mg"]])
                    run_deferred()
                    fw.barrier()
            if stop == "p2":
                fw.finish()
                return nc, list(I.keys())

            with ExitStack() as ph:
                x0c = sb("x0c", [128, 2, NT], BF16, ph)
                v1 = sb("v1", [128, 2, NT], BF16, ph)
                Bx0c, Bv1 = Buf(), Buf()
                hsk = sb("hsk", [128, 2], F32, ph)
                Bhsk = Buf()
                fw.dma("sp", hsk[:], I["hy_skip_l"][li], reads=[Bin], writes=[Bhsk])
                def p3a_gen(lj, p3, bsin, bout):
                    w1 = sb("hw1", [33, 64], F32, p3)
                    w2 = sb("hw2", [64, 64], F32, p3)
                    w3 = sb("hw3", [64, 512], F32, p3)
                    hb = sb("hb", [64, 4], F32, p3)
                    Bhw = Buf()
                    fw.dma("sp", w1[:], I["hy_w1"][lj], reads=[Bin], writes=[Bhw])
                    fw.dma("sp", w2[:], I["hy_w2"][lj], reads=[Bin], writes=[Bhw])
                    fw.dma("sp", w3[:], I["hy_w3"][lj], reads=[Bin], writes=[Bhw])
                    fw.dma("sp", hb[:, 0:1], I["hy_b1_l"][lj], reads=[Bin], writes=[Bhw])
                    fw.dma("sp", hb[:, 1:2], I["hy_b2_l"][lj], reads=[Bin], writes=[Bhw])
                    fw.dma("sp", hb[:, 2:3], I["hy_fr_l"][lj], reads=[Bin], writes=[Bhw])
                    fw.op("dve", lambda e: e.tensor_scalar(out=hb[:, 0:2], in0=hb[:, 0:2], scalar1=hb[:, 2:3], scalar2=None, op0=ALU.mult), reads=[Bhw], writes=[Bhw])
                    ft = sb("ft", [33, 512], F32, p3)
                    dec = sb("dec", [128, 2, 512], F32, p3)
                    Bft, Bdec = Buf(), Buf()
                    pre = sb("pre", [64, 512], F32, p3)
                    mk = sb("mk", [64, 512], F32, p3)
                    act1 = sb("act1", [64, 512], F32, p3)
                    act2 = sb("act2", [64, 512], F32, p3)
                    Bpre, Bmk, Ba1, Ba2 = Buf(), Buf(), Buf(), Buf()
                    hrow = sb("hrow", [128, 2, 512], BF16, p3)
                    Bhrow = Buf()

                    def sin_layer(ps_ap, bcol, out_t, Bout_, PBk, n):
                        fw.op("dve", lambda e: e.tensor_scalar(out=pre[:, :n], in0=ps_ap, scalar1=hb[:, 2:3], scalar2=hb[:, bcol:bcol + 1], op0=ALU.mult, op1=ALU.add),
                              reads=[PBk, Bhw], writes=[Bpre])
                        fw.op("dve", lambda e: e.tensor_scalar(out=mk[:, :n], in0=pre[:, :n], scalar1=PI, scalar2=None, op0=ALU.is_gt), reads=[Bpre], writes=[Bmk])
                        fw.op("dve", lambda e: e.scalar_tensor_tensor(out=pre[:, :n], in0=mk[:, :n], scalar=-2 * PI, in1=pre[:, :n], op0=ALU.mult, op1=ALU.add), reads=[Bmk, Bpre], writes=[Bpre])
                        fw.op("dve", lambda e: e.tensor_scalar(out=mk[:, :n], in0=pre[:, :n], scalar1=-PI, scalar2=None, op0=ALU.is_lt), reads=[Bpre], writes=[Bmk])
                        fw.op("dve", lambda e: e.scalar_tensor_tensor(out=pre[:, :n], in0=mk[:, :n], scalar=2 * PI, in1=pre[:, :n], op0=ALU.mult, op1=ALU.add), reads=[Bmk, Bpre], writes=[Bpre])
                        fw.op("act", lambda e: e.activation(out=out_t[:, :n], in_=pre[:, :n], func=AF.Sin), reads=[Bpre], writes=[Bout_])

                    hs_ = lj % 2
                    for (L, kf, kfr, kd_, kdr, hkey) in ((SEQ, "featsT", "featsTr", "decT", "decTr", f"hrev{hs_}"), (NCX, "featsTc", "featsTcr", "decTc", "decTcr", f"hrevc{hs_}")):
                        hdst = S[hkey].rearrange("(c p) x -> p c x", p=128)
                        for direction in range(2):
                            fkey, dkey = (kfr, kdr) if direction == 0 else (kf, kd_)
                            colb = 0 if direction == 0 else 256
                            for p0 in range(0, L, 512):
                                n = min(512, L - p0)
                                fw.dma("sp", ft[:, :n], I[fkey][:, p0:p0 + n], reads=[Bin], writes=[Bft])
                                fw.dma("sp", dec[:, :, :n], I[dkey][:, :, p0:p0 + n], reads=[Bin], writes=[Bdec])
                                fw.op("pe", lambda e: e.matmul(ps_all[0:64, bsin[0], :n], lhsT=w1[:, :], rhs=ft[:, :n], start=True, stop=True), reads=[Bhw, Bft], writes=[PB[bsin[0]]])
                                sin_layer(ps_all[0:64, bsin[0], :n], 0, act1, Ba1, PB[bsin[0]], n)
                                yield
                                fw.op("pe", lambda e: e.matmul(ps_all[0:64, bsin[1], :n], lhsT=w2[:, :], rhs=act1[:, :n], start=True, stop=True), reads=[Bhw, Ba1], writes=[PB[bsin[1]]])
                                sin_layer(ps_all[0:64, bsin[1], :n], 1, act2, Ba2, PB[bsin[1]], n)
                                yield
                                for cc in range(2):
                                    fw.op("pe", lambda e: e.matmul(ps_all[:, bout[cc], :n], lhsT=w3[:, colb + cc * 128: colb + (cc + 1) * 128], rhs=act2[:, :n], start=True, stop=True),
                                          reads=[Bhw, Ba2], writes=[PB[bout[cc]]])
                                    fw.op("dve", lambda e: e.tensor_tensor(out=hrow[:, cc, :n], in0=ps_all[:, bout[cc], :n], in1=dec[:, cc, :n], op=ALU.mult), reads=[PB[bout[cc]], Bdec], writes=[Bhrow])
                                if direction == 0:
                                    fw.dma("sp", hdst[:, :, p0:p0 + n], hrow[:, :, :n], reads=[Bhrow], writes=[B[hkey]])
                                else:
                                    i0 = 1 if p0 == 0 else 0
                                    fw.dma("sp", hdst[:, :, L - 1 + p0 + i0: L - 1 + p0 + n], hrow[:, :, i0:n], reads=[Bhrow], writes=[B[hkey]])
                                yield

                if li == 0:
                    with ExitStack() as p3:
                        for _ in p3a_gen(0, p3, (0, 1), (2, 3)):
                            pass
                        fw.barrier()
                if stop == "p3a":
                    fw.finish()
                    return nc, list(I.keys())
                with ExitStack() as p3:
                    cw = sb("cw", [128, 6, 3], F32, p3)
                    cb = sb("cb", [128, 6], F32, p3)
                    Bcw = Buf()
                    fw.dma("sp", cw[:], I["hy_cw_l"][li], reads=[Bin], writes=[Bcw])
                    fw.dma("sp", cb[:], I["hy_cb_l"][li], reads=[Bin], writes=[Bcw])
                    zc = [sb(f"zc{k}", [128, NT], BF16, p3) for k in range(2)]
                    Bzc = [Buf() for _ in range(2)]
                    yA = sb("yA", [128, NT], F32, p3)
                    yB = sb("yB", [128, NT], F32, p3)
                    ByA, ByB = Buf(), Buf()
                    zi = 0
                    for cc in range(2):
                        for role, chunk in (("x1", 2 + cc), ("v", 4 + cc), ("x0", cc)):
                            z_, Bz_ = zc[zi % 2], Bzc[zi % 2]
                            zi += 1
                            fw.dma("sp", z_[:], S["zhy"][:, chunk, :], reads=[B["zhy"]], writes=[Bz_])
                            y_, By_ = (yA, ByA) if role == "x1" else (yB, ByB)
                            fw.op("dve", lambda e: e.tensor_scalar(out=y_[:], in0=z_[:], scalar1=cw[:, chunk, 1:2], scalar2=cb[:, chunk:chunk + 1], op0=ALU.mult, op1=ALU.add),
                                  reads=[Bz_, Bcw], writes=[By_])
                            for (s0, e0) in ((0, NCX), (NCX, NT)):
                                fw.op("dve", lambda e: e.scalar_tensor_tensor(out=y_[:, s0 + 1:e0], in0=z_[:, s0:e0 - 1], scalar=cw[:, chunk, 0:1], in1=y_[:, s0 + 1:e0], op0=ALU.mult, op1=ALU.add),
                                      reads=[Bz_, Bcw, By_], writes=[By_])
                                fw.op("dve", lambda e: e.scalar_tensor_tensor(out=y_[:, s0:e0 - 1], in0=z_[:, s0 + 1:e0], scalar=cw[:, chunk, 2:3], in1=y_[:, s0:e0 - 1], op0=ALU.mult, op1=ALU.add),
                                      reads=[Bz_, Bcw, By_], writes=[By_])
                            if role == "v":
                                fw.op("dve", lambda e: e.tensor_tensor(out=v1[:, cc, :], in0=yB[:], in1=yA[:], op=ALU.mult), reads=[ByA, ByB], writes=[Bv1])
                            elif role == "x0":
                                fw.op("act", lambda e: e.copy(out=x0c[:, cc, :], in_=yB[:]), reads=[ByB], writes=[Bx0c])
                    fw.barrier()
                with ExitStack() as p3:
                    NTL = NT // 128
                    Vt = sb("Vt", [128, 256, NTL], BF16, p3)
                    BVt = Buf()
                    Ysb = sb("Ysb", [128, NTL, 256], BF16, p3)
                    BYsb = Buf()
                    G = [sb(f"G{k}", [128, 8064], BF16, p3) for k in range(2)]
                    BG = [Buf() for _ in range(2)]
                    Gc = sb("Gc", [128, 16, 384], BF16, p3)
                    BGc = Buf()
                    hyo = sb("hyo", [128, 2, NT], BF16, p3)
                    Bhyo = Buf()
                    tmpf = sb("tmpf", [128, 512], F32, p3)
                    Btmpf = Buf()
                    gen3a = p3a_gen(li + 1, p3, (0, 1), (6, 7)) if li + 1 < depth else None
                    hkl, hkc = f"hrev{li % 2}", f"hrevc{li % 2}"
                    k4 = 0
                    for cc in range(2):
                        for j0 in range(0, NTL, 4):
                            nj = min(4, NTL - j0)
                            bk = k4 % 2
                            k4 += 1
                            for k in range(nj):
                                fw.op("pe", lambda e: e.matmul(ps_all[:, bk, k * 128:(k + 1) * 128], lhsT=v1[:, cc, (j0 + k) * 128:(j0 + k + 1) * 128], rhs=ident_bf[:], start=True, stop=True),
                                      reads=[Bv1, Bident], writes=[PB[bk]], same=False, inc=(k == nj - 1))
                            dstv = Vt[:, cc * 128:(cc + 1) * 128, j0:j0 + nj].rearrange("p c j -> p j c")
                            srcv = ps_all[:, bk, 0:nj * 128].rearrange("p (j c) -> p j c", j=nj)
                            if bk == 0:
                                fw.op("dve", lambda e: e.tensor_copy(out=dstv, in_=srcv), reads=[PB[bk]], writes=[BVt])
                            else:
                                fw.op("act", lambda e: e.copy(out=dstv, in_=srcv), reads=[PB[bk]], writes=[BVt])
                    import os
                    NCH = int(os.environ.get('DEV_NCH', '256'))
                    for c in range(NCH):
                        g_, Bg_ = G[c % 2], BG[c % 2]
                        src = bass.AP(S[hkl].tensor, c * 2 * SEQ, [[1, 128], [1, 8064]])
                        fw.dma("sp", g_[:], src, reads=[B[hkl]], writes=[Bg_])
                        bk = 2 + (c // 16) % 2
                        col0 = (c % 16) * 32
                        ds = [0] + [d for d in range(-31, 32) if d != 0]
                        for di, d in enumerate(ds):
                            o_d = SEQ - 128 - 128 * d
                            if d >= 0:
                                oc0, oc1, j0_ = d, 32, 0
                            else:
                                oc0, oc1, j0_ = 0, 32 + d, -d
                            nn = oc1 - oc0
                            last = (di == len(ds) - 1) and (c % 16 == 15 or c == NCH - 1)
                            fw.op("pe", lambda e: e.matmul(ps_all[:, bk, col0 + oc0:col0 + oc1], lhsT=g_[:, o_d:o_d + 128], rhs=Vt[:, c, 2 + j0_:2 + j0_ + nn],
                                                           start=(di == 0 and c % 16 == 0), stop=last, skip_group_check=True),
                                  reads=[Bg_, BVt], writes=[PB[bk]], same=False, inc=(di == len(ds) - 1))
                        if c % 16 == 15 or c == NCH - 1:
                            c0 = c - (c % 16)
                            ncg = c - c0 + 1
                            dsty = Ysb[:, 2:NTL, c0:c0 + ncg].rearrange("p i c -> p c i")
                            srcy = ps_all[:, bk, 0:ncg * 32].rearrange("p (c i) -> p c i", c=ncg)
                            if bk == 2:
                                fw.op("dve", lambda e: e.tensor_copy(out=dsty, in_=srcy), reads=[PB[bk]], writes=[BYsb])
                            else:
                                fw.op("act", lambda e: e.copy(out=dsty, in_=srcy), reads=[PB[bk]], writes=[BYsb])
                        if gen3a is not None and c % 4 == 3:
                            next(gen3a, None)
                    for c0 in range(0, min(NCH, 256), 16):
                        src = bass.AP(S[hkc].tensor, c0 * 2 * NCX, [[1, 128], [2 * NCX, 16], [1, 384]])
                        fw.dma("sp", Gc[:], src, reads=[B[hkc]], writes=[BGc])
                        bk = 4 + (c0 // 16) % 2
                        for cl in range(16):
                            c = c0 + cl
                            for di, d in enumerate((0, 1, -1)):
                                o_d = NCX - 128 - 128 * d
                                if d == 0:
                                    oc0, oc1, j0_ = 0, 2, 0
                                elif d == 1:
                                    oc0, oc1, j0_ = 1, 2, 0
                                else:
                                    oc0, oc1, j0_ = 0, 1, 1
                                nn = oc1 - oc0
                                last = (di == 2 and cl == 15)
                                fw.op("pe", lambda e: e.matmul(ps_all[:, bk, cl * 2 + oc0:cl * 2 + oc1], lhsT=Gc[:, cl, o_d:o_d + 128], rhs=Vt[:, c, j0_:j0_ + nn],
                                                               start=(di == 0 and cl == 0), stop=last, skip_group_check=True),
                                      reads=[BGc, BVt], writes=[PB[bk]], same=False, inc=last)
                        dsty = Ysb[:, 0:2, c0:c0 + 16].rearrange("p i c -> p c i")
                        srcy = ps_all[:, bk, 0:32].rearrange("p (c i) -> p c i", c=16)
                        fw.op("dve", lambda e: e.tensor_copy(out=dsty, in_=srcy), reads=[PB[bk]], writes=[BYsb])
                    if gen3a is not None:
                        for _ in gen3a:
                            pass
                    k4 = 0
                    for cc in range(2):
                        for i0 in range(0, NTL, 4):
                            ni = min(4, NTL - i0)
                            bk = 6 + k4 % 2
                            k4 += 1
                            for k in range(ni):
                                fw.op("pe", lambda e: e.matmul(ps_all[:, bk, k * 128:(k + 1) * 128], lhsT=Ysb[:, i0 + k, cc * 128:(cc + 1) * 128], rhs=anti_bf[:], start=True, stop=True),
                                      reads=[BYsb, Banti], writes=[PB[bk]], same=False, inc=(k == ni - 1))
                            tsl = slice(i0 * 128, (i0 + ni) * 128)
                            nn = ni * 128
                            fw.op("dve", lambda e: e.scalar_tensor_tensor(out=tmpf[:, :nn], in0=v1[:, cc, tsl], scalar=hsk[:, cc:cc + 1], in1=ps_all[:, bk, :nn], op0=ALU.mult, op1=ALU.add),
                                  reads=[Bv1, Bhsk, PB[bk]], writes=[Btmpf])
                            fw.op("dve", lambda e: e.tensor_tensor(out=hyo[:, cc, tsl], in0=tmpf[:, :nn], in1=x0c[:, cc, tsl], op=ALU.mult), reads=[Btmpf, Bx0c], writes=[Bhyo])
                    fw.dma("sp", S["mg"][:, 0:2, :], hyo[:], reads=[Bhyo], writes=[B["mg"]])
                    fw.barrier()
            if stop == "p3":
                fw.finish()
                return nc, list(I.keys())

            with ExitStack() as lf:
                gT = sb("gT", [8, NT], BF16, lf)
                BgT = Buf()
                with ExitStack() as ph:
                    wout = sb("wout", [128, 8, D], BF16, ph)
                    Bwout = Buf()
                    fw.dma("pool", wout[:], I["w_out"][li].rearrange("(j p) n -> p j n", p=128), reads=[Bin], writes=[Bwout])
                    mgb2 = [sb(f"mgb{k}", [128, 8, 512], BF16, ph) for k in range(2)]
                    Bmgb2 = [Buf() for _ in range(2)]
                    hblk2 = [sb(f"hblk4{k}", [128, 8, 512], F32, ph) for k in range(2)]
                    Bhblk2 = [Buf() for _ in range(2)]
                    sq = sb("sq4", [128, 8, 512], BF16, ph)
                    Bsq = Buf()
                    rt = sb("rt4", [128, 512], F32, ph)
                    Brt = Buf()
                    tmp = sb("tmp4", [128, 512], F32, ph)
                    Btmp = Buf()
                    u2b = sb("u2b", [128, 8, 512], BF16, ph)
                    Bu2b = Buf()
                    if moe:
                        u2f = sb("u2f", [128, 8, 512], F32, ph)
                        Bu2f = Buf()
                        rtr = sb("rtr", [128, 8, NEXP], F32, ph)
                        Brtr = Buf()
                        fw.dma("sp", rtr[:], I["moe_router"][li // 2].rearrange("(j p) n -> p j n", p=128), reads=[Bin], writes=[Brtr])
                        lg = sb("lg", [128, 8], F32, ph)
                        l2 = sb("l2", [128, 8], F32, ph)
                        eq1 = sb("eq1", [128, 8], F32, ph)
                        eq2 = sb("eq2", [128, 8], F32, ph)
                        gts = sb("gts", [128, 8], F32, ph)
                        mm_ = sb("mm_", [128, 8], F32, ph)
                        Brt_ = Buf()
                    for bi4, (t0, n, seg) in enumerate(TBLK):
                        mgb, Bmgb, hblk, Bhblk = mgb2[bi4 % 2], Bmgb2[bi4 % 2], hblk2[bi4 % 2], Bhblk2[bi4 % 2]
                        fw.dma("sp", mgb[:, :, :n], S["mg"][:, :, t0:t0 + n], reads=[B["mg"]], writes=[Bmgb])
                        fw.dma("sp", hblk[:, :, :n], S["hT"][:, :, t0:t0 + n], reads=[B["hT"]], writes=[Bhblk])
                        for dc in range(8):
                            bk = dc % 2
                            mm_group(ps_all[:, bk, :n], [(wout[:, j, dc * 128:(dc + 1) * 128], mgb[:, j, :n]) for j in range(8)], [Bwout, Bmgb], PB[bk])
                            fw.op("dve", lambda e: e.scalar_tensor_tensor(out=hblk[:, dc, :n], in0=ps_all[:, bk, :n], scalar=prm[:, 2, dc, seg:seg + 1], in1=hblk[:, dc, :n], op0=ALU.mult, op1=ALU.add),
                                  reads=[PB[bk], Bprm, Bhblk], writes=[Bhblk])
                        fw.dma("sp", S["hT"][:, :, t0:t0 + n], hblk[:, :, :n], reads=[Bhblk], writes=[B["hT"]])
                        outs = [(u2b, Bu2b)] + ([(u2f, Bu2f)] if moe else [])
                        norm_block(hblk, Bhblk, n, seg, 3, sq, Bsq, rt, Brt, tmp, Btmp, outs)
                        fw.dma("sp", S["u2"][:, :, t0:t0 + n], u2b[:, :, :n], reads=[Bu2b], writes=[B["u2"]])
                        if moe:
                            for tt in range(n // 128):
                                tok0 = t0 + tt * 128
                                mm_group(ps_all[:, 2, 0:8], [(u2f[:, j, tt * 128:(tt + 1) * 128], rtr[:, j, :]) for j in range(8)], [Bu2f, Brtr], PB[2])
                                fw.op("dve", lambda e: e.tensor_copy(out=lg[:], in_=ps_all[:, 2, 0:8]), reads=[PB[2]], writes=[Brt_])
                                fw.op("dve", lambda e: e.reduce_max(out=mm_[:, 0:1], in_=lg[:], axis=AX.X), reads=[Brt_], writes=[Brt_])
                                fw.op("dve", lambda e: e.tensor_scalar(out=eq1[:], in0=lg[:], scalar1=mm_[:, 0:1], scalar2=None, op0=ALU.is_equal), reads=[Brt_], writes=[Brt_])
                                fw.op("dve", lambda e: e.scalar_tensor_tensor(out=l2[:], in0=eq1[:], scalar=-1e30, in1=lg[:], op0=ALU.mult, op1=ALU.add), reads=[Brt_], writes=[Brt_])
                                fw.op("dve", lambda e: e.reduce_max(out=mm_[:, 1:2], in_=l2[:], axis=AX.X), reads=[Brt_], writes=[Brt_])
                                fw.op("dve", lambda e: e.tensor_scalar(out=eq2[:], in0=l2[:], scalar1=mm_[:, 1:2], scalar2=None, op0=ALU.is_equal), reads=[Brt_], writes=[Brt_])
                                fw.op("dve", lambda e: e.tensor_tensor(out=mm_[:, 2:3], in0=mm_[:, 1:2], in1=mm_[:, 0:1], op=ALU.subtract), reads=[Brt_], writes=[Brt_])
                                fw.op("act", lambda e: e.activation(out=mm_[:, 3:4], in_=mm_[:, 2:3], func=AF.Exp), reads=[Brt_], writes=[Brt_])
                                fw.op("dve", lambda e: e.tensor_scalar(out=mm_[:, 4:5], in0=mm_[:, 3:4], scalar1=1.0, scalar2=None, op0=ALU.add), reads=[Brt_], writes=[Brt_])
                                fw.op("dve", lambda e: e.reciprocal(out=mm_[:, 5:6], in_=mm_[:, 4:5]), reads=[Brt_], writes=[Brt_])
                                fw.op("dve", lambda e: e.tensor_tensor(out=mm_[:, 6:7], in0=mm_[:, 3:4], in1=mm_[:, 5:6], op=ALU.mult), reads=[Brt_], writes=[Brt_])
                                fw.op("dve", lambda e: e.tensor_scalar(out=gts[:], in0=eq1[:], scalar1=mm_[:, 5:6], scalar2=None, op0=ALU.mult), reads=[Brt_], writes=[Brt_])
                                fw.op("dve", lambda e: e.scalar_tensor_tensor(out=gts[:], in0=eq2[:], scalar=mm_[:, 6:7], in1=gts[:], op0=ALU.mult, op1=ALU.add), reads=[Brt_], writes=[Brt_])
                                fw.op("pe", lambda e: e.matmul(ps_all[0:8, 3, 0:128], lhsT=gts[:], rhs=ident_f[:], start=True, stop=True), reads=[Brt_, Bidentf], writes=[PB[3]])
                                fw.op("dve", lambda e: e.tensor_copy(out=gT[:, tok0:tok0 + 128], in_=ps_all[0:8, 3, 0:128]), reads=[PB[3]], writes=[BgT])
                    fw.barrier()
                if stop == "p4":
                    if debug and moe:
                        with ExitStack() as ph:
                            dbg = sb("dbg4", [8, NT], F32, ph)
                            Bdbg = Buf()
                            fw.op("dve", lambda e: e.tensor_copy(out=dbg[:], in_=gT[:]), reads=[BgT], writes=[Bdbg])
                            fw.dma("sp", S["dbg_kv"][0:8, 0, :], dbg[:], reads=[Bdbg], writes=[Bout])
                    fw.finish()
                    return nc, list(I.keys())

                with ExitStack() as ph:
                    SBL = [(0, 1536), (1536, 1536), (3072, NT - 3072)]
                    if last:
                        SBL = [(NCX, 1536), (NCX + 1536, 1536), (NCX + 3072, 1024)]
                    SBW = 1536
                    if moe:
                        groups = [(e_, f0, 512) for e_ in range(NEXP) for f0 in range(0, DFE, 512)]
                    else:
                        groups = [(0, f0, min(512, DFF - f0)) for f0 in range(0, DFF, 512)]

                    def wviews(e_):
                        if moe:
                            return (I["moe_wg"][li // 2][e_].rearrange("(j p) f -> p j f", p=128),
                                    I["moe_wu"][li // 2][e_].rearrange("(j p) f -> p j f", p=128),
                                    I["moe_wd"][li // 2][e_].rearrange("(c p) d -> p c d", p=128))
                        return (I["ffn_wg"][li // 2].rearrange("(j p) f -> p j f", p=128),
                                I["ffn_wu"][li // 2].rearrange("(j p) f -> p j f", p=128),
                                I["ffn_wd"][li // 2].rearrange("(c p) d -> p c d", p=128))
                    wgb = [sb(f"wgb{k}", [128, 8, 512], BF16, ph) for k in range(2)]
                    wub = [sb(f"wub{k}", [128, 8, 512], BF16, ph) for k in range(2)]
                    wdb = [sb(f"wdb{k}", [128, 4, D], BF16, ph) for k in range(2)]
                    Bw = [Buf() for _ in range(2)]
                    u2k = sb("u2k", [128, 8, SBW], BF16, ph)
                    Bu2k = Buf()
                    acc = sb("acc", [128, 8, SBW], F32, ph)
                    Bacc = Buf()
                    Hb = [sb(f"Hb{k}", [128, 4, SBW], BF16, ph) for k in range(2)]
                    BH = [Buf() for _ in range(2)]
                    sgt = [sb(f"sgt{k}", [128, 512], BF16, ph) for k in range(2)]
                    Bsgt = [Buf() for _ in range(2)]
                    gbc = sb("gbc", [128, SBW], BF16, ph)
                    Bgbc = Buf()
                    hres = sb("hres", [128, SBW], F32, ph)
                    Bhres = Buf()
                    import os
                    NSB = int(os.environ.get('DEV_NSB', '3'))
                    NGR = int(os.environ.get('DEV_NGR', '999'))
                    groups = groups[:NGR]

                    Bwd = [Buf() for _ in range(2)]

                    def load_w(gi):
                        e_, f0, nf = groups[gi]
                        wg_, wu_, wd_ = wviews(e_)
                        k = gi % 2
                        fw.dma("pool", wgb[k][:, :, :nf], wg_[:, :, f0:f0 + nf], reads=[Bin], writes=[Bw[k]])
                        fw.dma("pool", wub[k][:, :, :nf], wu_[:, :, f0:f0 + nf], reads=[Bin], writes=[Bw[k]])

                    def load_wd(gi):
                        e_, f0, nf = groups[gi]
                        wg_, wu_, wd_ = wviews(e_)
                        k = gi % 2
                        fw.dma("pool", wdb[k][:, :nf // 128, :], wd_[:, f0 // 128:(f0 + nf) // 128, :], reads=[Bin], writes=[Bwd[k]])

                    for sbi in range(NSB):
                        t0, sw = SBL[sbi]
                        subs = [(s0, min(512, sw - s0)) for s0 in range(0, sw, 512)]
                        fw.dma("sp", u2k[:, :, :sw], S["u2"][:, :, t0:t0 + sw], reads=[B["u2"]], writes=[Bu2k])
                        fw.op("pool", lambda e: e.memset(acc[:], 0.0), writes=[Bacc])
                        load_w(0)
                        load_wd(0)
                        cnt5 = {"sg": 0, "ob": 0}

                        def phaseB_units(gi):
                            e_, f0, nf = groups[gi]
                            k = gi % 2
                            nfc = nf // 128
                            units = []
                            for dc in range(8):
                                for (s0, ns) in subs:
                                    units.append((k, nfc, dc, s0, ns))
                            return units

                        def emitB(unit):
                            k, nfc, dc, s0, ns = unit
                            ob = 6 + cnt5["ob"] % 2
                            cnt5["ob"] += 1
                            mm_group(ps_all[:, ob, :ns], [(wdb[k][:, fc2, dc * 128:(dc + 1) * 128], Hb[k][:, fc2, s0:s0 + ns]) for fc2 in range(nfc)], [Bwd[k], BH[k]], PB[ob])
                            fw.op("dve", lambda e: e.tensor_tensor(out=acc[:, dc, s0:s0 + ns], in0=acc[:, dc, s0:s0 + ns], in1=ps_all[:, ob, :ns], op=ALU.add),
                                  reads=[Bacc, PB[ob]], writes=[Bacc])

                        pendingB = []
                        cur_e = -1
                        for gi, (e_, f0, nf) in enumerate(groups):
                            if gi + 1 < len(groups):
                                load_w(gi + 1)
                            k = gi % 2
                            nfc = nf // 128
                            if moe and e_ != cur_e:
                                cur_e = e_
                                for (s0, ns) in subs:
                                    fw.op("pe", lambda e: e.matmul(ps_all[:, 7, :ns], lhsT=sel_bf[:, e_, :], rhs=gT[:, t0 + s0:t0 + s0 + ns], start=True, stop=True),
                                          reads=[Bsel, BgT], writes=[PB[7]])
                                    fw.op("act", lambda e: e.copy(out=gbc[:, s0:s0 + ns], in_=ps_all[:, 7, :ns]), reads=[PB[7]], writes=[Bgbc])
                            per_fc = (len(pendingB) + nfc - 1) // nfc if pendingB else 0
                            for fc in range(nfc):
                                for si, (s0, ns) in enumerate(subs):
                                    gbk, ubk = (2 + si, 4 + si) if si < 2 else (0, 1)
                                    gcol = 0
                                    ucol = 0
                                    mm_group(ps_all[:, gbk, gcol:gcol + ns], [(wgb[k][:, j, fc * 128:(fc + 1) * 128], u2k[:, j, s0:s0 + ns]) for j in range(8)], [Bw[k], Bu2k], PB[gbk])
                                    mm_group(ps_all[:, ubk, ucol:ucol + ns], [(wub[k][:, j, fc * 128:(fc + 1) * 128], u2k[:, j, s0:s0 + ns]) for j in range(8)], [Bw[k], Bu2k], PB[ubk])
                                    sgi = cnt5["sg"] % 2
                                    cnt5["sg"] += 1
                                    fw.op("act", lambda e: e.activation(out=sgt[sgi][:, :ns], in_=ps_all[:, gbk, gcol:gcol + ns], func=AF.Silu), reads=[PB[gbk]], writes=[Bsgt[sgi]])
                                    if moe:
                                        fw.op("dve", lambda e: e.tensor_tensor(out=sgt[sgi][:, :ns], in0=sgt[sgi][:, :ns], in1=gbc[:, s0:s0 + ns], op=ALU.mult), reads=[Bsgt[sgi], Bgbc], writes=[Bsgt[sgi]])
                                    fw.op("dve", lambda e: e.tensor_tensor(out=Hb[k][:, fc, s0:s0 + ns], in0=sgt[sgi][:, :ns], in1=ps_all[:, ubk, ucol:ucol + ns], op=ALU.mult),
                                          reads=[Bsgt[sgi], PB[ubk]], writes=[BH[k]])
                                for _ in range(per_fc):
                                    if pendingB:
                                        emitB(pendingB.pop(0))
                            while pendingB:
                                emitB(pendingB.pop(0))
                            if gi + 1 < len(groups):
                                load_wd(gi + 1)
                            pendingB = phaseB_units(gi)
                        while pendingB:
                            emitB(pendingB.pop(0))
                        for dc in range(8):
                            fw.dma("sp", hres[:, :sw], S["hT"][:, dc, t0:t0 + sw], reads=[B["hT"]], writes=[Bhres])
                            rngs = []
                            if t0 < NCX:
                                rngs.append((0, NCX - t0, 1))
                                rngs.append((NCX - t0, sw, 0))
                            else:
                                rngs.append((0, sw, 0))
                            for (c0, c1, seg) in rngs:
                                fw.op("dve", lambda e: e.scalar_tensor_tensor(out=hres[:, c0:c1], in0=acc[:, dc, c0:c1], scalar=prm[:, 5, dc, seg:seg + 1], in1=hres[:, c0:c1], op0=ALU.mult, op1=ALU.add),
                                      reads=[Bacc, Bprm, Bhres], writes=[Bhres])
                            fw.dma("sp", S["hT"][:, dc, t0:t0 + sw], hres[:, :sw], reads=[Bhres], writes=[B["hT"]])
                    fw.barrier()
            if stop == f"l{li}":
                fw.finish()
                return nc, list(I.keys())

        with ExitStack() as ph:
            hblk = sb("hblkF", [128, 8, 512], F32, ph)
            Bhblk = Buf()
            sq = sb("sqF", [128, 8, 512], BF16, ph)
            Bsq = Buf()
            rt = sb("rtF", [128, 512], F32, ph)
            Brt = Buf()
            hn = sb("hnF", [128, 8, 512], F32, ph)
            Bhn = Buf()
            ot = [sb(f"otF{k}", [128, D], F32, ph) for k in range(2)]
            Bot = [Buf() for _ in range(2)]
            kk = 0
            for (t0, n, seg) in TBLK[1:]:
                fw.dma("sp", hblk[:, :, :n], S["hT"][:, :, t0:t0 + n], reads=[B["hT"]], writes=[Bhblk])
                fw.op("act", lambda e: e.activation(out=sq[:, :, :n], in_=hblk[:, :, :n], func=AF.Square), reads=[Bhblk], writes=[Bsq])
                mm_group(ps_all[:, 0, :n], [(onesD[:], sq[:, j, :n]) for j in range(8)], [BonesD, Bsq], PB[0])
                rstd_from_ps(ps_all[:, 0, :n], rt[:, :n], Brt, PB[0])
                for j in range(8):
                    fw.op("dve", lambda e: e.scalar_tensor_tensor(out=hn[:, j, :n], in0=hblk[:, j, :n], scalar=gfin[:, j:j + 1], in1=rt[:, :n], op0=ALU.mult, op1=ALU.mult),
                          reads=[Bhblk, Bgfin, Brt], writes=[Bhn])
                for tt in range(n // 128):
                    o_, Bo_ = ot[kk % 2], Bot[kk % 2]
                    for half in range(2):
                        bk = 2 + 2 * (kk % 2) + half
                        for jj in range(4):
                            j = half * 4 + jj
                            fw.op("pe", lambda e: e.matmul(ps_all[:, bk, jj * 128:(jj + 1) * 128], lhsT=hn[:, j, tt * 128:(tt + 1) * 128], rhs=ident_f[:], start=True, stop=True),
                                  reads=[Bhn, Bidentf], writes=[PB[bk]], same=False, inc=(jj == 3))
                        if half == 0:
                            fw.op("dve", lambda e: e.tensor_copy(out=o_[:, 0:512], in_=ps_all[:, bk, :]), reads=[PB[bk]], writes=[Bo_])
                        else:
                            fw.op("act", lambda e: e.copy(out=o_[:, 512:1024], in_=ps_all[:, bk, :]), reads=[PB[bk]], writes=[Bo_])
                    r0 = t0 - NCX + tt * 128
                    fw.dma("sp", out[r0:r0 + 128, :], o_[:], reads=[Bo_], writes=[Bout])
                    kk += 1
        fw.finish()
        return nc, list(I.keys())

    return nc, list(I.keys())


def _in_map(inputs, b, consts, names, small=None):
    small = small if small is not None else _layer_small(inputs, b)
    m = {}
    for k in names:
        if k == "x":
            m[k] = np.ascontiguousarray(inputs["x"][b], dtype=np.float32)
        elif k == "ctx":
            m[k] = np.ascontiguousarray(inputs["ctx"][b], dtype=np.float32)
        elif k in small:
            m[k] = small[k]
        elif k in consts:
            m["k_" + k] = consts[k]
        else:
            m[k] = np.ascontiguousarray(inputs[k], dtype=np.float32)
    return m


def kernel(**inputs):
    consts = _constants()
    nc, names = build_program()
    big = {k: np.ascontiguousarray(inputs[k], dtype=np.float32) for k in names if k in BIG_KEYS}
    in_maps = []
    for b in range(8):
        m = _in_map(inputs, b, consts, [k for k in names if k not in BIG_KEYS])
        m.update(big)
        in_maps.append(m)
    res = run_bass_kernel_spmd(nc, in_maps, core_ids=list(range(8)))
    return np.stack([np.asarray(r["out"], np.float32) for r in res.results], axis=0)
```
